# Optimizing a Trainium2 kernel written in Bass

```python
import math
import jax, jax.numpy as jnp
from jax import lax
import numpy as np


D_MODEL = 1024
BATCH = 8
SEQ = 2048
DEPTH = 4

CHUNK = 64
D_MIX = D_MODEL
D_MLSTM = D_MIX // 2
D_S5 = D_MIX - D_MLSTM
MLSTM_HEADS = 4
MLSTM_HEAD_DIM = D_MLSTM // MLSTM_HEADS
CONV_WIDTH = 4
S5_GROUP = 16
S5_GROUPS = D_S5 // S5_GROUP
S5_STATE = 64
PLE_DIM = 256
N_EXPERT_GROUPS = 4
EXPERTS_PER_GROUP = 8
N_EXPERTS = N_EXPERT_GROUPS * EXPERTS_PER_GROUP
TOP_K = 2
D_EXPERT = D_MODEL // 2
MOE_BLOCK = 128
D_IN = 3 * D_MLSTM + 2 * MLSTM_HEADS + D_S5
DEEPNORM_ALPHA = (2 * DEPTH) ** 0.25
DEEPNORM_BETA = (8 * DEPTH) ** -0.25
EPS = 1e-5

kernel_name = 'hybrid_mlstm_s5_hmoe_deepnorm'


def layer_norm(x, g, b):
    xf = x.astype(jnp.float32)
    mu = xf.mean(-1, keepdims=True)
    var = jnp.square(xf - mu).mean(-1, keepdims=True)
    y = (xf - mu) * lax.rsqrt(var + EPS) * g.astype(jnp.float32) + b.astype(jnp.float32)
    return y.astype(x.dtype)


def rms_norm(x, g):
    xf = x.astype(jnp.float32)
    y = xf * lax.rsqrt(jnp.mean(xf * xf, -1, keepdims=True) + EPS) * g.astype(jnp.float32)
    return y.astype(x.dtype)


def head_norm(h, g):
    B, S, H, Dh = h.shape
    mu = h.mean(-1, keepdims=True)
    var = jnp.square(h - mu).mean(-1, keepdims=True)
    y = ((h - mu) * lax.rsqrt(var + EPS)).reshape(B, S, H * Dh)
    return y * g.astype(jnp.float32)


def causal_conv(u, w, b):
    C = u.shape[-1]
    y = lax.conv_general_dilated(u, w[:, None, :].astype(u.dtype), window_strides=(1,),
                                 padding=[(CONV_WIDTH - 1, 0)],
                                 dimension_numbers=('NWC', 'WIO', 'NWC'),
                                 feature_group_count=C)
    return y + b.astype(u.dtype)


def mlstm_chunkwise(q, k, v, i_pre, f_pre):
    B, S, H, Dh = q.shape
    NC = S // CHUNK
    f32 = jnp.float32

    def chunks(t):
        return t.astype(f32).reshape(B, NC, CHUNK, H, -1).transpose(0, 3, 1, 2, 4)

    qc, kc, vc = chunks(q), chunks(k), chunks(v)
    ig = chunks(i_pre[..., None])[..., 0]
    lf = jax.nn.log_sigmoid(chunks(f_pre[..., None])[..., 0])
    bcum = jnp.cumsum(lf, axis=-1)
    g = bcum[..., -1]
    a = g[..., None] - bcum + ig
    m_loc = a.max(-1)
    wk = jnp.exp(a - m_loc[..., None])[..., None] * kc
    chunk_C = jnp.einsum('bhcsk,bhcsv->bhckv', wk, vc)
    chunk_n = wk.sum(-2)

    def step(carry, inp):
        C, n, m = carry
        g_c, ml, Cc, nc = inp
        m_new = jnp.maximum(g_c + m, ml)
        s_old = jnp.exp(g_c + m - m_new)
        s_new = jnp.exp(ml - m_new)
        C_new = s_old[..., None, None] * C + s_new[..., None, None] * Cc
        n_new = s_old[..., None] * n + s_new[..., None] * nc
        return (C_new, n_new, m_new), (C, n, m)

    init = (jnp.zeros((B, H, Dh, Dh), f32), jnp.zeros((B, H, Dh), f32), jnp.zeros((B, H), f32))
    xs = (jnp.moveaxis(g, 2, 0), jnp.moveaxis(m_loc, 2, 0),
          jnp.moveaxis(chunk_C, 2, 0), jnp.moveaxis(chunk_n, 2, 0))
    _, (C_prev, n_prev, m_prev) = lax.scan(step, init, xs)
    C_prev = jnp.moveaxis(C_prev, 0, 2)
    n_prev = jnp.moveaxis(n_prev, 0, 2)
    m_prev = jnp.moveaxis(m_prev, 0, 2)

    causal = jnp.tril(jnp.ones((CHUNK, CHUNK), bool))
    log_d = jnp.where(causal, bcum[..., :, None] - bcum[..., None, :] + ig[..., None, :], -jnp.inf)
    log_inter = bcum + m_prev[..., None]
    m_t = jnp.maximum(log_inter, log_d.max(-1))
    s = jnp.einsum('bhctd,bhcsd->bhcts', qc, kc) * jnp.exp(log_d - m_t[..., None])
    inter = jnp.exp(log_inter - m_t)
    num = (jnp.einsum('bhcts,bhcsv->bhctv', s, vc)
           + inter[..., None] * jnp.einsum('bhctk,bhckv->bhctv', qc, C_prev))
    den = s.sum(-1) + inter * jnp.einsum('bhctk,bhck->bhct', qc, n_prev)
    den = jnp.maximum(jnp.abs(den), jnp.exp(-m_t))
    h = num / den[..., None]
    return h.transpose(0, 2, 3, 1, 4).reshape(B, S, H, Dh)


def complex_affine(e1, e2):
    a1r, a1i, b1r, b1i = e1
    a2r, a2i, b2r, b2i = e2
    return (a2r * a1r - a2i * a1i, a2r * a1i + a2i * a1r,
            a2r * b1r - a2i * b1i + b2r, a2r * b1i + a2i * b1r + b2i)


def s5_glu(u, lam_re, lam_im, log_dt, b_re, b_im, c_re, c_im, d_skip, w_glu, b_glu):
    B, S, _ = u.shape
    f32 = jnp.float32
    uf = u.astype(f32).reshape(B, S, S5_GROUPS, S5_GROUP)
    lr, li = lam_re.astype(f32), lam_im.astype(f32)
    dt = jnp.exp(log_dt.astype(f32))[:, None]
    er = jnp.exp(lr * dt)
    ar, ai = er * jnp.cos(li * dt), er * jnp.sin(li * dt)
    mag2 = lr * lr + li * li
    xr, xi = ar - 1.0, ai
    cr = (xr * lr + xi * li) / mag2
    ci = (xi * lr - xr * li) / mag2
    br, bi = b_re.astype(f32), b_im.astype(f32)
    bbr = cr[..., None] * br - ci[..., None] * bi
    bbi = cr[..., None] * bi + ci[..., None] * br
    bur = jnp.einsum('bsgc,gpc->bsgp', uf, bbr)
    bui = jnp.einsum('bsgc,gpc->bsgp', uf, bbi)
    a_r = jnp.broadcast_to(ar, bur.shape)
    a_i = jnp.broadcast_to(ai, bur.shape)
    _, _, sr, si = lax.associative_scan(complex_affine, (a_r, a_i, bur, bui), axis=1)
    y = (jnp.einsum('bsgp,gcp->bsgc', sr, c_re.astype(f32))
         - jnp.einsum('bsgp,gcp->bsgc', si, c_im.astype(f32))
         + d_skip.astype(f32) * uf).reshape(B, S, D_S5)
    gy = jax.nn.gelu(y)
    out = gy * jax.nn.sigmoid(gy @ w_glu.astype(f32) + b_glu.astype(f32))
    return out


def mixer(x, w_in, conv_w, conv_b, w_q, w_k, b_i, b_f, mh_g, lam_re, lam_im, log_dt,
          b_re, b_im, c_re, c_im, d_skip, w_glu, b_glu, s5_g, w_out):
    B, S, _ = x.shape
    H, Dh = MLSTM_HEADS, MLSTM_HEAD_DIM
    z = x @ w_in
    u_m, v, o_pre, i_pre, f_pre, u_s = jnp.split(
        z, [D_MLSTM, 2 * D_MLSTM, 3 * D_MLSTM, 3 * D_MLSTM + H, 3 * D_MLSTM + 2 * H], axis=-1)
    c = jax.nn.silu(causal_conv(u_m, conv_w, conv_b)).reshape(B, S, H, Dh)
    q = jnp.einsum('bshd,hde->bshe', c, w_q)
    k = jnp.einsum('bshd,hde->bshe', c, w_k) * (Dh ** -0.5)
    h = mlstm_chunkwise(q, k, v.reshape(B, S, H, Dh), i_pre + b_i, f_pre + b_f)
    h = jax.nn.sigmoid(o_pre.astype(jnp.float32)).reshape(B, S, H, Dh) * h
    y_m = head_norm(h, mh_g).astype(x.dtype)
    y_s = rms_norm(s5_glu(u_s, lam_re, lam_im, log_dt, b_re, b_im, c_re, c_im, d_skip,
                          w_glu, b_glu), s5_g).astype(x.dtype)
    return jnp.concatenate([y_m, y_s], axis=-1) @ w_out


def expert_dispatch(xf, e_idx, e_w, w_eg, w_eu, w_ed):
    M, D = xf.shape
    E = w_eg.shape[0]
    A = M * TOP_K
    flat_e = e_idx.reshape(A)
    flat_t = jnp.repeat(jnp.arange(M, dtype=jnp.int32), TOP_K)
    flat_w = e_w.reshape(A)
    order = jnp.argsort(flat_e)
    se, st, sw = flat_e[order], flat_t[order], flat_w[order]
    counts = jnp.bincount(flat_e, length=E)
    starts = jnp.cumsum(counts) - counts
    pcounts = (counts + MOE_BLOCK - 1) // MOE_BLOCK * MOE_BLOCK
    pends = jnp.cumsum(pcounts)
    pstarts = pends - pcounts
    dest = pstarts[se] + (jnp.arange(A) - starts[se])
    NB = -(-A // MOE_BLOCK) + E
    P = NB * MOE_BLOCK
    buf_t = jnp.full((P,), M, jnp.int32).at[dest].set(st)
    buf_w = jnp.zeros((P,), xf.dtype).at[dest].set(sw)
    blk_e = jnp.clip(jnp.searchsorted(pends, jnp.arange(NB) * MOE_BLOCK, side='right'), 0, E - 1)
    xpad = jnp.concatenate([xf, jnp.zeros((1, D), xf.dtype)], axis=0)

    def run_block(args):
        tok, w, e = args
        xb = xpad[tok]
        hb = jax.nn.silu(xb @ w_eg[e]) * (xb @ w_eu[e])
        return (hb @ w_ed[e]) * w[:, None]

    out = lax.map(run_block, (buf_t.reshape(NB, MOE_BLOCK), buf_w.reshape(NB, MOE_BLOCK), blk_e))
    y = jnp.zeros((M + 1, D), xf.dtype).at[buf_t].add(out.reshape(P, D))
    return y[:M]


def moe_ffn(x, w_grp, b_grp, w_rt, b_rt, w_eg, w_eu, w_ed):
    B, S, D = x.shape
    M = B * S
    xf = x.reshape(M, D)
    grp_prob = jax.nn.softmax((xf @ w_grp + b_grp).astype(jnp.float32), axis=-1)
    grp = jnp.argmax(grp_prob, axis=-1).astype(jnp.int32)
    p_grp = jnp.take_along_axis(grp_prob, grp[:, None], axis=-1)
    e_logits = (xf @ w_rt + b_rt).astype(jnp.float32).reshape(M, N_EXPERT_GROUPS, EXPERTS_PER_GROUP)
    e_in = jnp.take_along_axis(e_logits, grp[:, None, None], axis=1)[:, 0]
    top_v, top_i = lax.top_k(e_in, TOP_K)
    e_w = (p_grp * jax.nn.softmax(top_v, axis=-1)).astype(x.dtype)
    e_idx = grp[:, None] * EXPERTS_PER_GROUP + top_i.astype(jnp.int32)
    return expert_dispatch(xf, e_idx, e_w, w_eg, w_eu, w_ed).reshape(B, S, D)


def setup_inputs(seed: int = 0) -> dict:
    key = jax.random.key(seed)
    ks = iter(jax.random.split(key, 48))
    L, H, Dh, G, P = DEPTH, MLSTM_HEADS, MLSTM_HEAD_DIM, S5_GROUPS, S5_STATE

    def nrm(shape, scale):
        return scale * jax.random.normal(next(ks), shape, jnp.float32)

    d = {}
    d['x'] = nrm((BATCH, SEQ, D_MODEL), 1.0)
    d['p'] = nrm((DEPTH, BATCH, SEQ, PLE_DIM), 1.0)
    d['w_in'] = nrm((L, D_MODEL, D_IN), D_MODEL ** -0.5)
    d['conv_w'] = nrm((L, CONV_WIDTH, D_MLSTM), CONV_WIDTH ** -0.5)
    d['conv_b'] = nrm((L, D_MLSTM), 0.02)
    d['w_q'] = nrm((L, H, Dh, Dh), Dh ** -0.5)
    d['w_k'] = nrm((L, H, Dh, Dh), Dh ** -0.5)
    d['b_i'] = nrm((L, H), 0.1)
    d['b_f'] = jnp.linspace(3.0, 6.0, H, dtype=jnp.float32)[None, :] + nrm((L, H), 0.1)
    d['mh_g'] = 1.0 + nrm((L, D_MLSTM), 0.02)
    d['lam_re'] = -0.5 + nrm((L, G, P), 0.01)
    d['lam_im'] = jnp.pi * jnp.arange(P, dtype=jnp.float32)[None, None, :] + nrm((L, G, P), 0.01)
    d['log_dt'] = jax.random.uniform(next(ks), (L, G), jnp.float32, math.log(1e-3), math.log(1e-1))
    d['b_re'] = nrm((L, G, P, S5_GROUP), (2 * S5_GROUP) ** -0.5)
    d['b_im'] = nrm((L, G, P, S5_GROUP), (2 * S5_GROUP) ** -0.5)
    d['c_re'] = nrm((L, G, S5_GROUP, P), (2 * P) ** -0.5)
    d['c_im'] = nrm((L, G, S5_GROUP, P), (2 * P) ** -0.5)
    d['d_skip'] = nrm((L, G, S5_GROUP), 1.0)
    d['w_glu'] = nrm((L, D_S5, D_S5), D_S5 ** -0.5)
    d['b_glu'] = nrm((L, D_S5), 0.02)
    d['s5_g'] = 1.0 + nrm((L, D_S5), 0.02)
    d['w_out'] = nrm((L, D_MIX, D_MODEL), DEEPNORM_BETA * D_MIX ** -0.5)
    d['ln1_g'] = 1.0 + nrm((L, D_MODEL), 0.02)
    d['ln1_b'] = nrm((L, D_MODEL), 0.02)
    d['w_grp'] = nrm((L, D_MODEL, N_EXPERT_GROUPS), D_MODEL ** -0.5)
    d['b_grp'] = nrm((L, N_EXPERT_GROUPS), 0.01)
    d['w_rt'] = nrm((L, D_MODEL, N_EXPERTS), D_MODEL ** -0.5)
    d['b_rt'] = nrm((L, N_EXPERTS), 0.01)
    d['w_eg'] = nrm((L, N_EXPERTS, D_MODEL, D_EXPERT), D_MODEL ** -0.5)
    d['w_eu'] = nrm((L, N_EXPERTS, D_MODEL, D_EXPERT), D_MODEL ** -0.5)
    d['w_ed'] = nrm((L, N_EXPERTS, D_EXPERT, D_MODEL), DEEPNORM_BETA * D_EXPERT ** -0.5)
    d['ln2_g'] = 1.0 + nrm((L, D_MODEL), 0.02)
    d['ln2_b'] = nrm((L, D_MODEL), 0.02)
    d['w_pg'] = nrm((L, D_MODEL, D_MODEL), D_MODEL ** -0.5)
    d['b_pg'] = nrm((L, D_MODEL), 0.02)
    d['w_pp'] = nrm((L, PLE_DIM, D_MODEL), PLE_DIM ** -0.5)
    d['ple_g'] = 1.0 + nrm((L, D_MODEL), 0.02)
    return d


def reference(x, p, w_in, conv_w, conv_b, w_q, w_k, b_i, b_f, mh_g, lam_re, lam_im, log_dt,
              b_re, b_im, c_re, c_im, d_skip, w_glu, b_glu, s5_g, w_out, ln1_g, ln1_b,
              w_grp, b_grp, w_rt, b_rt, w_eg, w_eu, w_ed, ln2_g, ln2_b, w_pg, b_pg, w_pp, ple_g):
    for i in range(DEPTH):
        mix = mixer(x, w_in[i], conv_w[i], conv_b[i], w_q[i], w_k[i], b_i[i], b_f[i], mh_g[i],
                    lam_re[i], lam_im[i], log_dt[i], b_re[i], b_im[i], c_re[i], c_im[i],
                    d_skip[i], w_glu[i], b_glu[i], s5_g[i], w_out[i])
        x = layer_norm(DEEPNORM_ALPHA * x + mix, ln1_g[i], ln1_b[i])
        ffn = moe_ffn(x, w_grp[i], b_grp[i], w_rt[i], b_rt[i], w_eg[i], w_eu[i], w_ed[i])
        x = layer_norm(DEEPNORM_ALPHA * x + ffn, ln2_g[i], ln2_b[i])
        gate = jax.nn.sigmoid(x @ w_pg[i] + b_pg[i])
        ple = rms_norm(p[i] @ w_pp[i], ple_g[i])
        x = x + gate * ple
    return x
```

```python
import math
import bisect
import os as _os
import numpy as np
import concourse.bass as bass
import concourse.mybir as mybir
from concourse.bass_utils import run_bass_kernel_spmd
from contextlib import ExitStack

F32 = mybir.dt.float32
BF16 = mybir.dt.bfloat16
ALU = mybir.AluOpType
AF = mybir.ActivationFunctionType

D = 1024; S = 2048; NT = 16; DEPTH = 4
D_IN = 2056
ALPHA = (2 * DEPTH) ** 0.25
EPS = 1e-5
PI = math.pi


class T:
    def __init__(s, ap, name):
        s.ap = ap; s.name = name; s.w = []; s.r = []; s.dsem = None; s.dval = 0

    def __getitem__(s, idx):
        return s.ap[idx]


class K:
    def __init__(s, nc, es, needed=None):
        s.nc = nc; s.es = es
        s.dry = needed is None
        s.needed = needed or {}
        s.needed_set = {e: set(v) for e, v in s.needed.items()}
        s.waited = {}
        s.E = {'pe': nc.tensor, 'dve': nc.vector, 'act': nc.scalar, 'pool': nc.gpsimd, 'sp': nc.sync}
        s.sem = {e: es.enter_context(nc.semaphore('sem_' + e)) for e in s.E}
        s.cnt = {e: 0 for e in s.E}
        s.known = {e: {} for e in s.E}
        s.dma_tiles = []
        s.nt = 0

    def tile(s, ap, name=None):
        s.nt += 1
        return T(ap, name or ('t%d' % s.nt))

    def sb(s, name, shape, dt):
        return s.es.enter_context(s.nc.sbuf_tensor(name, shape, dt))

    def _wait(s, eng, ev):
        sem, val, deng, key = ev
        if s.known[eng].get(key, 0) >= val:
            return
        s.known[eng][key] = val
        if deng is not None:
            s.waited.setdefault(deng, set()).add(val)
            if not s.dry:
                rank = bisect.bisect_right(s.needed[deng], val)
                s.E[eng].wait_ge(sem, rank)
        elif not s.dry:
            s.E[eng].wait_ge(sem, val)

    def _deps(s, eng, reads, writes, skipkey=None):
        for t in reads:
            for ev in t.w:
                if ev[2] == eng and eng == 'pe':
                    continue
                s._wait(eng, ev)
        for t in writes:
            for ev in t.w + t.r:
                if ev[2] == eng:
                    continue
                if skipkey is not None and ev[3] == skipkey:
                    continue
                s._wait(eng, ev)

    def op(s, eng, fn, reads=(), writes=()):
        s._deps(eng, reads, writes)
        s.cnt[eng] += 1
        if not s.dry:
            ins = fn(s.E[eng])
            if s.cnt[eng] in s.needed_set.get(eng, ()):
                ins.then_inc(s.sem[eng], 1)
        ev = (s.sem[eng], s.cnt[eng], eng, eng)
        for t in writes:
            t.w = [ev]; t.r = []
        for t in reads:
            if t in writes:
                continue
            t.r = [e for e in t.r if e[3] != eng] + [ev]

    def dma(s, q, out_ap, in_ap, src, dst, parts=False, **kw):
        key = 'd_' + dst.name
        s._deps(q, [src], [dst], skipkey=key if parts else None)
        if dst.dsem is None:
            dst.dsem = True if s.dry else s.es.enter_context(s.nc.semaphore(key))
            s.dma_tiles.append(dst)
        dst.dval += 16
        if not s.dry:
            ins = s.E[q].dma_start(out=out_ap, in_=in_ap, **kw)
            ins.then_inc(dst.dsem, 16)
        ev = (dst.dsem, dst.dval, None, key)
        dst.w = [ev]; dst.r = []
        src.r = [e for e in src.r if e[3] != key] + [ev]

    def dma_scatter(s, out_ap, idx_ap, in_ap, src, idxt, dst, bound):
        key = 'd_' + dst.name
        s._deps('pool', [src, idxt], [dst], skipkey=key)
        if dst.dsem is None:
            dst.dsem = True if s.dry else s.es.enter_context(s.nc.semaphore(key))
            s.dma_tiles.append(dst)
        dst.dval += 16
        ev = (dst.dsem, dst.dval, None, key)
        if not s.dry:
            ins = s.nc.gpsimd.indirect_dma_start(out=out_ap, out_offset=bass.IndirectOffsetOnAxis(ap=idx_ap, axis=0),
                                                 in_=in_ap, in_offset=None, bounds_check=s.bnd_reg, oob_is_err=False)
            ins.then_inc(dst.dsem, 16)
        dst.w = [ev]; dst.r = []
        for t in (src, idxt):
            t.r = [e for e in t.r if e[3] != key] + [ev]

    def barrier(s):
        for e in s.E:
            for e2 in s.E:
                if e2 != e and s.cnt[e2] > 0:
                    s._wait(e, (s.sem[e2], s.cnt[e2], e2, e2))
            for t in s.dma_tiles:
                if t.dval > 0:
                    s._wait(e, (t.dsem, t.dval, None, 'd_' + t.name))

    def mm(s, ps, out_ap, lhsT_ap, rhs_ap, reads, start=True, stop=True):
        s.op('pe', lambda e: e.matmul(out_ap, lhsT=lhsT_ap, rhs=rhs_ap, start=start, stop=stop),
             reads=reads, writes=[ps])

    def tr(s, ps, out_ap, in_ap, ident, reads):
        s.op('pe', lambda e: e.transpose(out_ap, in_ap, ident[:]), reads=list(reads) + [ident], writes=[ps])

    def act(s, out_ap, in_ap, func, reads, writes, bias=None, scale=None, accum_out=None, eng='act'):
        kw = {}
        if bias is not None: kw['bias'] = bias
        if scale is not None: kw['scale'] = scale
        if accum_out is not None: kw['accum_out'] = accum_out
        s.op('act', lambda e: e.activation(out=out_ap, in_=in_ap, func=func, **kw), reads=reads, writes=writes)

    def tt(s, out_ap, in0, in1, op, reads, writes, eng='dve'):
        if eng == 'pool' and _os.environ.get('KPOOL', '0') != '1':
            eng = 'dve'
        s.op(eng, lambda e: e.tensor_tensor(out=out_ap, in0=in0, in1=in1, op=op), reads=reads, writes=writes)

    def ts(s, out_ap, in0, s1, s2, op0, op1, reads, writes, eng='dve'):
        if eng == 'pool' and _os.environ.get('KPOOL', '0') != '1':
            eng = 'dve'
        if op1 is None:
            s.op(eng, lambda e: e.tensor_scalar(out=out_ap, in0=in0, scalar1=s1, scalar2=None, op0=op0),
                 reads=reads, writes=writes)
        else:
            s.op(eng, lambda e: e.tensor_scalar(out=out_ap, in0=in0, scalar1=s1, scalar2=s2, op0=op0, op1=op1),
                 reads=reads, writes=writes)

    def stt(s, out_ap, in0, scalar, in1, op0, op1, reads, writes):
        s.op('dve', lambda e: e.scalar_tensor_tensor(out=out_ap, in0=in0, scalar=scalar, in1=in1, op0=op0, op1=op1),
             reads=reads, writes=writes)

    def rsqrt_eps(s, out_ap, in_ap, reads, writes, pre_scale=1.0):
        s.act(out_ap, in_ap, AF.Ln, reads, writes, bias=s.eps_ap, scale=pre_scale)
        s.act(out_ap, out_ap, AF.Exp, writes, writes, scale=-0.5)

    def copy(s, eng, out_ap, in_ap, reads, writes):
        if eng == 'act':
            s.op('act', lambda e: e.copy(out=out_ap, in_=in_ap), reads=reads, writes=writes)
        else:
            s.op(eng, lambda e: e.tensor_copy(out=out_ap, in_=in_ap), reads=reads, writes=writes)


PARAM_NAMES = ['w_in', 'conv_w', 'conv_b', 'w_q', 'w_k', 'bg', 'mh_g', 'lam_re', 'lam_im', 'logdt_x',
               'brp', 'bip', 'crp', 'cip', 'd_skip', 'w_glu', 'b_glu', 's5_g', 'w_out', 'ln1_g', 'ln1_b',
               'w_r', 'b_r', 'w_eg', 'w_eu', 'w_ed', 'ln2_g', 'ln2_b', 'w_pg', 'b_pg', 'w_pp', 'ple_g']


def build_program(layers=tuple(range(DEPTH)), phases=('A', 'B', 'C'), n_experts=32, dbg=None, L=DEPTH):
    _, waited = _build(layers, phases, n_experts, L, None)
    needed = {e: sorted(v) for e, v in waited.items()}
    nc, _ = _build(layers, phases, n_experts, L, needed)
    return nc


def _build(layers, phases, n_experts, L, needed):
    nc = bass.Bass("TRN2", target_bir_lowering=False)

    declared = []

    def din(name, shape, dt=F32):
        declared.append(name)
        return nc.dram_tensor(name, shape, dt, kind="ExternalInput").ap()

    x_in = din('x', [S, D])
    pT_in = din('pT', [L, 256, S])
    ident_in = din('ident', [128, 128]); tri_in = din('tri', [128, 128]); tau_in = din('tau', [128, 128])
    iota_in = din('iota256', [128, 256]); stri_in = din('stri', [128, 128]); cmeta_in = din('cmeta', [128, 16, 3])
    w_in = din('w_in', [L, D, D_IN]); conv_w = din('conv_w', [L, 4, 512]); conv_b = din('conv_b', [L, 512])
    w_q = din('w_q', [L, 4, 128, 128]); w_k = din('w_k', [L, 4, 128, 128]); bg_in = din('bg', [L, 8])
    mh_g = din('mh_g', [L, 512])
    lam_re = din('lam_re', [L, 2048]); lam_im = din('lam_im', [L, 2048]); logdt_x = din('logdt_x', [L, 2048])
    brp = din('brp', [L, 128, 2048]); bip = din('bip', [L, 128, 2048])
    crp = din('crp', [L, 128, 16, 128]); cip = din('cip', [L, 128, 16, 128])
    d_skip = din('d_skip', [L, 512]); w_glu = din('w_glu', [L, 512, 512]); b_glu = din('b_glu', [L, 512])
    s5_g = din('s5_g', [L, 512]); w_out = din('w_out', [L, D, D])
    ln1_g = din('ln1_g', [L, D]); ln1_b = din('ln1_b', [L, D])
    w_r = din('w_r', [L, D, 36]); b_r = din('b_r', [L, 36])
    if 'B' in phases:
        w_eg = din('w_eg', [L, 32, D, 512]); w_eu = din('w_eu', [L, 32, D, 512]); w_ed = din('w_ed', [L, 32, 512, D])
    ln2_g = din('ln2_g', [L, D]); ln2_b = din('ln2_b', [L, D])
    w_pg = din('w_pg', [L, D, D]); b_pg = din('b_pg', [L, D]); w_pp = din('w_pp', [L, 256, D]); ple_g = din('ple_g', [L, D])
    y_out = nc.dram_tensor('y', [S, D], F32, kind="ExternalOutput").ap()
    xs_d = nc.dram_tensor('xs_scr', [S, D], F32, kind="Internal").ap()
    cri_d = nc.dram_tensor('cri_scr', [2, 2048], F32, kind="Internal").ap()
    yd_d = nc.dram_tensor('yd_scr', [4096, D], F32, kind="Internal").ap()

    es = ExitStack()
    with es:
        k = K(nc, es, needed)
        k.bnd_reg = None
        if not k.dry:
            k.bnd_reg = nc.gpsimd.alloc_register('bnd')
            nc.gpsimd.reg_mov(k.bnd_reg, 4095)
        DI = k.tile(None, 'dram_in')
        XS = k.tile(xs_d, 'xs'); CRI = k.tile(cri_d, 'cri'); YO = k.tile(y_out, 'yo'); YD = k.tile(yd_d, 'yd')

        arena = k.sb('arena', [128, 32768], BF16)
        Xv = arena[:].bitcast(F32).rearrange("p (n d) -> p n d", n=NT)
        X = [k.tile(Xv[:, i, :], 'X%d' % i) for i in range(NT)]
        xt_t = k.sb('xt', [128, 8, S], BF16)
        XT = [k.tile(xt_t[:, :, n * 512:(n + 1) * 512], 'XT%d' % n) for n in range(4)]
        ust_t = k.sb('ust', [128, 4, S], BF16)
        UST = [k.tile(ust_t[:, :, n * 512:(n + 1) * 512], 'UST%d' % n) for n in range(4)]
        HT = UST
        ew_t = [k.sb('ew%d' % i, [128, 12288], BF16) for i in range(2)]
        EW = [k.tile(ew_t[i][:], 'EW%d' % i) for i in range(2)]
        cw_t = k.sb('cw', [128, 3, 16, 128], BF16); CW = k.tile(cw_t[:], 'CW')
        ktm_t = k.sb('ktm', [128, NT, 128], BF16); KTM = k.tile(ktm_t[:], 'KTM')
        row_t = [k.sb('row%d' % i, [128, D], F32) for i in range(2)]
        ROW = [k.tile(row_t[i][:], 'ROW%d' % i) for i in range(2)]
        xr_t = k.sb('xr', [128, 8, 128], F32); XR = k.tile(xr_t[:], 'XR')
        XLB = [k.tile(xr_t[:].rearrange("p a b -> p (a b)"), 'XLB0'),
               k.tile(ust_t[:, 0, :].bitcast(F32), 'XLB1')]
        ctmp_v = [xr_t[:].rearrange("p a b -> p (a b)"), ust_t[:, 2, :].bitcast(F32), ust_t[:, 3, :].bitcast(F32)]
        CTMP = [k.tile(ctmp_v[a][:, b * 512:(b + 1) * 512], 'CTMP%d' % (a * 2 + b)) for a in range(3) for b in range(2)]
        gw_t = k.sb('gw', [128, NT, 32], F32); GW = [k.tile(gw_t[:, i, :], 'GW%d' % i) for i in range(NT)]
        ident_t = k.sb('ident_s', [128, 128], F32); IDENT = k.tile(ident_t[:], 'IDENT')
        tri_t = k.sb('tri_s', [128, 128], F32); TRI = k.tile(tri_t[:], 'TRI')
        tau_t = k.sb('tau_s', [128, 128], F32); TAU = k.tile(tau_t[:], 'TAU')
        ones_t = k.sb('ones', [128, 128], F32); ONES = k.tile(ones_t[:], 'ONES')
        onesk_t = k.sb('onesk', [128, 128], F32); ONESK = k.tile(onesk_t[:], 'ONESK')
        sm_t = k.sb('small', [128, 512], F32)
        SM = k.tile(sm_t[:], 'SM')
        sm2_t = k.sb('small2', [128, 512], F32)
        smi_t = k.sb('smi', [128, 16], mybir.dt.int32)
        eps_t = k.sb('eps', [128, 1], F32)
        k.op('pool', lambda e: e.memset(eps_t[:], EPS), writes=[])
        k.eps_ap = eps_t[:]
        wr_t = k.sb('wr_s', [128, 8, 36], F32); WR = k.tile(wr_t[:], 'WR')
        wqk_t = k.sb('wqk', [128, 2, 4, 128], BF16); WQK = k.tile(wqk_t[:], 'WQK')
        gat_t = k.sb('gates', [128, NT, 8], F32); GATES = k.tile(gat_t[:], 'GATES')
        gx_t = k.sb('gx', [128, 5, NT, 4], F32); GX = k.tile(gx_t[:], 'GX')
        cst_t = k.sb('cst', [128, 129], F32); CST = k.tile(cst_t[:], 'CST')
        cbf_t = k.sb('cbf', [128, 129], BF16); CBF = k.tile(cbf_t[:], 'CBF')
        stb_t = k.sb('stb', [128, 2, 128], BF16); STB = [k.tile(stb_t[:, i, :], 'STB%d' % i) for i in range(2)]
        wv_t = k.sb('wv', [128, 2, 129], BF16); WV = [k.tile(wv_t[:, i, :], 'WV%d' % i) for i in range(2)]
        hh_t = k.sb('hh', [128, 2, 128], F32); HH = [k.tile(hh_t[:, i, :], 'HH%d' % i) for i in range(2)]
        ms_t = k.sb('ms', [128, 2, 32], F32); MS = [k.tile(ms_t[:, i, :], 'MS%d' % i) for i in range(2)]
        PS = []
        for b in range(8):
            pt = es.enter_context(nc.psum_tensor('ps%d' % b, [128, 512], F32))
            PS.append(k.tile(pt[:], 'PS%d' % b))

        o0 = 0
        vext_v = arena[:, o0:o0 + NT * 4 * 129].rearrange("p (n h c) -> p n h c", n=NT, h=4); o0 += NT * 4 * 129
        sigo_v = arena[:, o0:o0 + NT * 512].rearrange("p (n c) -> p n c", n=NT); o0 += NT * 512
        um_v = arena[:, o0:o0 + 4 * 2051].rearrange("p (h t) -> p h t", h=4); o0 += 4 * 2052
        c_v = arena[:, o0:o0 + S]; o0 += S
        qt_v = arena[:, o0:o0 + S]; o0 += S
        kt_v = arena[:, o0:o0 + S]; o0 += S
        assert o0 <= 32768, o0
        VEXT = [k.tile(vext_v[:, i], 'VEXT%d' % i) for i in range(NT)]
        SIGO = [k.tile(sigo_v[:, i], 'SIGO%d' % i) for i in range(NT)]
        UM = [k.tile(um_v[:, h], 'UM%d' % h) for h in range(4)]
        CC = k.tile(c_v, 'CC'); QT = k.tile(qt_v, 'QT'); KT = k.tile(kt_v, 'KT')
        o1 = 0
        tp_v = arena[:, 0:12 * 1024].bitcast(F32).rearrange("p (n c) -> p n c", n=12); o1 = 12 * 1024
        TP = [k.tile(tp_v[:, i], 'TP%d' % i) for i in range(12)]
        gyb_v = arena[:, o1:o1 + 2048].rearrange("p (n c) -> p n c", n=4); o1 += 2048
        GYB = k.tile(gyb_v, 'GYB')
        rb_v = arena[:, o1:o1 + 2048].rearrange("p (n c) -> p n c", n=4); o1 += 2048
        RB = [k.tile(rb_v[:, i], 'RB%d' % i) for i in range(4)]
        tab_v = arena[:, o1:o1 + 4 * 16 * 128].rearrange("p (a j t) -> p a j t", a=4, j=16); o1 += 4 * 16 * 128
        TAB = k.tile(tab_v, 'TAB')
        bb_v = arena[:, o1:o1 + 2 * 2048].rearrange("p (a c) -> p a c", a=2); o1 += 4096
        BB = k.tile(bb_v, 'BB')
        tp2_v = arena[:, o1:o1 + 4096].bitcast(F32).rearrange("p (n c) -> p n c", n=4); o1 += 4096
        TP2 = [k.tile(tp2_v[:, i], 'TP2_%d' % i) for i in range(4)]
        assert o1 <= 32768, o1

        iota_t = k.sb('iota_s', [128, 256], F32); IOTA = k.tile(iota_t[:], 'IOTA')
        stri_t = k.sb('stri_s', [128, 128], BF16); STRI = k.tile(stri_t[:], 'STRI')
        onesb_t = k.sb('onesb', [128, 128], BF16); ONESB = k.tile(onesb_t[:], 'ONESB')
        cmeta_t = k.sb('cmeta_s', [128, 16, 3], BF16); CMETA = k.tile(cmeta_t[:], 'CMETA')
        slot_t = k.sb('slot', [128, 2, 2, 8], F32); SLOT = [k.tile(slot_t[:, b], 'SLOT%d' % b) for b in range(2)]
        dest_tt = [[k.sb('dest%d%d' % (b, h), [128, 1], mybir.dt.int32) for h in range(2)] for b in range(2)]
        DEST = [[k.tile(dest_tt[b][h][:, :], 'DEST%d%d' % (b, h)) for h in range(2)] for b in range(2)]
        xtm_v = xt_t[:].rearrange("p k t -> p (k t)").rearrange("p (n d) -> p n d", n=NT)
        XTM = [k.tile(xtm_v[:, i, :], 'XTM%d' % i) for i in range(NT)]
        ustf = ust_t[:].rearrange("p k t -> p (k t)")
        xg_v = [ustf[:, b * 2048:(b + 1) * 2048].rearrange("p (k c) -> p k c", k=8) for b in range(2)]
        XG = [k.tile(xg_v[b], 'XG%d' % b) for b in range(2)]
        hte_v = [ustf[:, 4096 + b * 1024:4096 + (b + 1) * 1024].rearrange("p (k c) -> p k c", k=4) for b in range(2)]
        HTE = [k.tile(hte_v[b], 'HTE%d' % b) for b in range(2)]
        sg_v = [ustf[:, 6144 + b * 512:6144 + (b + 1) * 512].bitcast(F32) for b in range(2)]
        SG = [k.tile(sg_v[b], 'SG%d' % b) for b in range(2)]
        cwf = cw_t[:].rearrange("p a j c -> p (a j c)")
        out_v = [cwf[:, b * 2048:(b + 1) * 2048].bitcast(F32) for b in range(2)]
        OUTB = [k.tile(out_v[b], 'OUTB%d' % b) for b in range(2)]
        meta_v = cwf[:, 4096:4096 + 1024].rearrange("p (n e c) -> p n e c", n=NT, e=32)
        META = k.tile(meta_v, 'META')
        mall_v = sm_t[:, 0:256].bitcast(BF16).rearrange("p (n e) -> p n e", n=NT)
        MALL = k.tile(mall_v, 'MALL')
        sel_v = [row_t[b][:].bitcast(BF16).rearrange("p (n c) -> p n c", n=8) for b in range(2)]
        sync = 'sp'
        k.dma(sync, iota_t[:], iota_in, DI, IOTA)
        k.dma('pool', stri_t[:], stri_in, DI, STRI)
        k.dma('pool', cmeta_t[:], cmeta_in, DI, CMETA)
        k.op('pool', lambda e: e.memset(onesb_t[:], 1.0), writes=[ONESB])
        k.dma(sync, ident_t[:], ident_in, DI, IDENT)
        k.dma(sync, tri_t[:], tri_in, DI, TRI)
        k.dma(sync, tau_t[:], tau_in, DI, TAU)
        k.op('pool', lambda e: e.memset(ones_t[:], 1.0), writes=[ONES])
        k.op('pool', lambda e: e.memset(onesk_t[:], 1.0 / 512.0), writes=[ONESK])
        xin_v = x_in.rearrange("(n p) d -> p n d", p=128)
        for i in range(NT):
            k.dma(sync, X[i][:], xin_v[:, i, :], DI, X[i])

        evac_flip = [0]

        def evac(out_ap, in_ap, reads, writes):
            evac_flip[0] ^= 1
            k.copy('act' if evac_flip[0] else 'dve', out_ap, in_ap, reads, writes)

        def build_xT(extra=None):
            for i in range(NT):
                for half in range(2):
                    ps = PS[(2 * i + half) % 4]
                    for q in range(4):
                        kk = half * 4 + q
                        k.tr(ps, ps[:, q * 128:(q + 1) * 128], X[i][:, kk * 128:(kk + 1) * 128], IDENT, [X[i]])
                    psv = ps[:].rearrange("p (q t) -> p q t", q=4)
                    if extra is None:
                        evac(xt_t[:, half * 4:half * 4 + 4, i * 128:(i + 1) * 128], psv, [ps], [XT[i // 4]])
                    else:
                        k.copy('dve', xr_t[:, half * 4:half * 4 + 4, :], psv, [ps], [XR])
                        k.copy('act', xt_t[:, half * 4:half * 4 + 4, i * 128:(i + 1) * 128], xr_t[:, half * 4:half * 4 + 4, :],
                               [XR], [XT[i // 4]])
                if extra is not None:
                    extra(i)

        def load_row(row, vec_ap):
            k.dma(sync, row[:], vec_ap.partition_broadcast(128), DI, row)

        def layer_norm_tile(i, G, Bt):
            st = MS[i % 2]
            xi = X[i]
            k.op('dve', lambda e: e.bn_stats(out=st[:, 0:6], in_=xi[:, 0:512]), reads=[xi], writes=[st])
            k.op('dve', lambda e: e.bn_stats(out=st[:, 6:12], in_=xi[:, 512:1024]), reads=[xi, st], writes=[st])
            k.op('dve', lambda e: e.bn_aggr(out=st[:, 12:14], in_=st[:, 0:12]), reads=[st], writes=[st])
            k.rsqrt_eps(st[:, 14:15], st[:, 13:14], [st], [st])
            k.ts(xi[:], xi[:], st[:, 12:13], st[:, 14:15], ALU.subtract, ALU.mult, [xi, st], [xi])
            k.tt(xi[:], xi[:], G[:], ALU.mult, [xi, G], [xi], eng='pool')
            k.tt(xi[:], xi[:], Bt[:], ALU.add, [xi, Bt], [xi], eng='pool')

        for li in layers:
            if 'A' in phases:
                build_xT()
                for i in range(NT):
                    k.dma(sync, xs_d.rearrange("(n p) d -> p n d", p=128)[:, i, :], X[i][:], X[i], XS, parts=True)
                k.barrier()
                for j in range(4):
                    k.dma(sync, sm_t[:, j * 4:j * 4 + 4], conv_w[li][j].rearrange("(h p) -> p h", p=128), DI, SM,
                          parts=(j > 0), allow_slow_non_contiguous=True)
                for (c0, src) in ((16, conv_b), (20, mh_g), (24, d_skip), (28, b_glu), (32, s5_g)):
                    k.dma(sync, sm_t[:, c0:c0 + 4], src[li].rearrange("(h p) -> p h", p=128), DI, SM, parts=True,
                          allow_slow_non_contiguous=True)
                k.dma(sync, sm_t[:, 40:48], bg_in[li].partition_broadcast(128), DI, SM, parts=True)
                k.dma('pool', wqk_t[:, 0], w_q[li].rearrange("h d e -> d h e"), DI, WQK)
                k.dma('pool', wqk_t[:, 1], w_k[li].rearrange("h d e -> d h e"), DI, WQK, parts=True)

                def load_w(buf, col0, ncols):
                    k.dma('pool', ew_t[buf][:, 0:8 * ncols].rearrange("p (k c) -> p k c", k=8),
                          w_in[li][:, col0:col0 + ncols].rearrange("(k p) c -> p k c", p=128), DI, EW[buf],
                          allow_slow_non_contiguous=(ncols < 128))
                    return ew_t[buf][:, 0:8 * ncols].rearrange("p (k c) -> p k c", k=8)

                def fm_piece(buf, col0, dest_fn, dest_tiles):
                    W = load_w(buf, col0, 512)
                    for m in range(4):
                        for n in range(4):
                            ps = PS[(m * 4 + n) % 4]
                            for kk in range(8):
                                k.mm(ps, ps[:], W[:, kk, m * 128:(m + 1) * 128], xt_t[:, kk, n * 512:(n + 1) * 512],
                                     [EW[buf], XT[n]], start=(kk == 0), stop=(kk == 7))
                            evac(dest_fn(m, n), ps[:], [ps], [dest_tiles(m, n)])

                fm_piece(0, 1544, lambda m, n: ust_t[:, m, n * 512:(n + 1) * 512], lambda m, n: UST[n])
                for h in range(4):
                    k.op('pool', lambda e, h=h: e.memset(um_v[:, h, 0:3], 0.0), writes=[UM[h]])
                fm_piece(1, 0, lambda m, n: um_v[:, m, 3 + n * 512:3 + (n + 1) * 512], lambda m, n: UM[m])
                W = load_w(0, 512, 512)
                for i in range(NT):
                    ps = PS[i % 4]
                    for kk in range(8):
                        k.mm(ps, ps[:], xt_t[:, kk, i * 128:(i + 1) * 128], W[:, kk, :], [EW[0], XT[i // 4]],
                             start=(kk == 0), stop=(kk == 7))
                    k.op('pool', lambda e, i=i: e.memset(vext_v[:, i, :, 128:129], 1.0), writes=[VEXT[i]])
                    evac(vext_v[:, i, :, 0:128], ps[:].rearrange("p (h c) -> p h c", h=4), [ps], [VEXT[i]])
                W = load_w(1, 1024, 512)
                for i in range(NT):
                    ps = PS[i % 4]
                    for kk in range(8):
                        k.mm(ps, ps[:], xt_t[:, kk, i * 128:(i + 1) * 128], W[:, kk, :], [EW[1], XT[i // 4]],
                             start=(kk == 0), stop=(kk == 7))
                    k.act(sigo_v[:, i, :], ps[:], AF.Sigmoid, [ps], [SIGO[i]])
                W = load_w(0, 1536, 8)
                for i in range(NT):
                    ps = PS[i % 4]
                    for kk in range(8):
                        k.mm(ps, ps[:, 0:8], xt_t[:, kk, i * 128:(i + 1) * 128], W[:, kk, :], [EW[0], XT[i // 4]],
                             start=(kk == 0), stop=(kk == 7))
                    k.tt(gat_t[:, i, :], ps[:, 0:8], sm_t[:, 40:48], ALU.add, [ps, SM], [GATES])

                LF = gx_t[:, 0]; BC = gx_t[:, 1]; GG = gx_t[:, 2]; AA = gx_t[:, 3]; EE = gx_t[:, 4]
                k.act(LF, gat_t[:, :, 4:8], AF.Sigmoid, [GATES], [GX])
                k.act(LF, LF, AF.Ln, [GX], [GX])
                ps = PS[4]
                for i in range(NT):
                    k.mm(ps, ps[:, i * 8:i * 8 + 4], tri_t[:], gx_t[:, 0, i, :], [TRI, GX], start=True, stop=True)
                    k.mm(ps, ps[:, i * 8 + 4:i * 8 + 8], ones_t[:], gx_t[:, 0, i, :], [ONES, GX], start=True, stop=True)
                psv = ps[:, 0:128].rearrange("p (n c) -> p n c", n=NT)
                k.copy('dve', BC, psv[:, :, 0:4], [ps], [GX])
                k.act(GG, psv[:, :, 4:8], AF.Exp, [ps], [GX])
                k.tt(AA, gat_t[:, :, 0:4], BC, ALU.subtract, [GATES, GX], [GX])
                k.act(AA, AA, AF.Exp, [GX], [GX])
                k.act(EE, BC, AF.Exp, [GX], [GX])

                for h in range(4):
                    for n in range(4):
                        acc = ROW[n % 2]
                        k.ts(acc[:, 0:512], um_v[:, h, n * 512:n * 512 + 512], sm_t[:, h:h + 1], None,
                             ALU.mult, None, [UM[h], SM], [acc])
                        for j in range(1, 4):
                            k.stt(acc[:, 0:512], um_v[:, h, n * 512 + j:n * 512 + j + 512],
                                  sm_t[:, j * 4 + h:j * 4 + h + 1], acc[:, 0:512], ALU.mult, ALU.add,
                                  [UM[h], SM, acc], [acc])
                        k.act(c_v[:, n * 512:(n + 1) * 512], acc[:, 0:512], AF.Silu, [acc, SM], [CC],
                              bias=sm_t[:, 16 + h:17 + h])
                    for n in range(4):
                        ps = PS[n % 4]
                        k.mm(ps, ps[:], wqk_t[:, 0, h, :], c_v[:, n * 512:(n + 1) * 512], [WQK, CC])
                        evac(qt_v[:, n * 512:(n + 1) * 512], ps[:], [ps], [QT])
                        ps = PS[(n + 2) % 4]
                        k.mm(ps, ps[:], wqk_t[:, 1, h, :], c_v[:, n * 512:(n + 1) * 512], [WQK, CC])
                        k.act(kt_v[:, n * 512:(n + 1) * 512], ps[:], AF.Copy, [ps], [KT], scale=128.0 ** -0.5)
                    for i4 in range(4):
                        ps = PS[i4 % 4]
                        for q in range(4):
                            i = i4 * 4 + q
                            k.mm(ps, ps[:, q * 128:(q + 1) * 128], c_v[:, i * 128:(i + 1) * 128], wqk_t[:, 1, h, :],
                                 [CC, WQK])
                        k.act(ktm_t[:, i4 * 4:i4 * 4 + 4, :], ps[:].rearrange("p (q c) -> p q c", q=4), AF.Copy,
                              [ps], [KTM], scale=128.0 ** -0.5)
                    k.op('pool', lambda e: e.memset(cst_t[:], 0.0), writes=[CST])
                    k.op('pool', lambda e: e.memset(cbf_t[:], 0.0), writes=[CBF])
                    for i in range(NT):
                        sl = slice(i * 128, (i + 1) * 128)
                        b2 = i % 2
                        psS = PS[4 + b2]; psN = PS[6 + b2]; psC = PS[b2]; psT = PS[2 + b2]
                        st = STB[b2]; wv = WV[b2]; hh = HH[b2]; ms = MS[b2]
                        k.mm(psS, psS[:, 0:128], kt_v[:, sl], qt_v[:, sl], [KT, QT])
                        k.tt(st[:], psS[:, 0:128], tri_t[:], ALU.mult, [psS, TRI], [st])
                        k.act(wv[:], vext_v[:, i, h, :], AF.Copy, [VEXT[i], GX], [wv], scale=gx_t[:, 3, i, h:h + 1])
                        k.mm(psN, psN[:, 0:129], st[:], wv[:], [st, wv], start=True, stop=False)
                        k.mm(psN, psN[:, 0:129], qt_v[:, sl], cbf_t[:], [QT, CBF], start=False, stop=True)
                        k.mm(psC, psC[:, 0:129], ktm_t[:, i, :], wv[:], [KTM, wv])
                        k.ts(ms[:, 3:4], psN[:, 128:129], gx_t[:, 4, i, h:h + 1], None, ALU.mult, None, [psN, GX], [ms])
                        k.stt(ms[:, 0:1], ms[:, 3:4], -1.0, ms[:, 3:4], ALU.mult, ALU.max, [ms], [ms])
                        k.ts(ms[:, 0:1], ms[:, 0:1], 1.0, None, ALU.max, None, [ms], [ms])
                        k.op('dve', lambda e, ms=ms: e.reciprocal(out=ms[:, 1:2], in_=ms[:, 0:1]), reads=[ms], writes=[ms])
                        k.tt(ms[:, 2:3], ms[:, 1:2], gx_t[:, 4, i, h:h + 1], ALU.mult, [ms, GX], [ms])
                        k.stt(hh[:], psN[:, 0:128], ms[:, 2:3], sigo_v[:, i, h * 128:(h + 1) * 128], ALU.mult, ALU.mult,
                              [psN, ms, SIGO[i]], [hh])
                        k.tt(cst_t[:], psC[:, 0:129], cst_t[:], ALU.add, [psC, CST], [CST])
                        k.ts(cst_t[:], cst_t[:], gx_t[:, 2, i, h:h + 1], None, ALU.mult, None, [CST, GX], [CST])
                        k.copy('act', cbf_t[:], cst_t[:], [CST], [CBF])
                        k.op('dve', lambda e, ms=ms, hh=hh: e.bn_stats(out=ms[:, 4:10], in_=hh[:]), reads=[hh, ms], writes=[ms])
                        k.op('dve', lambda e, ms=ms: e.bn_aggr(out=ms[:, 10:12], in_=ms[:, 4:10]), reads=[ms], writes=[ms])
                        k.rsqrt_eps(ms[:, 12:13], ms[:, 11:12], [ms], [ms])
                        k.ts(hh[:], hh[:], ms[:, 10:11], ms[:, 12:13], ALU.subtract, ALU.mult, [hh, ms], [hh])
                        k.tr(psT, psT[:, 0:128], hh[:], IDENT, [hh])
                        k.act(xt_t[:, h, sl], psT[:, 0:128], AF.Copy, [psT, SM], [XT[i // 4]], scale=sm_t[:, 20 + h:21 + h])
                k.barrier()

                for (c0, src) in ((64, lam_re), (80, lam_im), (96, logdt_x)):
                    k.dma(sync, sm_t[:, c0:c0 + 16], src[li].rearrange("(j q) -> q j", q=128), DI, SM, parts=True,
                          allow_slow_non_contiguous=True)
                LR = sm_t[:, 64:80]; LI = sm_t[:, 80:96]; LDT = sm_t[:, 96:112]
                c = lambda a: sm_t[:, a:a + 16]
                DT = c(112); LRDT = c(128); LIDT = c(144); ER = c(160); CO = c(176); SI = c(192); AR = c(208); AI = c(224)
                MAG = c(240); XRr = c(256); T1 = c(272); T2 = c(288); CR = c(304); CI = c(320)
                VC128 = c(336); VS128 = c(352); ZR = c(368); ZI = c(384); ZT = c(400); ZT2 = c(416); T3 = c(432)
                smo = lambda o, a, f, **kw: k.act(o, a, f, [SM], [SM], **kw)
                smt = lambda o, a, b, op: k.tt(o, a, b, op, [SM], [SM])
                sms = lambda o, a, s1, s2, op0, op1: k.ts(o, a, s1, s2, op0, op1, [SM], [SM])
                smo(DT, LDT, AF.Exp)
                smt(LRDT, LR, DT, ALU.mult); smt(LIDT, LI, DT, ALU.mult)
                smo(ER, LRDT, AF.Exp)

                def sincos(o_sin, o_cos, ang, tmp):
                    sms(smi_t[:], ang, 1.0 / (2 * PI), None, ALU.mult, None)
                    k.stt(tmp, smi_t[:], -2 * PI, ang, ALU.mult, ALU.add, [SM], [SM])
                    smo(o_sin, tmp, AF.Sin, scale=0.999999)
                    sms(o_cos, ang, 0.5 * PI, None, ALU.add, None)
                    sms(smi_t[:], o_cos, 1.0 / (2 * PI), None, ALU.mult, None)
                    k.stt(tmp, smi_t[:], -2 * PI, o_cos, ALU.mult, ALU.add, [SM], [SM])
                    smo(o_cos, tmp, AF.Sin, scale=0.999999)
                sincos(SI, CO, LIDT, T1)
                smt(AR, ER, CO, ALU.mult); smt(AI, ER, SI, ALU.mult)
                smt(MAG, LR, LR, ALU.mult); smt(T1, LI, LI, ALU.mult); smt(MAG, MAG, T1, ALU.add)
                k.op('dve', lambda e: e.reciprocal(out=MAG, in_=MAG), reads=[SM], writes=[SM])
                sms(XRr, AR, -1.0, None, ALU.add, None)
                smt(T1, XRr, LR, ALU.mult); smt(T2, AI, LI, ALU.mult); smt(T1, T1, T2, ALU.add); smt(CR, T1, MAG, ALU.mult)
                smt(T1, AI, LR, ALU.mult); smt(T2, XRr, LI, ALU.mult); smt(T1, T1, T2, ALU.subtract); smt(CI, T1, MAG, ALU.mult)
                sms(T3, LIDT, 128.0, None, ALU.mult, None)
                sincos(VS128, VC128, T3, T1)
                smo(T2, LRDT, AF.Exp, scale=128.0)
                smt(VC128, VC128, T2, ALU.mult); smt(VS128, VS128, T2, ALU.mult)
                k.dma(sync, cri_d[0].rearrange("(j q) -> q j", q=128), CR, SM, CRI, allow_slow_non_contiguous=True)
                k.dma(sync, cri_d[1].rearrange("(j q) -> q j", q=128), CI, SM, CRI, parts=True, allow_slow_non_contiguous=True)
                for ct in range(4):
                    cs = slice(ct * 512, (ct + 1) * 512)
                    k.dma(sync, TP[0][:], brp[li][:, cs], DI, TP[0])
                    k.dma(sync, TP[1][:], bip[li][:, cs], DI, TP[1])
                    k.dma(sync, TP[2][:], cri_d[0, cs].partition_broadcast(128), CRI, TP[2])
                    k.dma(sync, TP[3][:], cri_d[1, cs].partition_broadcast(128), CRI, TP[3])
                    k.tt(TP[4][:], TP[2][:], TP[0][:], ALU.mult, [TP[2], TP[0]], [TP[4]])
                    k.tt(TP[5][:], TP[3][:], TP[1][:], ALU.mult, [TP[3], TP[1]], [TP[5]])
                    k.tt(bb_v[:, 0, cs], TP[4][:], TP[5][:], ALU.subtract, [TP[4], TP[5]], [BB])
                    k.tt(TP[4][:], TP[2][:], TP[1][:], ALU.mult, [TP[2], TP[1]], [TP[4]])
                    k.tt(TP[5][:], TP[3][:], TP[0][:], ALU.mult, [TP[3], TP[0]], [TP[5]])
                    k.tt(bb_v[:, 1, cs], TP[4][:], TP[5][:], ALU.add, [TP[4], TP[5]], [BB])
                for jb in range(4):
                    jq = slice(jb * 4, jb * 4 + 4)
                    t1_ = TP[6 + jb % 2]; t2_ = TP[8 + jb % 2]
                    v1_ = t1_[:].rearrange("p (j c) -> p j c", j=4); v2_ = t2_[:].rearrange("p (j c) -> p j c", j=4)
                    k.dma(sync, v1_, crp[li][:, jq, :], DI, t1_)
                    k.dma(sync, v2_, cip[li][:, jq, :], DI, t2_)
                    k.copy('act', cw_t[:, 0, jq, :], v1_, [t1_], [CW])
                    k.act(cw_t[:, 1, jq, :], v1_, AF.Copy, [t1_], [CW], scale=-1.0)
                    k.act(cw_t[:, 2, jq, :], v2_, AF.Copy, [t2_], [CW], scale=-1.0)
                for jb in range(4):
                    jsl = slice(jb * 4, jb * 4 + 4)
                    v3 = lambda t: t[:].rearrange("p (j c) -> p j c", j=4)
                    taub = tau_t[:].unsqueeze(1).broadcast_to([128, 4, 128])
                    lidb = sm_t[:, 144 + jb * 4:144 + jb * 4 + 4].unsqueeze(2).broadcast_to([128, 4, 128])
                    lrdb = sm_t[:, 128 + jb * 4:128 + jb * 4 + 4].unsqueeze(2).broadcast_to([128, 4, 128])
                    ANG, TMPa, SINT, COST, LTt, MAGP, MAGN = TP[0], TP[1], TP[2], TP[3], TP[4], TP[5], TP[6]
                    k.tt(v3(ANG), taub, lidb, ALU.mult, [TAU, SM], [ANG])
                    KI = TP[7]; kiv = KI[:].bitcast(mybir.dt.int32)
                    k.ts(kiv, ANG[:], 1.0 / (2 * PI), None, ALU.mult, None, [ANG], [KI])
                    k.stt(TMPa[:], kiv, -2 * PI, ANG[:], ALU.mult, ALU.add, [KI, ANG], [TMPa])
                    k.act(SINT[:], TMPa[:], AF.Sin, [TMPa], [SINT], scale=0.999999)
                    k.ts(ANG[:], ANG[:], 0.5 * PI, None, ALU.add, None, [ANG], [ANG])
                    k.ts(kiv, ANG[:], 1.0 / (2 * PI), None, ALU.mult, None, [ANG], [KI])
                    k.stt(TMPa[:], kiv, -2 * PI, ANG[:], ALU.mult, ALU.add, [KI, ANG], [TMPa])
                    k.act(COST[:], TMPa[:], AF.Sin, [TMPa], [COST], scale=0.999999)
                    k.tt(v3(LTt), taub, lrdb, ALU.mult, [TAU, SM], [LTt])
                    k.act(MAGP[:], LTt[:], AF.Exp, [LTt], [MAGP])
                    k.act(MAGN[:], LTt[:], AF.Exp, [LTt], [MAGN], scale=-1.0)
                    k.tt(tab_v[:, 0, jsl, :], v3(MAGN), v3(COST), ALU.mult, [MAGN, COST], [TAB])
                    k.tt(tab_v[:, 1, jsl, :], v3(MAGN), v3(SINT), ALU.mult, [MAGN, SINT], [TAB])
                    k.tt(tab_v[:, 2, jsl, :], v3(MAGP), v3(COST), ALU.mult, [MAGP, COST], [TAB])
                    k.tt(tab_v[:, 3, jsl, :], v3(MAGP), v3(SINT), ALU.mult, [MAGP, SINT], [TAB])
                k.dma('pool', ew_t[0][:, 0:2048].rearrange("p (k c) -> p k c", k=4),
                      w_glu[li].rearrange("(k p) c -> p k c", p=128), DI, EW[0])
                WGLU = ew_t[0][:, 0:2048].rearrange("p (k c) -> p k c", k=4)
                k.op('pool', lambda e: e.memset(sm_t[:, 368:400], 0.0), reads=[SM], writes=[SM])
                P1, P2, P3, P4, SR, SIi = TP[0], TP[1], TP[2], TP[3], TP[4], TP[5]
                GYF = TP[6:10]
                YF = TP[10]; TQ = TP[11]
                b4 = lambda ap: ap.unsqueeze(1).broadcast_to([128, 4, 128])
                v4 = lambda t: t[:].rearrange("p (c t) -> p c t", c=4)
                for n in range(4):
                    ns = slice(n * 512, (n + 1) * 512)
                    for ct in range(4):
                        psY = PS[4 + ct % 2]
                        for g2 in range(2):
                            grp = []
                            for q in range(2):
                                jj = 2 * g2 + q
                                j = ct * 4 + jj
                                psR = PS[q]; psI = PS[2 + q]
                                Pq = TP[0:4] if q == 0 else TP2
                                Sq = (TP[4], TP[5]) if q == 0 else (TP[10], TP[11])
                                k.mm(psR, psR[:], bb_v[:, 0, j * 128:(j + 1) * 128], ust_t[:, ct, ns], [BB, UST[n]])
                                k.mm(psI, psI[:], bb_v[:, 1, j * 128:(j + 1) * 128], ust_t[:, ct, ns], [BB, UST[n]])
                                pr = psR[:].rearrange("p (c t) -> p c t", c=4); pi_ = psI[:].rearrange("p (c t) -> p c t", c=4)
                                k.tt(v4(Pq[0]), pr, b4(tab_v[:, 0, j, :]), ALU.mult, [psR, TAB], [Pq[0]])
                                k.tt(v4(Pq[1]), pi_, b4(tab_v[:, 1, j, :]), ALU.mult, [psI, TAB], [Pq[1]])
                                k.tt(v4(Pq[2]), pi_, b4(tab_v[:, 0, j, :]), ALU.mult, [psI, TAB], [Pq[2]])
                                k.tt(v4(Pq[3]), pr, b4(tab_v[:, 1, j, :]), ALU.mult, [psR, TAB], [Pq[3]])
                                grp.append((jj, j, Pq, Sq))
                            for cc in range(4):
                                cs = slice(cc * 128, (cc + 1) * 128)
                                last = cc * 128 + 127
                                for (jj, j, Pq, Sq) in grp:
                                    k.op('dve', lambda e, cs=cs, j=j, Pq=Pq, Sq=Sq: e.tensor_tensor_scan(
                                        out=Sq[0][:, cs], data0=Pq[0][:, cs], data1=Pq[1][:, cs], initial=sm_t[:, 368 + j:369 + j],
                                        op0=ALU.add, op1=ALU.add), reads=[Pq[0], Pq[1], SM], writes=[Sq[0]])
                                    k.op('dve', lambda e, cs=cs, j=j, Pq=Pq, Sq=Sq: e.tensor_tensor_scan(
                                        out=Sq[1][:, cs], data0=Pq[2][:, cs], data1=Pq[3][:, cs], initial=sm_t[:, 384 + j:385 + j],
                                        op0=ALU.add, op1=ALU.subtract), reads=[Pq[2], Pq[3], SM], writes=[Sq[1]])
                                for (jj, j, Pq, Sq) in grp:
                                    k.ts(sm_t[:, 400 + j:401 + j], Sq[1][:, last:last + 1], sm_t[:, 352 + j:353 + j], None,
                                         ALU.mult, None, [Sq[1], SM], [SM])
                                    k.ts(sm_t[:, 416 + j:417 + j], Sq[0][:, last:last + 1], sm_t[:, 352 + j:353 + j], None,
                                         ALU.mult, None, [Sq[0], SM], [SM])
                                for (jj, j, Pq, Sq) in grp:
                                    k.stt(sm_t[:, 368 + j:369 + j], Sq[0][:, last:last + 1], sm_t[:, 336 + j:337 + j],
                                          sm_t[:, 400 + j:401 + j], ALU.mult, ALU.subtract, [Sq[0], SM], [SM])
                                    k.stt(sm_t[:, 384 + j:385 + j], Sq[1][:, last:last + 1], sm_t[:, 336 + j:337 + j],
                                          sm_t[:, 416 + j:417 + j], ALU.mult, ALU.add, [Sq[1], SM], [SM])
                            for (jj, j, Pq, Sq) in grp:
                                SRq, SIq = Sq
                                k.tt(v4(RB[0]), v4(SRq), b4(tab_v[:, 2, j, :]), ALU.mult, [SRq, TAB], [RB[0]], eng='pool')
                                k.tt(v4(RB[1]), v4(SIq), b4(tab_v[:, 3, j, :]), ALU.mult, [SIq, TAB], [RB[1]], eng='pool')
                                k.tt(v4(RB[2]), v4(SIq), b4(tab_v[:, 2, j, :]), ALU.mult, [SIq, TAB], [RB[2]], eng='pool')
                                k.tt(v4(RB[3]), v4(SRq), b4(tab_v[:, 3, j, :]), ALU.mult, [SRq, TAB], [RB[3]], eng='pool')
                                for r, wsel in enumerate((0, 1, 2, 2)):
                                    k.mm(psY, psY[:], cw_t[:, wsel, j, :], RB[r][:], [CW, RB[r]],
                                         start=(jj == 0 and r == 0), stop=(jj == 3 and r == 3))
                        k.stt(YF[:], ust_t[:, ct, ns], sm_t[:, 24 + ct:25 + ct], psY[:], ALU.mult, ALU.add,
                              [UST[n], SM, psY], [YF])
                        k.tt(TQ[:], YF[:], YF[:], ALU.mult, [YF], [TQ], eng='pool')
                        k.ts(TQ[:], TQ[:], 0.044715, 1.0, ALU.mult, ALU.add, [TQ], [TQ], eng='pool')
                        k.tt(TQ[:], TQ[:], YF[:], ALU.mult, [TQ, YF], [TQ], eng='pool')
                        k.act(TQ[:], TQ[:], AF.Sigmoid, [TQ], [TQ], scale=2.0 * math.sqrt(2.0 / PI))
                        k.tt(GYF[ct][:], YF[:], TQ[:], ALU.mult, [YF, TQ], [GYF[ct]], eng='pool')
                        k.copy('act', gyb_v[:, ct, :], GYF[ct][:], [GYF[ct]], [GYB])
                    for cto in range(4):
                        ps = PS[6 + cto % 2]
                        for ci in range(4):
                            k.mm(ps, ps[:], WGLU[:, ci, cto * 128:(cto + 1) * 128], gyb_v[:, ci, :], [EW[0], GYB],
                                 start=(ci == 0), stop=(ci == 3))
                        k.act(TQ[:], ps[:], AF.Sigmoid, [ps, SM], [TQ], bias=sm_t[:, 28 + cto:29 + cto])
                        k.tt(GYF[cto][:], GYF[cto][:], TQ[:], ALU.mult, [GYF[cto], TQ], [GYF[cto]])
                        k.act(TP[cto][:], GYF[cto][:], AF.Square, [GYF[cto]], [TP[cto]])
                    ps = PS[6]
                    for ct in range(4):
                        k.mm(ps, ps[:], onesk_t[:], TP[ct][:], [ONESK, TP[ct]], start=(ct == 0), stop=(ct == 3))
                    k.rsqrt_eps(YF[:], ps[:], [ps], [YF])
                    for ct in range(4):
                        k.stt(xt_t[:, 4 + ct, ns], GYF[ct][:], sm_t[:, 32 + ct:33 + ct], YF[:], ALU.mult, ALU.mult,
                              [GYF[ct], SM, YF], [XT[n]])
                k.barrier()

                load_row(ROW[0], ln1_g[li]); load_row(ROW[1], ln1_b[li])
                for hf in range(2):
                    k.dma('pool', ew_t[hf][:, 0:4096].rearrange("p (k c) -> p k c", k=8),
                          w_out[li][:, hf * 512:(hf + 1) * 512].rearrange("(k p) c -> p k c", p=128), DI, EW[hf])
                for i in range(NT):
                    xlt = XLB[1]
                    xsv = xs_d.rearrange("(n p) d -> p n d", p=128)
                    k.dma(sync, xlt[:], xsv[:, i, :], XS, xlt)
                    for hf in range(2):
                        ps = PS[(2 * i + hf) % 4]
                        W = ew_t[hf][:, 0:4096].rearrange("p (k c) -> p k c", k=8)
                        for kk in range(8):
                            k.mm(ps, ps[:], xt_t[:, kk, i * 128:(i + 1) * 128], W[:, kk, :], [XT[i // 4], EW[hf]],
                                 start=(kk == 0), stop=(kk == 7))
                        k.stt(X[i][:, hf * 512:(hf + 1) * 512], xlt[:, hf * 512:(hf + 1) * 512], ALPHA, ps[:], ALU.mult, ALU.add, [xlt, ps], [X[i]])
                    layer_norm_tile(i, ROW[0], ROW[1])
                k.barrier()

            if 'B' in phases:
                k.dma(sync, wr_t[:], w_r[li].rearrange("(k p) c -> p k c", p=128), DI, WR)
                k.dma(sync, sm2_t[:, 0:36], b_r[li].partition_broadcast(128), DI, SM)
                LG = sm2_t[:, 64:100]

                _dbg = _os.environ.get('KDEBUG', '')
                _cut = int(_dbg[1:]) if _dbg.startswith('r') else 99

                def routing(i):
                    if _cut < 1: return
                    ps = PS[4 + i % 2]
                    for kk in range(8):
                        k.mm(ps, ps[:, 0:36], xr_t[:, kk, :], wr_t[:, kk, :], [XR, WR], start=(kk == 0), stop=(kk == 7))
                    q = lambda a, n=1: sm2_t[:, a:a + n]
                    if _cut < 2: return
                    k.tt(LG, ps[:, 0:36], sm2_t[:, 0:36], ALU.add, [ps, SM], [SM])
                    GMX = q(100); NGM = q(101); SE = q(102); OH = q(104, 4); MK = q(108, 4); EM = q(112, 32)
                    M8 = q(144, 8); NV2 = q(152); W1 = q(153); W2 = q(154); C1 = q(155); C2 = q(156); EJ = q(160, 4)
                    M1 = q(164, 32); M2 = q(196, 32)
                    sS = lambda fn: k.op('dve', fn, reads=[SM], writes=[SM])
                    if _cut < 3: return
                    sS(lambda e: e.tensor_reduce(out=GMX, in_=sm2_t[:, 64:68], axis=mybir.AxisListType.X, op=ALU.max))
                    k.ts(NGM, GMX, -1.0, None, ALU.mult, None, [SM], [SM])
                    k.act(EJ, sm2_t[:, 64:68], AF.Exp, [SM], [SM], bias=NGM, accum_out=SE)
                    sS(lambda e: e.reciprocal(out=SE, in_=SE))
                    k.ts(OH, sm2_t[:, 64:68], GMX, None, ALU.is_equal, None, [SM], [SM])
                    k.ts(MK, OH, -1.0, 1e30, ALU.add, ALU.mult, [SM], [SM])
                    k.tt(EM.rearrange("p (g e) -> p g e", g=4), sm2_t[:, 68:100].rearrange("p (g e) -> p g e", g=4),
                         MK.unsqueeze(2).broadcast_to([128, 4, 8]), ALU.add, [SM], [SM])
                    if _cut < 4: return
                    sS(lambda e: e.max(out=M8, in_=EM))
                    if _cut < 5: return
                    k.ts(NV2, sm2_t[:, 145:146], -1.0, None, ALU.mult, None, [SM], [SM])
                    k.act(W1, sm2_t[:, 144:145], AF.Sigmoid, [SM], [SM], bias=NV2)
                    k.ts(W2, W1, -1.0, 1.0, ALU.mult, ALU.add, [SM], [SM])
                    k.tt(C1, W1, SE, ALU.mult, [SM], [SM]); k.tt(C2, W2, SE, ALU.mult, [SM], [SM])
                    k.ts(M1, EM, sm2_t[:, 144:145], C1, ALU.is_equal, ALU.mult, [SM], [SM])
                    k.ts(M2, EM, sm2_t[:, 145:146], C2, ALU.is_equal, ALU.mult, [SM], [SM])
                    k.tt(M1, M1, M2, ALU.add, [SM], [SM])
                    k.copy('dve', meta_v[:, i, :, 1], M1, [SM], [META])
                    k.ts(meta_v[:, i, :, 0], EM, sm2_t[:, 145:146], None, ALU.is_equal, None, [SM], [META])
                    k.ts(mall_v[:, i, :], M1, 0.0, None, ALU.is_gt, None, [SM], [MALL])

                k.op('pool', lambda e: e.memset(out_v[0], 0.0), writes=[OUTB[0]])
                ydv = yd_d.rearrange("(n p) d -> p n d", p=128)
                for r in range(32):
                    k.dma(sync, ydv[:, r, :], out_v[0], OUTB[0], YD, parts=True)
                k._wait('pool', (YD.dsem, YD.dval, None, 'd_yd'))
                for i in range(NT):
                    for half in range(2):
                        ps = PS[(2 * i + half) % 4]
                        for q in range(4):
                            kk = half * 4 + q
                            k.tr(ps, ps[:, q * 128:(q + 1) * 128], X[i][:, kk * 128:(kk + 1) * 128], IDENT, [X[i]])
                        k.copy('dve', xr_t[:, half * 4:half * 4 + 4, :], ps[:].rearrange("p (q t) -> p q t", q=4), [ps], [XR])
                    k.copy('act', xtm_v[:, i, :], X[i][:], [X[i]], [XTM[i]])
                    routing(i)
                POSv = gw_t
                POS = k.tile(gw_t[:], 'POS')
                for i in range(NT):
                    ps = PS[4 + i % 2]
                    k.mm(ps, ps[:, 0:32], stri_t[:], mall_v[:, i, :], [STRI, MALL], start=True, stop=(i == 0))
                    for j in range(i):
                        k.mm(ps, ps[:, 0:32], onesb_t[:], mall_v[:, j, :], [ONESB, MALL], start=False, stop=(j == i - 1))
                    k.stt(gw_t[:, i, :], ps[:, 0:32], 1.0, mall_v[:, i, :], ALU.add, ALU.mult, [ps, MALL], [POS])
                k.ts(gw_t[:].rearrange("p n e -> p (n e)"), gw_t[:].rearrange("p n e -> p (n e)"), -1.0, None, ALU.add, None,
                     [POS], [POS])

                k.op('pool', lambda e: e.memset(out_v[0], 0.0), writes=[OUTB[0]])
                ydv = yd_d.rearrange("(n p) d -> p n d", p=128)
                for r in range(32):
                    k.dma(sync, ydv[:, r, :], out_v[0], OUTB[0], YD, parts=True)
                k._wait('pool', (YD.dsem, YD.dval, None, 'd_yd'))
                for i in range(NT):
                    for half in range(2):
                        ps = PS[(2 * i + half) % 4]
                        for q in range(4):
                            kk = half * 4 + q
                            k.tr(ps, ps[:, q * 128:(q + 1) * 128], X[i][:, kk * 128:(kk + 1) * 128], IDENT, [X[i]])
                        k.copy('dve', xr_t[:, half * 4:half * 4 + 4, :], ps[:].rearrange("p (q t) -> p q t", q=4), [ps], [XR])
                    k.copy('act', xtm_v[:, i, :], X[i][:], [X[i]], [XTM[i]])
                    routing(i)
                POSv = gw_t
                POS = k.tile(gw_t[:], 'POS')
                for i in range(NT):
                    ps = PS[4 + i % 2]
                    k.mm(ps, ps[:, 0:32], stri_t[:], mall_v[:, i, :], [STRI, MALL], start=True, stop=(i == 0))
                    for j in range(i):
                        k.mm(ps, ps[:, 0:32], onesb_t[:], mall_v[:, j, :], [ONESB, MALL], start=False, stop=(j == i - 1))
                    k.stt(gw_t[:, i, :], ps[:, 0:32], 1.0, mall_v[:, i, :], ALU.add, ALU.mult, [ps, MALL], [POS])
                k.ts(gw_t[:].rearrange("p n e -> p (n e)"), gw_t[:].rearrange("p n e -> p (n e)"), -1.0, None, ALU.add, None,
                     [POS], [POS])

                def load_expert(e, buf):
                    k.dma('pool', ew_t[buf][:, 0:4096].rearrange("p (k c) -> p k c", k=8),
                          w_eg[li, e].rearrange("(k p) c -> p k c", p=128), DI, EW[buf])
                    k.dma('pool', ew_t[buf][:, 4096:8192].rearrange("p (k c) -> p k c", k=8),
                          w_eu[li, e].rearrange("(k p) c -> p k c", p=128), DI, EW[buf], parts=True)
                    k.dma('pool', ew_t[buf][:, 8192:12288].rearrange("p (k c) -> p k c", k=4),
                          w_ed[li, e].rearrange("(k p) c -> p k c", p=128), DI, EW[buf], parts=True)

                if n_experts > 0:
                    load_expert(0, 0)
                for e_ in range(n_experts):
                    buf = e_ % 2
                    if e_ + 1 < n_experts:
                        load_expert(e_ + 1, 1 - buf)
                    WG = ew_t[buf][:, 0:4096].rearrange("p (k c) -> p k c", k=8)
                    WU = ew_t[buf][:, 4096:8192].rearrange("p (k c) -> p k c", k=8)
                    WD = ew_t[buf][:, 8192:12288].rearrange("p (k c) -> p k c", k=4)
                    for i in range(NT):
                        selt = ROW[i // 8]
                        k.ts(sel_v[i // 8][:, i % 8, :], iota_t[:], gw_t[:, i, e_:e_ + 1], None, ALU.is_equal, None,
                             [IOTA, POS], [selt], eng=('dve' if (i % 2 == 0 or _os.environ.get('KSEL', 'dve') == 'dve') else 'pool'))
                    xg = XG[buf]; xgv = xg_v[buf]
                    for kf in range(8):
                        ps = PS[kf % 4]
                        for i in range(NT):
                            k.mm(ps, ps[:, 0:256], xtm_v[:, i, kf * 128:(kf + 1) * 128], sel_v[i // 8][:, i % 8, :],
                                 [XTM[i], ROW[i // 8]], start=(i == 0), stop=(i == NT - 1))
                        evac(xgv[:, kf, :], ps[:, 0:256], [ps], [xg])
                    psM = PS[4]
                    for sh in range(2):
                        for i in range(NT):
                            k.mm(psM, psM[:, sh * 8:sh * 8 + 3], sel_v[i // 8][:, i % 8, sh * 128:(sh + 1) * 128],
                                 cmeta_t[:, i, :], [ROW[i // 8], CMETA], start=(i == 0), stop=(i == NT - 1))
                        for i in range(NT):
                            k.mm(psM, psM[:, sh * 8 + 3:sh * 8 + 5], sel_v[i // 8][:, i % 8, sh * 128:(sh + 1) * 128],
                                 meta_v[:, i, e_, :], [ROW[i // 8], META], start=(i == 0), stop=(i == NT - 1))
                    sl = SLOT[buf]; slv = slot_t[:, buf]
                    k.copy('dve', slv.rearrange("p a c -> p (a c)"), psM[:, 0:16], [psM], [sl])
                    k.stt(slv[:, :, 5], slv[:, :, 0], 128.0, slv[:, :, 1], ALU.mult, ALU.add, [sl], [sl])
                    k.stt(slv[:, :, 5], slv[:, :, 3], 2048.0, slv[:, :, 5], ALU.mult, ALU.add, [sl], [sl])
                    k.ts(slv[:, :, 6], slv[:, :, 2], -1.0e6, 1.0e6, ALU.mult, ALU.add, [sl], [sl])
                    for sh in range(2):
                        k.tt(dest_tt[buf][sh][:, :], slv[:, sh, 5:6], slv[:, sh, 6:7], ALU.add, [sl], [DEST[buf][sh]])
                    hte = HTE[buf]; htev = hte_v[buf]
                    for m in range(4):
                        psg = PS[5 + (m % 2) * 2 - (m % 2)]; psu = PS[6 + (m % 2)]
                        psg = PS[4 + (m % 2) * 2 + 1] if False else PS[5] if m % 2 == 0 else PS[7]
                        psu = PS[6] if m % 2 == 0 else PS[4]
                        for kk in range(8):
                            k.mm(psg, psg[:, 0:256], WG[:, kk, m * 128:(m + 1) * 128], xgv[:, kk, :], [EW[buf], xg],
                                 start=(kk == 0), stop=(kk == 7))
                        for kk in range(8):
                            k.mm(psu, psu[:, 256:512], WU[:, kk, m * 128:(m + 1) * 128], xgv[:, kk, :], [EW[buf], xg],
                                 start=(kk == 0), stop=(kk == 7))
                        sg = SG[m % 2]
                        k.act(sg[:], psg[:, 0:256], AF.Silu, [psg], [sg])
                        k.tt(htev[:, m, :], sg[:], psu[:, 256:512], ALU.mult, [sg, psu], [hte])
                    for sh in range(2):
                        ob = OUTB[sh]
                        for hf in range(2):
                            ps = PS[(sh * 2 + hf) % 4]
                            for m in range(4):
                                k.mm(ps, ps[:], htev[:, m, sh * 128:(sh + 1) * 128], WD[:, m, hf * 512:(hf + 1) * 512],
                                     [hte, EW[buf]], start=(m == 0), stop=(m == 3))
                            k.act(out_v[sh][:, hf * 512:(hf + 1) * 512], ps[:], AF.Copy, [ps, sl], [ob], scale=slv[:, sh, 4:5])
                        k.dma_scatter(yd_d[:, :], dest_tt[buf][sh][:, :], out_v[sh], ob, DEST[buf][sh], YD, 4095)
                if _os.environ.get('KDEBUG', '') == 'dbgslot':
                    k.barrier()
                    k.copy('dve', X[0][:, 0:16], slot_t[:, 1].rearrange("p a c -> p (a c)"), [SLOT[1]], [X[0]])
                    k.copy('dve', X[0][:, 16:17], dest_tt[1][0][:, :], [DEST[1][0]], [X[0]])
                    k.copy('dve', X[0][:, 17:18], dest_tt[1][1][:, :], [DEST[1][1]], [X[0]])
                    k.copy('dve', X[0][:, 32:64], gw_t[:, 0, :], [POS], [X[0]])
                    k.copy('dve', X[0][:, 64:96], mall_v[:, 0, :], [MALL], [X[0]])
                    k.copy('dve', X[0][:, 96:128], meta_v[:, 0, :, 1], [META], [X[0]])
                    k.copy('dve', X[0][:, 128:160], gw_t[:, 15, :], [POS], [X[0]])
                for i in range(NT if _os.environ.get('KDEBUG', '') != 'dbgslot' else 0):
                    for kq in range(2):
                        ob = OUTB[kq]
                        k.dma(sync, out_v[kq], ydv[:, kq * 16 + i, :], YD, ob)
                        if kq == 0:
                            k.stt(X[i][:], X[i][:], ALPHA, out_v[kq], ALU.mult, ALU.add, [X[i], ob], [X[i]])
                        else:
                            k.tt(X[i][:], X[i][:], out_v[kq], ALU.add, [X[i], ob], [X[i]], eng='pool')
                k.barrier()

            if 'C' in phases:
                load_row(ROW[0], ln2_g[li]); load_row(ROW[1], ln2_b[li])
                for i in range(NT):
                    layer_norm_tile(i, ROW[0], ROW[1])
                build_xT()
                load_row(ROW[0], b_pg[li]); load_row(ROW[1], ple_g[li])
                for hf in range(2):
                    k.dma('pool', ew_t[hf][:, 0:4096].rearrange("p (k c) -> p k c", k=8),
                          w_pg[li][:, hf * 512:(hf + 1) * 512].rearrange("(k p) c -> p k c", p=128), DI, EW[hf])
                k.dma('pool', ew_t[0][:, 4096:6144].rearrange("p (k c) -> p k c", k=2),
                      w_pp[li].rearrange("(k p) c -> p k c", p=128), DI, EW[0], parts=True)
                WPP = ew_t[0][:, 4096:6144].rearrange("p (k c) -> p k c", k=2)
                PTt = [UST[n] for n in range(4)]
                for n in range(4):
                    k.dma('pool', ust_t[:, 0:2, n * 512:(n + 1) * 512],
                          pT_in[li][:, n * 512:(n + 1) * 512].rearrange("(k p) t -> p k t", p=128), DI, UST[n])
                for i in range(NT):
                    sl = slice(i * 128, (i + 1) * 128)
                    ms = MS[i % 2]
                    psG = [PS[0 + (i % 2) * 4], PS[1 + (i % 2) * 4]]
                    psP = [PS[2 + (i % 2) * 4], PS[3 + (i % 2) * 4]]
                    for hf in range(2):
                        W = ew_t[hf][:, 0:4096].rearrange("p (k c) -> p k c", k=8)
                        for kk in range(8):
                            k.mm(psG[hf], psG[hf][:], xt_t[:, kk, sl], W[:, kk, :], [XT[i // 4], EW[hf]],
                                 start=(kk == 0), stop=(kk == 7))
                        for k2 in range(2):
                            k.mm(psP[hf], psP[hf][:], ust_t[:, k2, sl], WPP[:, k2, hf * 512:(hf + 1) * 512],
                                 [UST[i // 4], EW[0]], start=(k2 == 0), stop=(k2 == 1))
                    tq = CTMP[(i % 2) * 3]; tg = CTMP[(i % 2) * 3 + 1]; tp_ = CTMP[(i % 2) * 3 + 2]
                    for hf in range(2):
                        k.act(tq[:], psP[hf][:], AF.Square, [psP[hf]], [tq, ms], accum_out=ms[:, 16 + hf:17 + hf])
                    k.tt(ms[:, 18:19], ms[:, 16:17], ms[:, 17:18], ALU.add, [ms], [ms])
                    k.rsqrt_eps(ms[:, 19:20], ms[:, 18:19], [ms], [ms], pre_scale=1.0 / 1024.0)
                    for hf in range(2):
                        hs = slice(hf * 512, (hf + 1) * 512)
                        k.tt(tg[:], psG[hf][:], row_t[0][:, hs], ALU.add, [psG[hf], ROW[0]], [tg])
                        k.act(tg[:], tg[:], AF.Sigmoid, [tg], [tg])
                        k.stt(tp_[:], psP[hf][:], ms[:, 19:20], row_t[1][:, hs], ALU.mult, ALU.mult, [psP[hf], ms, ROW[1]], [tp_])
                        k.tt(tg[:], tg[:], tp_[:], ALU.mult, [tg, tp_], [tg], eng='pool')
                        k.tt(X[i][:, hs], X[i][:, hs], tg[:], ALU.add, [X[i], tg], [X[i]], eng='pool')
                k.barrier()

        for _pi in range(int(_os.environ.get('KPAD', '0'))):
            k.op('dve', lambda e: e.memset(sm2_t[:, 300:301], 0.0), writes=[])
        yv = y_out.rearrange("(n p) d -> p n d", p=128)
        for i in range(NT):
            k.dma(sync, yv[:, i, :], X[i][:], X[i], YO, parts=True)
        if not k.dry:
            nc.sync.wait_ge(YO.dsem, YO.dval)
    nc._declared_inputs = declared
    return nc, k.waited


def prep_shared(inp):
    f = lambda a: np.ascontiguousarray(np.asarray(a, dtype=np.float32))
    L = DEPTH
    sh = {}
    for n in ['w_in', 'conv_w', 'conv_b', 'w_q', 'w_k', 'mh_g', 'w_glu', 'b_glu', 's5_g', 'w_out', 'ln1_g', 'ln1_b',
              'w_eg', 'w_eu', 'w_ed', 'ln2_g', 'ln2_b', 'w_pg', 'b_pg', 'w_pp', 'ple_g']:
        sh[n] = f(inp[n])
    sh['bg'] = f(np.concatenate([np.asarray(inp['b_i']), np.asarray(inp['b_f'])], axis=1))
    sh['lam_re'] = f(np.asarray(inp['lam_re']).reshape(L, 2048))
    sh['lam_im'] = f(np.asarray(inp['lam_im']).reshape(L, 2048))
    sh['logdt_x'] = f(np.repeat(np.asarray(inp['log_dt'])[:, :, None], 64, axis=2).reshape(L, 2048))
    sh['d_skip'] = f(np.asarray(inp['d_skip']).reshape(L, 512))
    b_re = np.asarray(inp['b_re']); b_im = np.asarray(inp['b_im'])
    c_re = np.asarray(inp['c_re']); c_im = np.asarray(inp['c_im'])
    brp = np.zeros((L, 128, 32, 64), np.float32); bip = np.zeros((L, 128, 32, 64), np.float32)
    crp = np.zeros((L, 2, 64, 16, 128), np.float32); cip = np.zeros((L, 2, 64, 16, 128), np.float32)
    for g in range(32):
        r0 = (g % 8) * 16
        brp[:, r0:r0 + 16, g, :] = np.transpose(b_re[:, g], (0, 2, 1))
        bip[:, r0:r0 + 16, g, :] = np.transpose(b_im[:, g], (0, 2, 1))
        j, gi = g // 2, g % 2
        crp[:, gi, :, j, r0:r0 + 16] = np.transpose(c_re[:, g], (0, 2, 1))
        cip[:, gi, :, j, r0:r0 + 16] = np.transpose(c_im[:, g], (0, 2, 1))
    sh['brp'] = brp.reshape(L, 128, 2048); sh['bip'] = bip.reshape(L, 128, 2048)
    sh['crp'] = crp.reshape(L, 128, 16, 128); sh['cip'] = cip.reshape(L, 128, 16, 128)
    sh['w_r'] = f(np.concatenate([np.asarray(inp['w_grp']), np.asarray(inp['w_rt'])], axis=2))
    sh['b_r'] = f(np.concatenate([np.asarray(inp['b_grp']), np.asarray(inp['b_rt'])], axis=1))
    sh['ident'] = np.eye(128, dtype=np.float32)
    sh['tri'] = np.triu(np.ones((128, 128), np.float32))
    sh['tau'] = np.ascontiguousarray(np.broadcast_to(np.arange(128, dtype=np.float32), (128, 128)))
    sh['iota256'] = np.ascontiguousarray(np.broadcast_to(np.arange(256, dtype=np.float32), (128, 256)))
    sh['stri'] = np.triu(np.ones((128, 128), np.float32), k=1)
    cm = np.zeros((128, 16, 3), np.float32)
    cm[:, :, 0] = np.arange(16, dtype=np.float32)[None, :]
    cm[:, :, 1] = np.arange(128, dtype=np.float32)[:, None]
    cm[:, :, 2] = 1.0
    sh['cmeta'] = cm
    return sh


_NC_CACHE = {}
PER_LAYER = ['w_in', 'conv_w', 'conv_b', 'w_q', 'w_k', 'bg', 'mh_g', 'lam_re', 'lam_im', 'logdt_x', 'brp', 'bip', 'crp',
             'cip', 'd_skip', 'w_glu', 'b_glu', 's5_g', 'w_out', 'ln1_g', 'ln1_b', 'w_r', 'b_r', 'w_eg', 'w_eu', 'w_ed',
             'ln2_g', 'ln2_b', 'w_pg', 'b_pg', 'w_pp', 'ple_g']


def _launch(nc, sh, xs, pTs, li):
    names = set(nc._declared_inputs)
    base = {}
    for kname in names:
        if kname in ('x', 'pT'):
            continue
        a = sh[kname]
        base[kname] = np.ascontiguousarray(a[li:li + 1]) if kname in PER_LAYER else a
    in_maps = []
    for b in range(8):
        m = dict(base)
        m['x'] = xs[b]
        m['pT'] = pTs[b][li:li + 1]
        in_maps.append(m)
    res = run_bass_kernel_spmd(nc, in_maps, core_ids=list(range(8)))
    return [np.ascontiguousarray(res.results[b]['y']) for b in range(8)]


def kernel(**inputs):
    sh = prep_shared(inputs)
    x = np.asarray(inputs['x'], dtype=np.float32)
    p = np.asarray(inputs['p'], dtype=np.float32)
    if 'full' not in _NC_CACHE:
        _NC_CACHE['full'] = build_program()
    nc = _NC_CACHE['full']
    names = set(nc._declared_inputs)
    base = {kname: sh[kname] for kname in names if kname not in ('x', 'pT')}
    in_maps = []
    for b in range(8):
        m = dict(base)
        m['x'] = np.ascontiguousarray(x[b])
        m['pT'] = np.ascontiguousarray(np.transpose(p[:, b], (0, 2, 1)))
        in_maps.append(m)
    res = run_bass_kernel_spmd(nc, in_maps, core_ids=list(range(8)))
    return np.stack([res.results[b]['y'] for b in range(8)], axis=0).astype(np.float32)
```

```python
import math
import bisect
import os as _os
import numpy as np
import concourse.bass as bass
import concourse.mybir as mybir
from concourse.bass_utils import run_bass_kernel_spmd
from contextlib import ExitStack

F32 = mybir.dt.float32
BF16 = mybir.dt.bfloat16
ALU = mybir.AluOpType
AF = mybir.ActivationFunctionType

D = 1024; S = 2048; NT = 16; DEPTH = 4
D_IN = 2056
ALPHA = (2 * DEPTH) ** 0.25
EPS = 1e-5
PI = math.pi


class T:
    def __init__(s, ap, name):
        s.ap = ap; s.name = name; s.w = []; s.r = []; s.dsem = None; s.dval = 0

    def __getitem__(s, idx):
        return s.ap[idx]


class K:
    def __init__(s, nc, es, needed=None):
        s.nc = nc; s.es = es
        s.dry = needed is None
        s.needed = needed or {}
        s.needed_set = {e: set(v) for e, v in s.needed.items()}
        s.waited = {}
        s.E = {'pe': nc.tensor, 'dve': nc.vector, 'act': nc.scalar, 'pool': nc.gpsimd, 'sp': nc.sync}
        s.sem = {e: es.enter_context(nc.semaphore('sem_' + e)) for e in s.E}
        s.cnt = {e: 0 for e in s.E}
        s.known = {e: {} for e in s.E}
        s.dma_tiles = []
        s.nt = 0

    def tile(s, ap, name=None):
        s.nt += 1
        return T(ap, name or ('t%d' % s.nt))

    def sb(s, name, shape, dt):
        return s.es.enter_context(s.nc.sbuf_tensor(name, shape, dt))

    def _wait(s, eng, ev):
        sem, val, deng, key = ev
        if s.known[eng].get(key, 0) >= val:
            return
        s.known[eng][key] = val
        if deng is not None:
            s.waited.setdefault(deng, set()).add(val)
            if not s.dry:
                rank = bisect.bisect_right(s.needed[deng], val)
                s.E[eng].wait_ge(sem, rank)
        elif not s.dry:
            s.E[eng].wait_ge(sem, val)

    def _deps(s, eng, reads, writes, skipkey=None):
        for t in reads:
            for ev in t.w:
                if ev[2] == eng and eng == 'pe':
                    continue
                s._wait(eng, ev)
        for t in writes:
            for ev in t.w + t.r:
                if ev[2] == eng:
                    continue
                if skipkey is not None and ev[3] == skipkey:
                    continue
                s._wait(eng, ev)

    def op(s, eng, fn, reads=(), writes=()):
        s._deps(eng, reads, writes)
        s.cnt[eng] += 1
        if not s.dry:
            ins = fn(s.E[eng])
            if s.cnt[eng] in s.needed_set.get(eng, ()):
                ins.then_inc(s.sem[eng], 1)
        ev = (s.sem[eng], s.cnt[eng], eng, eng)
        for t in writes:
            t.w = [ev]; t.r = []
        for t in reads:
            if t in writes:
                continue
            t.r = [e for e in t.r if e[3] != eng] + [ev]

    def dma(s, q, out_ap, in_ap, src, dst, parts=False, **kw):
        key = 'd_' + dst.name
        s._deps(q, [src], [dst], skipkey=key if parts else None)
        if dst.dsem is None:
            dst.dsem = True if s.dry else s.es.enter_context(s.nc.semaphore(key))
            s.dma_tiles.append(dst)
        dst.dval += 16
        if not s.dry:
            ins = s.E[q].dma_start(out=out_ap, in_=in_ap, **kw)
            ins.then_inc(dst.dsem, 16)
        ev = (dst.dsem, dst.dval, None, key)
        dst.w = [ev]; dst.r = []
        src.r = [e for e in src.r if e[3] != key] + [ev]

    def dma_scatter(s, out_ap, idx_ap, in_ap, src, idxt, dst, bound):
        key = 'd_' + dst.name
        s._deps('pool', [src, idxt], [dst], skipkey=key)
        if dst.dsem is None:
            dst.dsem = True if s.dry else s.es.enter_context(s.nc.semaphore(key))
            s.dma_tiles.append(dst)
        dst.dval += 16
        ev = (dst.dsem, dst.dval, None, key)
        if not s.dry:
            ins = s.nc.gpsimd.indirect_dma_start(out=out_ap, out_offset=bass.IndirectOffsetOnAxis(ap=idx_ap, axis=0),
                                                 in_=in_ap, in_offset=None, bounds_check=s.bnd_reg, oob_is_err=False)
            ins.then_inc(dst.dsem, 16)
        dst.w = [ev]; dst.r = []
        for t in (src, idxt):
            t.r = [e for e in t.r if e[3] != key] + [ev]

    def barrier(s):
        for e in s.E:
            for e2 in s.E:
                if e2 != e and s.cnt[e2] > 0:
                    s._wait(e, (s.sem[e2], s.cnt[e2], e2, e2))
            for t in s.dma_tiles:
                if t.dval > 0:
                    s._wait(e, (t.dsem, t.dval, None, 'd_' + t.name))

    def mm(s, ps, out_ap, lhsT_ap, rhs_ap, reads, start=True, stop=True):
        s.op('pe', lambda e: e.matmul(out_ap, lhsT=lhsT_ap, rhs=rhs_ap, start=start, stop=stop),
             reads=reads, writes=[ps])

    def tr(s, ps, out_ap, in_ap, ident, reads):
        s.op('pe', lambda e: e.transpose(out_ap, in_ap, ident[:]), reads=list(reads) + [ident], writes=[ps])

    def act(s, out_ap, in_ap, func, reads, writes, bias=None, scale=None, accum_out=None, eng='act'):
        kw = {}
        if bias is not None: kw['bias'] = bias
        if scale is not None: kw['scale'] = scale
        if accum_out is not None: kw['accum_out'] = accum_out
        s.op('act', lambda e: e.activation(out=out_ap, in_=in_ap, func=func, **kw), reads=reads, writes=writes)

    def tt(s, out_ap, in0, in1, op, reads, writes, eng='dve'):
        if eng == 'pool' and _os.environ.get('KPOOL', '0') != '1':
            eng = 'dve'
        if eng == 'POOL':
            eng = 'pool'
        s.op(eng, lambda e: e.tensor_tensor(out=out_ap, in0=in0, in1=in1, op=op), reads=reads, writes=writes)

    def ts(s, out_ap, in0, s1, s2, op0, op1, reads, writes, eng='dve'):
        if eng == 'pool' and _os.environ.get('KPOOL', '0') != '1':
            eng = 'dve'
        if op1 is None:
            s.op(eng, lambda e: e.tensor_scalar(out=out_ap, in0=in0, scalar1=s1, scalar2=None, op0=op0),
                 reads=reads, writes=writes)
        else:
            s.op(eng, lambda e: e.tensor_scalar(out=out_ap, in0=in0, scalar1=s1, scalar2=s2, op0=op0, op1=op1),
                 reads=reads, writes=writes)

    def stt(s, out_ap, in0, scalar, in1, op0, op1, reads, writes):
        s.op('dve', lambda e: e.scalar_tensor_tensor(out=out_ap, in0=in0, scalar=scalar, in1=in1, op0=op0, op1=op1),
             reads=reads, writes=writes)

    def rsqrt_eps(s, out_ap, in_ap, reads, writes, pre_scale=1.0):
        s.act(out_ap, in_ap, AF.Ln, reads, writes, bias=s.eps_ap, scale=pre_scale)
        s.act(out_ap, out_ap, AF.Exp, writes, writes, scale=-0.5)

    def copy(s, eng, out_ap, in_ap, reads, writes):
        if eng == 'act':
            s.op('act', lambda e: e.copy(out=out_ap, in_=in_ap), reads=reads, writes=writes)
        else:
            s.op(eng, lambda e: e.tensor_copy(out=out_ap, in_=in_ap), reads=reads, writes=writes)


PARAM_NAMES = ['w_in', 'conv_w', 'conv_b', 'w_q', 'w_k', 'bg', 'mh_g', 'lam_re', 'lam_im', 'logdt_x',
               'brp', 'bip', 'crp', 'cip', 'd_skip', 'w_glu', 'b_glu', 's5_g', 'w_out', 'ln1_g', 'ln1_b',
               'w_r', 'b_r', 'w_eg', 'w_eu', 'w_ed', 'ln2_g', 'ln2_b', 'w_pg', 'b_pg', 'w_pp', 'ple_g']


def build_program(layers=tuple(range(DEPTH)), phases=('A', 'B', 'C'), n_experts=32, dbg=None, L=DEPTH):
    _, waited = _build(layers, phases, n_experts, L, None)
    needed = {e: sorted(v) for e, v in waited.items()}
    nc, _ = _build(layers, phases, n_experts, L, needed)
    return nc


def _build(layers, phases, n_experts, L, needed):
    nc = bass.Bass("TRN2", target_bir_lowering=False)

    declared = []

    def din(name, shape, dt=F32):
        declared.append(name)
        return nc.dram_tensor(name, shape, dt, kind="ExternalInput").ap()

    x_in = din('x', [S, D])
    pT_in = din('pT', [L, 256, S])
    ident_in = din('ident', [128, 128]); tri_in = din('tri', [128, 128]); tau_in = din('tau', [128, 128])
    iota_in = din('iota256', [128, 256]); stri_in = din('stri', [128, 128]); cmeta_in = din('cmeta', [128, 16, 3])
    w_in = din('w_in', [L, D, D_IN]); conv_w = din('conv_w', [L, 4, 512]); conv_b = din('conv_b', [L, 512])
    w_q = din('w_q', [L, 4, 128, 128]); w_k = din('w_k', [L, 4, 128, 128]); bg_in = din('bg', [L, 8])
    mh_g = din('mh_g', [L, 512])
    lam_re = din('lam_re', [L, 2048]); lam_im = din('lam_im', [L, 2048]); logdt_x = din('logdt_x', [L, 2048])
    brp = din('brp', [L, 128, 2048]); bip = din('bip', [L, 128, 2048])
    crp = din('crp', [L, 128, 16, 128]); cip = din('cip', [L, 128, 16, 128])
    d_skip = din('d_skip', [L, 512]); w_glu = din('w_glu', [L, 512, 512]); b_glu = din('b_glu', [L, 512])
    s5_g = din('s5_g', [L, 512]); w_out = din('w_out', [L, D, D])
    ln1_g = din('ln1_g', [L, D]); ln1_b = din('ln1_b', [L, D])
    w_r = din('w_r', [L, D, 36]); b_r = din('b_r', [L, 36])
    if 'B' in phases:
        w_eg = din('w_eg', [L, 32, D, 512]); w_eu = din('w_eu', [L, 32, D, 512]); w_ed = din('w_ed', [L, 32, 512, D])
    ln2_g = din('ln2_g', [L, D]); ln2_b = din('ln2_b', [L, D])
    w_pg = din('w_pg', [L, D, D]); b_pg = din('b_pg', [L, D]); w_pp = din('w_pp', [L, 256, D]); ple_g = din('ple_g', [L, D])
    y_out = nc.dram_tensor('y', [S, D], F32, kind="ExternalOutput").ap()
    xs_d = nc.dram_tensor('xs_scr', [S, D], F32, kind="Internal").ap()
    cri_d = nc.dram_tensor('cri_scr', [2, 2048], F32, kind="Internal").ap()
    yd_d = nc.dram_tensor('yd_scr', [4096, D], F32, kind="Internal").ap()

    es = ExitStack()
    with es:
        k = K(nc, es, needed)
        k.bnd_reg = None
        if not k.dry:
            k.bnd_reg = nc.gpsimd.alloc_register('bnd')
            nc.gpsimd.reg_mov(k.bnd_reg, 4095)
        DI = k.tile(None, 'dram_in')
        XS = k.tile(xs_d, 'xs'); CRI = k.tile(cri_d, 'cri'); YO = k.tile(y_out, 'yo'); YD = k.tile(yd_d, 'yd')

        arena = k.sb('arena', [128, 32768], BF16)
        Xv = arena[:].bitcast(F32).rearrange("p (n d) -> p n d", n=NT)
        X = [k.tile(Xv[:, i, :], 'X%d' % i) for i in range(NT)]
        xt_t = k.sb('xt', [128, 8, S], BF16)
        XT = [k.tile(xt_t[:, :, n * 512:(n + 1) * 512], 'XT%d' % n) for n in range(4)]
        ust_t = k.sb('ust', [128, 4, S], BF16)
        UST = [k.tile(ust_t[:, :, n * 512:(n + 1) * 512], 'UST%d' % n) for n in range(4)]
        HT = UST
        ew_t = [k.sb('ew%d' % i, [128, 12288], BF16) for i in range(2)]
        EW = [k.tile(ew_t[i][:], 'EW%d' % i) for i in range(2)]
        cw_t = k.sb('cw', [128, 3, 16, 128], BF16); CW = k.tile(cw_t[:], 'CW')
        ktm_t = k.sb('ktm', [128, NT, 128], BF16); KTM = k.tile(ktm_t[:], 'KTM')
        row_t = [k.sb('row%d' % i, [128, D], F32) for i in range(2)]
        ROW = [k.tile(row_t[i][:], 'ROW%d' % i) for i in range(2)]
        xr_t = k.sb('xr', [128, 8, 128], F32); XR = k.tile(xr_t[:], 'XR')
        XLB = [k.tile(xr_t[:].rearrange("p a b -> p (a b)"), 'XLB0'),
               k.tile(ust_t[:, 0, :].bitcast(F32), 'XLB1')]
        ctmp_v = [xr_t[:].rearrange("p a b -> p (a b)"), ust_t[:, 2, :].bitcast(F32), ust_t[:, 3, :].bitcast(F32)]
        CTMP = [k.tile(ctmp_v[a][:, b * 512:(b + 1) * 512], 'CTMP%d' % (a * 2 + b)) for a in range(3) for b in range(2)]
        gw_t = k.sb('gw', [128, NT, 32], F32); GW = [k.tile(gw_t[:, i, :], 'GW%d' % i) for i in range(NT)]
        ident_t = k.sb('ident_s', [128, 128], F32); IDENT = k.tile(ident_t[:], 'IDENT')
        tri_t = k.sb('tri_s', [128, 128], F32); TRI = k.tile(tri_t[:], 'TRI')
        tau_t = k.sb('tau_s', [128, 128], F32); TAU = k.tile(tau_t[:], 'TAU')
        ones_t = k.sb('ones', [128, 128], F32); ONES = k.tile(ones_t[:], 'ONES')
        onesk_t = k.sb('onesk', [128, 128], F32); ONESK = k.tile(onesk_t[:], 'ONESK')
        sm_t = k.sb('small', [128, 512], F32)
        SM = k.tile(sm_t[:], 'SM')
        sm2_t = k.sb('small2', [128, 512], F32)
        smi_t = k.sb('smi', [128, 16], mybir.dt.int32)
        eps_t = k.sb('eps', [128, 1], F32)
        k.op('pool', lambda e: e.memset(eps_t[:], EPS), writes=[])
        k.eps_ap = eps_t[:]
        wr_t = k.sb('wr_s', [128, 8, 36], F32); WR = k.tile(wr_t[:], 'WR')
        wqk_t = k.sb('wqk', [128, 2, 4, 128], BF16); WQK = k.tile(wqk_t[:], 'WQK')
        gat_t = k.sb('gates', [128, NT, 8], F32); GATES = k.tile(gat_t[:], 'GATES')
        gx_t = k.sb('gx', [128, 5, NT, 4], F32); GX = k.tile(gx_t[:], 'GX')
        cst_t = k.sb('cst', [128, 129], F32); CST = k.tile(cst_t[:], 'CST')
        cbf_t = k.sb('cbf', [128, 129], BF16); CBF = k.tile(cbf_t[:], 'CBF')
        stb_t = k.sb('stb', [128, 2, 128], BF16); STB = [k.tile(stb_t[:, i, :], 'STB%d' % i) for i in range(2)]
        wv_t = k.sb('wv', [128, 2, 129], BF16); WV = [k.tile(wv_t[:, i, :], 'WV%d' % i) for i in range(2)]
        hh_t = k.sb('hh', [128, 2, 128], F32); HH = [k.tile(hh_t[:, i, :], 'HH%d' % i) for i in range(2)]
        ms_t = k.sb('ms', [128, 2, 32], F32); MS = [k.tile(ms_t[:, i, :], 'MS%d' % i) for i in range(2)]
        PS = []
        for b in range(8):
            pt = es.enter_context(nc.psum_tensor('ps%d' % b, [128, 512], F32))
            PS.append(k.tile(pt[:], 'PS%d' % b))

        o0 = 0
        vext_v = arena[:, o0:o0 + NT * 4 * 129].rearrange("p (n h c) -> p n h c", n=NT, h=4); o0 += NT * 4 * 129
        sigo_v = arena[:, o0:o0 + NT * 512].rearrange("p (n c) -> p n c", n=NT); o0 += NT * 512
        um_v = arena[:, o0:o0 + 4 * 2051].rearrange("p (h t) -> p h t", h=4); o0 += 4 * 2052
        c_v = arena[:, o0:o0 + S]; o0 += S
        qt_v = arena[:, o0:o0 + S]; o0 += S
        kt_v = arena[:, o0:o0 + S]; o0 += S
        assert o0 <= 32768, o0
        VEXT = [k.tile(vext_v[:, i], 'VEXT%d' % i) for i in range(NT)]
        SIGO = [k.tile(sigo_v[:, i], 'SIGO%d' % i) for i in range(NT)]
        UM = [k.tile(um_v[:, h], 'UM%d' % h) for h in range(4)]
        CC = k.tile(c_v, 'CC'); QT = k.tile(qt_v, 'QT'); KT = k.tile(kt_v, 'KT')
        o1 = 0
        tp_v = arena[:, 0:12 * 1024].bitcast(F32).rearrange("p (n c) -> p n c", n=12); o1 = 12 * 1024
        TP = [k.tile(tp_v[:, i], 'TP%d' % i) for i in range(12)]
        gyb_v = arena[:, o1:o1 + 2048].rearrange("p (n c) -> p n c", n=4); o1 += 2048
        GYB = k.tile(gyb_v, 'GYB')
        rb_v = arena[:, o1:o1 + 2048].rearrange("p (n c) -> p n c", n=4); o1 += 2048
        RB = [k.tile(rb_v[:, i], 'RB%d' % i) for i in range(4)]
        tab_v = arena[:, o1:o1 + 4 * 16 * 128].rearrange("p (a j t) -> p a j t", a=4, j=16); o1 += 4 * 16 * 128
        TAB = k.tile(tab_v, 'TAB')
        bb_v = arena[:, o1:o1 + 2 * 2048].rearrange("p (a c) -> p a c", a=2); o1 += 4096
        BB = k.tile(bb_v, 'BB')
        tp2_v = arena[:, o1:o1 + 4096].bitcast(F32).rearrange("p (n c) -> p n c", n=4); o1 += 4096
        TP2 = [k.tile(tp2_v[:, i], 'TP2_%d' % i) for i in range(4)]
        assert o1 <= 32768, o1

        iota_t = k.sb('iota_s', [128, 256], F32); IOTA = k.tile(iota_t[:], 'IOTA')
        stri_t = k.sb('stri_s', [128, 128], BF16); STRI = k.tile(stri_t[:], 'STRI')
        onesb_t = k.sb('onesb', [128, 128], BF16); ONESB = k.tile(onesb_t[:], 'ONESB')
        cmeta_t = k.sb('cmeta_s', [128, 16, 3], BF16); CMETA = k.tile(cmeta_t[:], 'CMETA')
        slot_t = k.sb('slot', [128, 2, 2, 8], F32); SLOT = [k.tile(slot_t[:, b], 'SLOT%d' % b) for b in range(2)]
        dest_tt = [[k.sb('dest%d%d' % (b, h), [128, 1], mybir.dt.int32) for h in range(2)] for b in range(2)]
        DEST = [[k.tile(dest_tt[b][h][:, :], 'DEST%d%d' % (b, h)) for h in range(2)] for b in range(2)]
        xtm_v = xt_t[:].rearrange("p k t -> p (k t)").rearrange("p (n d) -> p n d", n=NT)
        XTM = [k.tile(xtm_v[:, i, :], 'XTM%d' % i) for i in range(NT)]
        ustf = ust_t[:].rearrange("p k t -> p (k t)")
        xg_v = [ustf[:, b * 2048:(b + 1) * 2048].rearrange("p (k c) -> p k c", k=8) for b in range(2)]
        XG = [k.tile(xg_v[b], 'XG%d' % b) for b in range(2)]
        hte_v = [ustf[:, 4096 + b * 1024:4096 + (b + 1) * 1024].rearrange("p (k c) -> p k c", k=4) for b in range(2)]
        HTE = [k.tile(hte_v[b], 'HTE%d' % b) for b in range(2)]
        sg_v = [ustf[:, 6144 + b * 512:6144 + (b + 1) * 512].bitcast(F32) for b in range(2)]
        SG = [k.tile(sg_v[b], 'SG%d' % b) for b in range(2)]
        cwf = cw_t[:].rearrange("p a j c -> p (a j c)")
        out_v = [cwf[:, b * 2048:(b + 1) * 2048].bitcast(F32) for b in range(2)]
        OUTB = [k.tile(out_v[b], 'OUTB%d' % b) for b in range(2)]
        meta_v = cwf[:, 4096:4096 + 1024].rearrange("p (n e c) -> p n e c", n=NT, e=32)
        META = k.tile(meta_v, 'META')
        mall_v = sm_t[:, 0:256].bitcast(BF16).rearrange("p (n e) -> p n e", n=NT)
        MALL = k.tile(mall_v, 'MALL')
        sel_v = [row_t[b][:].bitcast(BF16).rearrange("p (n c) -> p n c", n=8) for b in range(2)]
        sync = 'sp'
        k.dma(sync, iota_t[:], iota_in, DI, IOTA)
        k.dma('pool', stri_t[:], stri_in, DI, STRI)
        k.dma('pool', cmeta_t[:], cmeta_in, DI, CMETA)
        k.op('pool', lambda e: e.memset(onesb_t[:], 1.0), writes=[ONESB])
        k.dma(sync, ident_t[:], ident_in, DI, IDENT)
        k.dma(sync, tri_t[:], tri_in, DI, TRI)
        k.dma(sync, tau_t[:], tau_in, DI, TAU)
        k.op('pool', lambda e: e.memset(ones_t[:], 1.0), writes=[ONES])
        k.op('pool', lambda e: e.memset(onesk_t[:], 1.0 / 512.0), writes=[ONESK])
        xin_v = x_in.rearrange("(n p) d -> p n d", p=128)
        for i in range(NT):
            k.dma(sync, X[i][:], xin_v[:, i, :], DI, X[i])

        evac_flip = [0]

        def evac(out_ap, in_ap, reads, writes):
            evac_flip[0] ^= 1
            k.copy('act' if evac_flip[0] else 'dve', out_ap, in_ap, reads, writes)

        def build_xT(extra=None):
            for i in range(NT):
                for half in range(2):
                    ps = PS[(2 * i + half) % 4]
                    for q in range(4):
                        kk = half * 4 + q
                        k.tr(ps, ps[:, q * 128:(q + 1) * 128], X[i][:, kk * 128:(kk + 1) * 128], IDENT, [X[i]])
                    psv = ps[:].rearrange("p (q t) -> p q t", q=4)
                    if extra is None:
                        evac(xt_t[:, half * 4:half * 4 + 4, i * 128:(i + 1) * 128], psv, [ps], [XT[i // 4]])
                    else:
                        k.copy('dve', xr_t[:, half * 4:half * 4 + 4, :], psv, [ps], [XR])
                        k.copy('act', xt_t[:, half * 4:half * 4 + 4, i * 128:(i + 1) * 128], xr_t[:, half * 4:half * 4 + 4, :],
                               [XR], [XT[i // 4]])
                if extra is not None:
                    extra(i)

        def load_row(row, vec_ap):
            k.dma(sync, row[:], vec_ap.partition_broadcast(128), DI, row)

        def layer_norm_tile(i, G, Bt):
            st = MS[i % 2]
            xi = X[i]
            k.op('dve', lambda e: e.bn_stats(out=st[:, 0:6], in_=xi[:, 0:512]), reads=[xi], writes=[st])
            k.op('dve', lambda e: e.bn_stats(out=st[:, 6:12], in_=xi[:, 512:1024]), reads=[xi, st], writes=[st])
            k.op('dve', lambda e: e.bn_aggr(out=st[:, 12:14], in_=st[:, 0:12]), reads=[st], writes=[st])
            k.rsqrt_eps(st[:, 14:15], st[:, 13:14], [st], [st])
            k.ts(xi[:], xi[:], st[:, 12:13], st[:, 14:15], ALU.subtract, ALU.mult, [xi, st], [xi])
            k.tt(xi[:], xi[:], G[:], ALU.mult, [xi, G], [xi], eng='pool')
            k.tt(xi[:], xi[:], Bt[:], ALU.add, [xi, Bt], [xi], eng='pool')

        for li in layers:
            if 'A' in phases:
                build_xT()
                for i in range(NT):
                    k.dma(sync, xs_d.rearrange("(n p) d -> p n d", p=128)[:, i, :], X[i][:], X[i], XS, parts=True)
                k.barrier()
                for j in range(4):
                    k.dma(sync, sm_t[:, j * 4:j * 4 + 4], conv_w[li][j].rearrange("(h p) -> p h", p=128), DI, SM,
                          parts=(j > 0), allow_slow_non_contiguous=True)
                for (c0, src) in ((16, conv_b), (20, mh_g), (24, d_skip), (28, b_glu), (32, s5_g)):
                    k.dma(sync, sm_t[:, c0:c0 + 4], src[li].rearrange("(h p) -> p h", p=128), DI, SM, parts=True,
                          allow_slow_non_contiguous=True)
                k.dma(sync, sm_t[:, 40:48], bg_in[li].partition_broadcast(128), DI, SM, parts=True)
                k.dma('pool', wqk_t[:, 0], w_q[li].rearrange("h d e -> d h e"), DI, WQK)
                k.dma('pool', wqk_t[:, 1], w_k[li].rearrange("h d e -> d h e"), DI, WQK, parts=True)

                def load_w(buf, col0, ncols):
                    k.dma('pool', ew_t[buf][:, 0:8 * ncols].rearrange("p (k c) -> p k c", k=8),
                          w_in[li][:, col0:col0 + ncols].rearrange("(k p) c -> p k c", p=128), DI, EW[buf],
                          allow_slow_non_contiguous=(ncols < 128))
                    return ew_t[buf][:, 0:8 * ncols].rearrange("p (k c) -> p k c", k=8)

                def fm_piece(buf, col0, dest_fn, dest_tiles):
                    W = load_w(buf, col0, 512)
                    for m in range(4):
                        for n in range(4):
                            ps = PS[(m * 4 + n) % 4]
                            for kk in range(8):
                                k.mm(ps, ps[:], W[:, kk, m * 128:(m + 1) * 128], xt_t[:, kk, n * 512:(n + 1) * 512],
                                     [EW[buf], XT[n]], start=(kk == 0), stop=(kk == 7))
                            evac(dest_fn(m, n), ps[:], [ps], [dest_tiles(m, n)])

                fm_piece(0, 1544, lambda m, n: ust_t[:, m, n * 512:(n + 1) * 512], lambda m, n: UST[n])
                for h in range(4):
                    k.op('pool', lambda e, h=h: e.memset(um_v[:, h, 0:3], 0.0), writes=[UM[h]])
                fm_piece(1, 0, lambda m, n: um_v[:, m, 3 + n * 512:3 + (n + 1) * 512], lambda m, n: UM[m])
                W = load_w(0, 512, 512)
                for i in range(NT):
                    ps = PS[i % 4]
                    for kk in range(8):
                        k.mm(ps, ps[:], xt_t[:, kk, i * 128:(i + 1) * 128], W[:, kk, :], [EW[0], XT[i // 4]],
                             start=(kk == 0), stop=(kk == 7))
                    k.op('pool', lambda e, i=i: e.memset(vext_v[:, i, :, 128:129], 1.0), writes=[VEXT[i]])
                    evac(vext_v[:, i, :, 0:128], ps[:].rearrange("p (h c) -> p h c", h=4), [ps], [VEXT[i]])
                W = load_w(1, 1024, 512)
                for i in range(NT):
                    ps = PS[i % 4]
                    for kk in range(8):
                        k.mm(ps, ps[:], xt_t[:, kk, i * 128:(i + 1) * 128], W[:, kk, :], [EW[1], XT[i // 4]],
                             start=(kk == 0), stop=(kk == 7))
                    k.act(sigo_v[:, i, :], ps[:], AF.Sigmoid, [ps], [SIGO[i]])
                W = load_w(0, 1536, 8)
                for i in range(NT):
                    ps = PS[i % 4]
                    for kk in range(8):
                        k.mm(ps, ps[:, 0:8], xt_t[:, kk, i * 128:(i + 1) * 128], W[:, kk, :], [EW[0], XT[i // 4]],
                             start=(kk == 0), stop=(kk == 7))
                    k.tt(gat_t[:, i, :], ps[:, 0:8], sm_t[:, 40:48], ALU.add, [ps, SM], [GATES])

                LF = gx_t[:, 0]; BC = gx_t[:, 1]; GG = gx_t[:, 2]; AA = gx_t[:, 3]; EE = gx_t[:, 4]
                k.act(LF, gat_t[:, :, 4:8], AF.Sigmoid, [GATES], [GX])
                k.act(LF, LF, AF.Ln, [GX], [GX])
                ps = PS[4]
                for i in range(NT):
                    k.mm(ps, ps[:, i * 8:i * 8 + 4], tri_t[:], gx_t[:, 0, i, :], [TRI, GX], start=True, stop=True)
                    k.mm(ps, ps[:, i * 8 + 4:i * 8 + 8], ones_t[:], gx_t[:, 0, i, :], [ONES, GX], start=True, stop=True)
                psv = ps[:, 0:128].rearrange("p (n c) -> p n c", n=NT)
                k.copy('dve', BC, psv[:, :, 0:4], [ps], [GX])
                k.act(GG, psv[:, :, 4:8], AF.Exp, [ps], [GX])
                k.tt(AA, gat_t[:, :, 0:4], BC, ALU.subtract, [GATES, GX], [GX])
                k.act(AA, AA, AF.Exp, [GX], [GX])
                k.act(EE, BC, AF.Exp, [GX], [GX])

                for h in range(4):
                    for n in range(4):
                        acc = ROW[n % 2]
                        k.ts(acc[:, 0:512], um_v[:, h, n * 512:n * 512 + 512], sm_t[:, h:h + 1], None,
                             ALU.mult, None, [UM[h], SM], [acc])
                        for j in range(1, 4):
                            k.stt(acc[:, 0:512], um_v[:, h, n * 512 + j:n * 512 + j + 512],
                                  sm_t[:, j * 4 + h:j * 4 + h + 1], acc[:, 0:512], ALU.mult, ALU.add,
                                  [UM[h], SM, acc], [acc])
                        k.act(c_v[:, n * 512:(n + 1) * 512], acc[:, 0:512], AF.Silu, [acc, SM], [CC],
                              bias=sm_t[:, 16 + h:17 + h])
                    for n in range(4):
                        ps = PS[n % 4]
                        k.mm(ps, ps[:], wqk_t[:, 0, h, :], c_v[:, n * 512:(n + 1) * 512], [WQK, CC])
                        evac(qt_v[:, n * 512:(n + 1) * 512], ps[:], [ps], [QT])
                        ps = PS[(n + 2) % 4]
                        k.mm(ps, ps[:], wqk_t[:, 1, h, :], c_v[:, n * 512:(n + 1) * 512], [WQK, CC])
                        k.act(kt_v[:, n * 512:(n + 1) * 512], ps[:], AF.Copy, [ps], [KT], scale=128.0 ** -0.5)
                    for i4 in range(4):
                        ps = PS[i4 % 4]
                        for q in range(4):
                            i = i4 * 4 + q
                            k.mm(ps, ps[:, q * 128:(q + 1) * 128], c_v[:, i * 128:(i + 1) * 128], wqk_t[:, 1, h, :],
                                 [CC, WQK])
                        k.act(ktm_t[:, i4 * 4:i4 * 4 + 4, :], ps[:].rearrange("p (q c) -> p q c", q=4), AF.Copy,
                              [ps], [KTM], scale=128.0 ** -0.5)
                    k.op('pool', lambda e: e.memset(cst_t[:], 0.0), writes=[CST])
                    k.op('pool', lambda e: e.memset(cbf_t[:], 0.0), writes=[CBF])
                    def stage1(i, h=h):
                        sl = slice(i * 128, (i + 1) * 128)
                        b2 = i % 2
                        psS = PS[4 + b2]; psN = PS[6 + b2]; psC = PS[b2]
                        st = STB[b2]; wv = WV[b2]
                        k.mm(psS, psS[:, 0:128], kt_v[:, sl], qt_v[:, sl], [KT, QT])
                        k.tt(st[:], psS[:, 0:128], tri_t[:], ALU.mult, [psS, TRI], [st])
                        k.act(wv[:], vext_v[:, i, h, :], AF.Copy, [VEXT[i], GX], [wv], scale=gx_t[:, 3, i, h:h + 1])
                        k.mm(psN, psN[:, 0:129], st[:], wv[:], [st, wv], start=True, stop=False)
                        k.mm(psC, psC[:, 0:129], ktm_t[:, i, :], wv[:], [KTM, wv])

                    def stage2(i, h=h):
                        sl = slice(i * 128, (i + 1) * 128)
                        b2 = i % 2
                        psN = PS[6 + b2]; psC = PS[b2]; psT = PS[2 + b2]
                        hh = HH[b2]; ms = MS[b2]
                        k.mm(psN, psN[:, 0:129], qt_v[:, sl], cbf_t[:], [QT, CBF], start=False, stop=True)
                        k.tt(cst_t[:], psC[:, 0:129], cst_t[:], ALU.add, [psC, CST], [CST])
                        k.ts(cst_t[:], cst_t[:], gx_t[:, 2, i, h:h + 1], None, ALU.mult, None, [CST, GX], [CST])
                        k.copy('act', cbf_t[:], cst_t[:], [CST], [CBF])
                        k.ts(ms[:, 3:4], psN[:, 128:129], gx_t[:, 4, i, h:h + 1], None, ALU.mult, None, [psN, GX], [ms])
                        k.stt(ms[:, 0:1], ms[:, 3:4], -1.0, ms[:, 3:4], ALU.mult, ALU.max, [ms], [ms])
                        k.ts(ms[:, 0:1], ms[:, 0:1], 1.0, None, ALU.max, None, [ms], [ms])
                        k.op('dve', lambda e, ms=ms: e.reciprocal(out=ms[:, 1:2], in_=ms[:, 0:1]), reads=[ms], writes=[ms])
                        k.tt(ms[:, 2:3], ms[:, 1:2], gx_t[:, 4, i, h:h + 1], ALU.mult, [ms, GX], [ms])
                        k.stt(hh[:], psN[:, 0:128], ms[:, 2:3], sigo_v[:, i, h * 128:(h + 1) * 128], ALU.mult, ALU.mult,
                              [psN, ms, SIGO[i]], [hh])
                        k.op('dve', lambda e, ms=ms, hh=hh: e.bn_stats(out=ms[:, 4:10], in_=hh[:]), reads=[hh, ms], writes=[ms])
                        k.op('dve', lambda e, ms=ms: e.bn_aggr(out=ms[:, 10:12], in_=ms[:, 4:10]), reads=[ms], writes=[ms])
                        k.rsqrt_eps(ms[:, 12:13], ms[:, 11:12], [ms], [ms])
                        k.ts(hh[:], hh[:], ms[:, 10:11], ms[:, 12:13], ALU.subtract, ALU.mult, [hh, ms], [hh])
                        k.tr(psT, psT[:, 0:128], hh[:], IDENT, [hh])
                        k.act(xt_t[:, h, sl], psT[:, 0:128], AF.Copy, [psT, SM], [XT[i // 4]], scale=sm_t[:, 20 + h:21 + h])

                    stage1(0)
                    for i in range(NT):
                        if i + 1 < NT:
                            stage1(i + 1)
                        stage2(i)
                k.barrier()

                for (c0, src) in ((64, lam_re), (80, lam_im), (96, logdt_x)):
                    k.dma(sync, sm_t[:, c0:c0 + 16], src[li].rearrange("(j q) -> q j", q=128), DI, SM, parts=True,
                          allow_slow_non_contiguous=True)
                LR = sm_t[:, 64:80]; LI = sm_t[:, 80:96]; LDT = sm_t[:, 96:112]
                c = lambda a: sm_t[:, a:a + 16]
                DT = c(112); LRDT = c(128); LIDT = c(144); ER = c(160); CO = c(176); SI = c(192); AR = c(208); AI = c(224)
                MAG = c(240); XRr = c(256); T1 = c(272); T2 = c(288); CR = c(304); CI = c(320)
                VC128 = c(336); VS128 = c(352); ZR = c(368); ZI = c(384); ZT = c(400); ZT2 = c(416); T3 = c(432)
                smo = lambda o, a, f, **kw: k.act(o, a, f, [SM], [SM], **kw)
                smt = lambda o, a, b, op: k.tt(o, a, b, op, [SM], [SM])
                sms = lambda o, a, s1, s2, op0, op1: k.ts(o, a, s1, s2, op0, op1, [SM], [SM])
                smo(DT, LDT, AF.Exp)
                smt(LRDT, LR, DT, ALU.mult); smt(LIDT, LI, DT, ALU.mult)
                smo(ER, LRDT, AF.Exp)

                def sincos(o_sin, o_cos, ang, tmp):
                    sms(smi_t[:], ang, 1.0 / (2 * PI), None, ALU.mult, None)
                    k.stt(tmp, smi_t[:], -2 * PI, ang, ALU.mult, ALU.add, [SM], [SM])
                    smo(o_sin, tmp, AF.Sin, scale=0.999999)
                    sms(o_cos, ang, 0.5 * PI, None, ALU.add, None)
                    sms(smi_t[:], o_cos, 1.0 / (2 * PI), None, ALU.mult, None)
                    k.stt(tmp, smi_t[:], -2 * PI, o_cos, ALU.mult, ALU.add, [SM], [SM])
                    smo(o_cos, tmp, AF.Sin, scale=0.999999)
                sincos(SI, CO, LIDT, T1)
                smt(AR, ER, CO, ALU.mult); smt(AI, ER, SI, ALU.mult)
                smt(MAG, LR, LR, ALU.mult); smt(T1, LI, LI, ALU.mult); smt(MAG, MAG, T1, ALU.add)
                k.op('dve', lambda e: e.reciprocal(out=MAG, in_=MAG), reads=[SM], writes=[SM])
                sms(XRr, AR, -1.0, None, ALU.add, None)
                smt(T1, XRr, LR, ALU.mult); smt(T2, AI, LI, ALU.mult); smt(T1, T1, T2, ALU.add); smt(CR, T1, MAG, ALU.mult)
                smt(T1, AI, LR, ALU.mult); smt(T2, XRr, LI, ALU.mult); smt(T1, T1, T2, ALU.subtract); smt(CI, T1, MAG, ALU.mult)
                sms(T3, LIDT, 128.0, None, ALU.mult, None)
                sincos(VS128, VC128, T3, T1)
                smo(T2, LRDT, AF.Exp, scale=128.0)
                smt(VC128, VC128, T2, ALU.mult); smt(VS128, VS128, T2, ALU.mult)
                k.dma(sync, cri_d[0].rearrange("(j q) -> q j", q=128), CR, SM, CRI, allow_slow_non_contiguous=True)
                k.dma(sync, cri_d[1].rearrange("(j q) -> q j", q=128), CI, SM, CRI, parts=True, allow_slow_non_contiguous=True)
                for ct in range(4):
                    cs = slice(ct * 512, (ct + 1) * 512)
                    k.dma(sync, TP[0][:], brp[li][:, cs], DI, TP[0])
                    k.dma(sync, TP[1][:], bip[li][:, cs], DI, TP[1])
                    k.dma(sync, TP[2][:], cri_d[0, cs].partition_broadcast(128), CRI, TP[2])
                    k.dma(sync, TP[3][:], cri_d[1, cs].partition_broadcast(128), CRI, TP[3])
                    k.tt(TP[4][:], TP[2][:], TP[0][:], ALU.mult, [TP[2], TP[0]], [TP[4]])
                    k.tt(TP[5][:], TP[3][:], TP[1][:], ALU.mult, [TP[3], TP[1]], [TP[5]])
                    k.tt(bb_v[:, 0, cs], TP[4][:], TP[5][:], ALU.subtract, [TP[4], TP[5]], [BB])
                    k.tt(TP[4][:], TP[2][:], TP[1][:], ALU.mult, [TP[2], TP[1]], [TP[4]])
                    k.tt(TP[5][:], TP[3][:], TP[0][:], ALU.mult, [TP[3], TP[0]], [TP[5]])
                    k.tt(bb_v[:, 1, cs], TP[4][:], TP[5][:], ALU.add, [TP[4], TP[5]], [BB])
                for jb in range(4):
                    jq = slice(jb * 4, jb * 4 + 4)
                    t1_ = TP[6 + jb % 2]; t2_ = TP[8 + jb % 2]
                    v1_ = t1_[:].rearrange("p (j c) -> p j c", j=4); v2_ = t2_[:].rearrange("p (j c) -> p j c", j=4)
                    k.dma(sync, v1_, crp[li][:, jq, :], DI, t1_)
                    k.dma(sync, v2_, cip[li][:, jq, :], DI, t2_)
                    k.copy('act', cw_t[:, 0, jq, :], v1_, [t1_], [CW])
                    k.act(cw_t[:, 1, jq, :], v1_, AF.Copy, [t1_], [CW], scale=-1.0)
                    k.act(cw_t[:, 2, jq, :], v2_, AF.Copy, [t2_], [CW], scale=-1.0)
                for jb in range(4):
                    jsl = slice(jb * 4, jb * 4 + 4)
                    v3 = lambda t: t[:].rearrange("p (j c) -> p j c", j=4)
                    taub = tau_t[:].unsqueeze(1).broadcast_to([128, 4, 128])
                    lidb = sm_t[:, 144 + jb * 4:144 + jb * 4 + 4].unsqueeze(2).broadcast_to([128, 4, 128])
                    lrdb = sm_t[:, 128 + jb * 4:128 + jb * 4 + 4].unsqueeze(2).broadcast_to([128, 4, 128])
                    ANG, TMPa, SINT, COST, LTt, MAGP, MAGN = TP[0], TP[1], TP[2], TP[3], TP[4], TP[5], TP[6]
                    k.tt(v3(ANG), taub, lidb, ALU.mult, [TAU, SM], [ANG])
                    KI = TP[7]; kiv = KI[:].bitcast(mybir.dt.int32)
                    k.ts(kiv, ANG[:], 1.0 / (2 * PI), None, ALU.mult, None, [ANG], [KI])
                    k.stt(TMPa[:], kiv, -2 * PI, ANG[:], ALU.mult, ALU.add, [KI, ANG], [TMPa])
                    k.act(SINT[:], TMPa[:], AF.Sin, [TMPa], [SINT], scale=0.999999)
                    k.ts(ANG[:], ANG[:], 0.5 * PI, None, ALU.add, None, [ANG], [ANG])
                    k.ts(kiv, ANG[:], 1.0 / (2 * PI), None, ALU.mult, None, [ANG], [KI])
                    k.stt(TMPa[:], kiv, -2 * PI, ANG[:], ALU.mult, ALU.add, [KI, ANG], [TMPa])
                    k.act(COST[:], TMPa[:], AF.Sin, [TMPa], [COST], scale=0.999999)
                    k.tt(v3(LTt), taub, lrdb, ALU.mult, [TAU, SM], [LTt])
                    k.act(MAGP[:], LTt[:], AF.Exp, [LTt], [MAGP])
                    k.act(MAGN[:], LTt[:], AF.Exp, [LTt], [MAGN], scale=-1.0)
                    k.tt(tab_v[:, 0, jsl, :], v3(MAGN), v3(COST), ALU.mult, [MAGN, COST], [TAB])
                    k.tt(tab_v[:, 1, jsl, :], v3(MAGN), v3(SINT), ALU.mult, [MAGN, SINT], [TAB])
                    k.tt(tab_v[:, 2, jsl, :], v3(MAGP), v3(COST), ALU.mult, [MAGP, COST], [TAB])
                    k.tt(tab_v[:, 3, jsl, :], v3(MAGP), v3(SINT), ALU.mult, [MAGP, SINT], [TAB])
                k.dma('pool', ew_t[0][:, 0:2048].rearrange("p (k c) -> p k c", k=4),
                      w_glu[li].rearrange("(k p) c -> p k c", p=128), DI, EW[0])
                WGLU = ew_t[0][:, 0:2048].rearrange("p (k c) -> p k c", k=4)
                k.op('pool', lambda e: e.memset(sm_t[:, 368:400], 0.0), reads=[SM], writes=[SM])
                P1, P2, P3, P4, SR, SIi = TP[0], TP[1], TP[2], TP[3], TP[4], TP[5]
                GYF = TP[6:10]
                YF = TP[10]; TQ = TP[11]
                b4 = lambda ap: ap.unsqueeze(1).broadcast_to([128, 4, 128])
                v4 = lambda t: t[:].rearrange("p (c t) -> p c t", c=4)
                for n in range(4):
                    ns = slice(n * 512, (n + 1) * 512)
                    for ct in range(4):
                        psY = PS[4 + ct % 2]
                        for g2 in range(2):
                            grp = []
                            for q in range(2):
                                jj = 2 * g2 + q
                                j = ct * 4 + jj
                                psR = PS[q]; psI = PS[2 + q]
                                Pq = TP[0:4] if q == 0 else TP2
                                Sq = (TP[4], TP[5]) if q == 0 else (TP[10], TP[11])
                                k.mm(psR, psR[:], bb_v[:, 0, j * 128:(j + 1) * 128], ust_t[:, ct, ns], [BB, UST[n]])
                                k.mm(psI, psI[:], bb_v[:, 1, j * 128:(j + 1) * 128], ust_t[:, ct, ns], [BB, UST[n]])
                                pr = psR[:].rearrange("p (c t) -> p c t", c=4); pi_ = psI[:].rearrange("p (c t) -> p c t", c=4)
                                k.tt(v4(Pq[0]), pr, b4(tab_v[:, 0, j, :]), ALU.mult, [psR, TAB], [Pq[0]])
                                k.tt(v4(Pq[1]), pi_, b4(tab_v[:, 1, j, :]), ALU.mult, [psI, TAB], [Pq[1]])
                                k.tt(v4(Pq[2]), pi_, b4(tab_v[:, 0, j, :]), ALU.mult, [psI, TAB], [Pq[2]])
                                k.tt(v4(Pq[3]), pr, b4(tab_v[:, 1, j, :]), ALU.mult, [psR, TAB], [Pq[3]])
                                grp.append((jj, j, Pq, Sq))
                            for cc in range(4):
                                cs = slice(cc * 128, (cc + 1) * 128)
                                last = cc * 128 + 127
                                for (jj, j, Pq, Sq) in grp:
                                    k.op('dve', lambda e, cs=cs, j=j, Pq=Pq, Sq=Sq: e.tensor_tensor_scan(
                                        out=Sq[0][:, cs], data0=Pq[0][:, cs], data1=Pq[1][:, cs], initial=sm_t[:, 368 + j:369 + j],
                                        op0=ALU.add, op1=ALU.add), reads=[Pq[0], Pq[1], SM], writes=[Sq[0]])
                                    k.op('dve', lambda e, cs=cs, j=j, Pq=Pq, Sq=Sq: e.tensor_tensor_scan(
                                        out=Sq[1][:, cs], data0=Pq[2][:, cs], data1=Pq[3][:, cs], initial=sm_t[:, 384 + j:385 + j],
                                        op0=ALU.add, op1=ALU.subtract), reads=[Pq[2], Pq[3], SM], writes=[Sq[1]])
                                for (jj, j, Pq, Sq) in grp:
                                    k.ts(sm_t[:, 400 + j:401 + j], Sq[1][:, last:last + 1], sm_t[:, 352 + j:353 + j], None,
                                         ALU.mult, None, [Sq[1], SM], [SM])
                                    k.ts(sm_t[:, 416 + j:417 + j], Sq[0][:, last:last + 1], sm_t[:, 352 + j:353 + j], None,
                                         ALU.mult, None, [Sq[0], SM], [SM])
                                for (jj, j, Pq, Sq) in grp:
                                    k.stt(sm_t[:, 368 + j:369 + j], Sq[0][:, last:last + 1], sm_t[:, 336 + j:337 + j],
                                          sm_t[:, 400 + j:401 + j], ALU.mult, ALU.subtract, [Sq[0], SM], [SM])
                                    k.stt(sm_t[:, 384 + j:385 + j], Sq[1][:, last:last + 1], sm_t[:, 336 + j:337 + j],
                                          sm_t[:, 416 + j:417 + j], ALU.mult, ALU.add, [Sq[1], SM], [SM])
                            for (jj, j, Pq, Sq) in grp:
                                SRq, SIq = Sq
                                k.tt(v4(RB[0]), v4(SRq), b4(tab_v[:, 2, j, :]), ALU.mult, [SRq, TAB], [RB[0]], eng='pool')
                                k.tt(v4(RB[1]), v4(SIq), b4(tab_v[:, 3, j, :]), ALU.mult, [SIq, TAB], [RB[1]], eng='pool')
                                k.tt(v4(RB[2]), v4(SIq), b4(tab_v[:, 2, j, :]), ALU.mult, [SIq, TAB], [RB[2]], eng='pool')
                                k.tt(v4(RB[3]), v4(SRq), b4(tab_v[:, 3, j, :]), ALU.mult, [SRq, TAB], [RB[3]], eng='pool')
                                for r, wsel in enumerate((0, 1, 2, 2)):
                                    k.mm(psY, psY[:], cw_t[:, wsel, j, :], RB[r][:], [CW, RB[r]],
                                         start=(jj == 0 and r == 0), stop=(jj == 3 and r == 3))
                        k.stt(YF[:], ust_t[:, ct, ns], sm_t[:, 24 + ct:25 + ct], psY[:], ALU.mult, ALU.add,
                              [UST[n], SM, psY], [YF])
                        k.tt(TQ[:], YF[:], YF[:], ALU.mult, [YF], [TQ], eng='pool')
                        k.ts(TQ[:], TQ[:], 0.044715, 1.0, ALU.mult, ALU.add, [TQ], [TQ], eng='pool')
                        k.tt(TQ[:], TQ[:], YF[:], ALU.mult, [TQ, YF], [TQ], eng='pool')
                        k.act(TQ[:], TQ[:], AF.Sigmoid, [TQ], [TQ], scale=2.0 * math.sqrt(2.0 / PI))
                        k.tt(GYF[ct][:], YF[:], TQ[:], ALU.mult, [YF, TQ], [GYF[ct]], eng='pool')
                        k.copy('act', gyb_v[:, ct, :], GYF[ct][:], [GYF[ct]], [GYB])
                    for cto in range(4):
                        ps = PS[6 + cto % 2]
                        for ci in range(4):
                            k.mm(ps, ps[:], WGLU[:, ci, cto * 128:(cto + 1) * 128], gyb_v[:, ci, :], [EW[0], GYB],
                                 start=(ci == 0), stop=(ci == 3))
                        k.act(TQ[:], ps[:], AF.Sigmoid, [ps, SM], [TQ], bias=sm_t[:, 28 + cto:29 + cto])
                        k.tt(GYF[cto][:], GYF[cto][:], TQ[:], ALU.mult, [GYF[cto], TQ], [GYF[cto]])
                        k.act(TP[cto][:], GYF[cto][:], AF.Square, [GYF[cto]], [TP[cto]])
                    ps = PS[6]
                    for ct in range(4):
                        k.mm(ps, ps[:], onesk_t[:], TP[ct][:], [ONESK, TP[ct]], start=(ct == 0), stop=(ct == 3))
                    k.rsqrt_eps(YF[:], ps[:], [ps], [YF])
                    for ct in range(4):
                        k.stt(xt_t[:, 4 + ct, ns], GYF[ct][:], sm_t[:, 32 + ct:33 + ct], YF[:], ALU.mult, ALU.mult,
                              [GYF[ct], SM, YF], [XT[n]])
                k.barrier()

                load_row(ROW[0], ln1_g[li]); load_row(ROW[1], ln1_b[li])
                for hf in range(2):
                    k.dma('pool', ew_t[hf][:, 0:4096].rearrange("p (k c) -> p k c", k=8),
                          w_out[li][:, hf * 512:(hf + 1) * 512].rearrange("(k p) c -> p k c", p=128), DI, EW[hf])
                for i in range(NT):
                    xlt = XLB[1]
                    xsv = xs_d.rearrange("(n p) d -> p n d", p=128)
                    k.dma(sync, xlt[:], xsv[:, i, :], XS, xlt)
                    for hf in range(2):
                        ps = PS[(2 * i + hf) % 4]
                        W = ew_t[hf][:, 0:4096].rearrange("p (k c) -> p k c", k=8)
                        for kk in range(8):
                            k.mm(ps, ps[:], xt_t[:, kk, i * 128:(i + 1) * 128], W[:, kk, :], [XT[i // 4], EW[hf]],
                                 start=(kk == 0), stop=(kk == 7))
                        k.stt(X[i][:, hf * 512:(hf + 1) * 512], xlt[:, hf * 512:(hf + 1) * 512], ALPHA, ps[:], ALU.mult, ALU.add, [xlt, ps], [X[i]])
                    layer_norm_tile(i, ROW[0], ROW[1])
                k.barrier()

            if 'B' in phases:
                k.dma(sync, wr_t[:], w_r[li].rearrange("(k p) c -> p k c", p=128), DI, WR)
                k.dma(sync, gx_t[:].rearrange("p a n c -> p (a n c)")[:, 0:36], b_r[li].partition_broadcast(128), DI, GX)
                BIASR = gx_t[:].rearrange("p a n c -> p (a n c)")[:, 0:36]
                POS = k.tile(gw_t[:], 'POS')
                lgg_v = hh_t[:, 0, 0:64].rearrange("p (n g) -> p n g", n=NT)
                ej_v = hh_t[:, 0, 64:128].rearrange("p (n g) -> p n g", n=NT)
                oh_v = hh_t[:, 1, 0:64].rearrange("p (n g) -> p n g", n=NT)
                mk_v = hh_t[:, 1, 64:128].rearrange("p (n g) -> p n g", n=NT)
                vec = lambda a: sm_t[:, 256 + a * 16:256 + (a + 1) * 16]
                GMX, SEv, Dv, W1v, W2v, C1v, C2v = [vec(a) for a in range(7)]
                em_v = sm2_t[:].rearrange("p (n e) -> p n e", n=NT)
                m8_v = gat_t

                def routing(i):
                    ps = PS[4 + i % 2]
                    for kk in range(8):
                        k.mm(ps, ps[:, 0:36], xr_t[:, kk, :], wr_t[:, kk, :], [XR, WR], start=(kk == 0), stop=(kk == 7))
                    k.tt(lgg_v[:, i, :], ps[:, 0:4], BIASR[:, 0:4], ALU.add, [ps, GX], [HH[0]])
                    k.tt(gw_t[:, i, :], ps[:, 4:36], BIASR[:, 4:36], ALU.add, [ps, GX], [POS])

                def routing_batched():
                    b3 = lambda ap, n: ap.unsqueeze(2).broadcast_to([128, NT, n])
                    k.op('dve', lambda e: e.tensor_reduce(out=GMX, in_=lgg_v, axis=mybir.AxisListType.X, op=ALU.max),
                         reads=[HH[0]], writes=[SM])
                    k.tt(lgg_v, lgg_v, b3(GMX, 4), ALU.subtract, [HH[0], SM], [HH[0]])
                    k.act(ej_v, lgg_v, AF.Exp, [HH[0]], [HH[0]])
                    k.op('dve', lambda e: e.tensor_reduce(out=SEv, in_=ej_v, axis=mybir.AxisListType.X, op=ALU.add),
                         reads=[HH[0]], writes=[SM])
                    k.op('dve', lambda e: e.reciprocal(out=SEv, in_=SEv), reads=[SM], writes=[SM])
                    k.ts(oh_v, lgg_v, 0.0, None, ALU.is_equal, None, [HH[0]], [HH[1]])
                    k.ts(mk_v, oh_v, -1.0, 1e30, ALU.add, ALU.mult, [HH[1]], [HH[1]])
                    k.tt(sm2_t[:].rearrange("p (n g e) -> p n g e", n=NT, g=4),
                         gw_t[:].rearrange("p n (g e) -> p n g e", g=4),
                         mk_v.unsqueeze(3).broadcast_to([128, NT, 4, 8]), ALU.add, [POS, HH[1]], [SM])
                    for i in range(NT):
                        k.op('dve', lambda e, i=i: e.max(out=m8_v[:, i, :], in_=em_v[:, i, :]), reads=[SM], writes=[GATES])
                    V1 = m8_v[:, :, 0]; V2 = m8_v[:, :, 1]
                    k.tt(Dv, V1, V2, ALU.subtract, [GATES], [SM])
                    k.act(W1v, Dv, AF.Sigmoid, [SM], [SM])
                    k.ts(W2v, W1v, -1.0, 1.0, ALU.mult, ALU.add, [SM], [SM])
                    k.tt(C1v, W1v, SEv, ALU.mult, [SM], [SM]); k.tt(C2v, W2v, SEv, ALU.mult, [SM], [SM])
                    k.tt(gw_t[:], em_v, b3(V1, 32), ALU.is_equal, [SM, GATES], [POS])
                    k.tt(gw_t[:], gw_t[:], b3(C1v, 32), ALU.mult, [POS, SM], [POS])
                    k.tt(meta_v[:, :, :, 0], em_v, b3(V2, 32), ALU.is_equal, [SM, GATES], [META])
                    k.tt(em_v, meta_v[:, :, :, 0], b3(C2v, 32), ALU.mult, [META, SM], [SM])
                    k.tt(gw_t[:], gw_t[:], em_v, ALU.add, [POS, SM], [POS])
                    k.copy('dve', meta_v[:, :, :, 1], gw_t[:], [POS], [META])
                    k.ts(mall_v, gw_t[:], 0.0, None, ALU.is_gt, None, [POS], [MALL])

                k.op('pool', lambda e: e.memset(out_v[0], 0.0), writes=[OUTB[0]])
                ydv = yd_d.rearrange("(n p) d -> p n d", p=128)
                for r in range(32):
                    k.dma(sync, ydv[:, r, :], out_v[0], OUTB[0], YD, parts=True)
                k._wait('pool', (YD.dsem, YD.dval, None, 'd_yd'))
                for i in range(NT):
                    for half in range(2):
                        ps = PS[(2 * i + half) % 4]
                        for q in range(4):
                            kk = half * 4 + q
                            k.tr(ps, ps[:, q * 128:(q + 1) * 128], X[i][:, kk * 128:(kk + 1) * 128], IDENT, [X[i]])
                        k.copy('dve', xr_t[:, half * 4:half * 4 + 4, :], ps[:].rearrange("p (q t) -> p q t", q=4), [ps], [XR])
                    k.copy('act', xtm_v[:, i, :], X[i][:], [X[i]], [XTM[i]])
                    routing(i)
                routing_batched()
                for i in range(NT):
                    ps = PS[4 + i % 2]
                    k.mm(ps, ps[:, 0:32], stri_t[:], mall_v[:, i, :], [STRI, MALL], start=True, stop=(i == 0))
                    for j in range(i):
                        k.mm(ps, ps[:, 0:32], onesb_t[:], mall_v[:, j, :], [ONESB, MALL], start=False, stop=(j == i - 1))
                    k.stt(gw_t[:, i, :], ps[:, 0:32], 1.0, mall_v[:, i, :], ALU.add, ALU.mult, [ps, MALL], [POS])
                k.ts(gw_t[:].rearrange("p n e -> p (n e)"), gw_t[:].rearrange("p n e -> p (n e)"), -1.0, None, ALU.add, None,
                     [POS], [POS])

                k.op('pool', lambda e: e.memset(out_v[0], 0.0), writes=[OUTB[0]])
                ydv = yd_d.rearrange("(n p) d -> p n d", p=128)
                for r in range(32):
                    k.dma(sync, ydv[:, r, :], out_v[0], OUTB[0], YD, parts=True)
                k._wait('pool', (YD.dsem, YD.dval, None, 'd_yd'))
                for i in range(NT):
                    for half in range(2):
                        ps = PS[(2 * i + half) % 4]
                        for q in range(4):
                            kk = half * 4 + q
                            k.tr(ps, ps[:, q * 128:(q + 1) * 128], X[i][:, kk * 128:(kk + 1) * 128], IDENT, [X[i]])
                        k.copy('dve', xr_t[:, half * 4:half * 4 + 4, :], ps[:].rearrange("p (q t) -> p q t", q=4), [ps], [XR])
                    k.copy('act', xtm_v[:, i, :], X[i][:], [X[i]], [XTM[i]])
                    routing(i)
                routing_batched()
                for i in range(NT):
                    ps = PS[4 + i % 2]
                    k.mm(ps, ps[:, 0:32], stri_t[:], mall_v[:, i, :], [STRI, MALL], start=True, stop=(i == 0))
                    for j in range(i):
                        k.mm(ps, ps[:, 0:32], onesb_t[:], mall_v[:, j, :], [ONESB, MALL], start=False, stop=(j == i - 1))
                    k.stt(gw_t[:, i, :], ps[:, 0:32], 1.0, mall_v[:, i, :], ALU.add, ALU.mult, [ps, MALL], [POS])
                k.ts(gw_t[:].rearrange("p n e -> p (n e)"), gw_t[:].rearrange("p n e -> p (n e)"), -1.0, None, ALU.add, None,
                     [POS], [POS])

                def load_expert(e, buf):
                    k.dma('pool', ew_t[buf][:, 0:4096].rearrange("p (k c) -> p k c", k=8),
                          w_eg[li, e].rearrange("(k p) c -> p k c", p=128), DI, EW[buf])
                    k.dma('pool', ew_t[buf][:, 4096:8192].rearrange("p (k c) -> p k c", k=8),
                          w_eu[li, e].rearrange("(k p) c -> p k c", p=128), DI, EW[buf], parts=True)
                    k.dma('pool', ew_t[buf][:, 8192:12288].rearrange("p (k c) -> p k c", k=4),
                          w_ed[li, e].rearrange("(k p) c -> p k c", p=128), DI, EW[buf], parts=True)

                if n_experts > 0:
                    load_expert(0, 0)
                for e_ in range(n_experts):
                    buf = e_ % 2
                    if e_ + 1 < n_experts:
                        load_expert(e_ + 1, 1 - buf)
                    WG = ew_t[buf][:, 0:4096].rearrange("p (k c) -> p k c", k=8)
                    WU = ew_t[buf][:, 4096:8192].rearrange("p (k c) -> p k c", k=8)
                    WD = ew_t[buf][:, 8192:12288].rearrange("p (k c) -> p k c", k=4)
                    for i in range(NT):
                        selt = ROW[i // 8]
                        k.ts(sel_v[i // 8][:, i % 8, :], iota_t[:], gw_t[:, i, e_:e_ + 1], None, ALU.is_equal, None,
                             [IOTA, POS], [selt], eng=('dve' if (i % 2 == 0 or _os.environ.get('KSEL', 'dve') == 'dve') else 'pool'))
                    xg = XG[buf]; xgv = xg_v[buf]
                    for kf in range(8):
                        ps = PS[kf % 4]
                        for i in range(NT):
                            k.mm(ps, ps[:, 0:256], xtm_v[:, i, kf * 128:(kf + 1) * 128], sel_v[i // 8][:, i % 8, :],
                                 [XTM[i], ROW[i // 8]], start=(i == 0), stop=(i == NT - 1))
                        evac(xgv[:, kf, :], ps[:, 0:256], [ps], [xg])
                    psM = PS[4]
                    for sh in range(2):
                        for i in range(NT):
                            k.mm(psM, psM[:, sh * 8:sh * 8 + 3], sel_v[i // 8][:, i % 8, sh * 128:(sh + 1) * 128],
                                 cmeta_t[:, i, :], [ROW[i // 8], CMETA], start=(i == 0), stop=(i == NT - 1))
                        for i in range(NT):
                            k.mm(psM, psM[:, sh * 8 + 3:sh * 8 + 5], sel_v[i // 8][:, i % 8, sh * 128:(sh + 1) * 128],
                                 meta_v[:, i, e_, :], [ROW[i // 8], META], start=(i == 0), stop=(i == NT - 1))
                    sl = SLOT[buf]; slv = slot_t[:, buf]
                    k.copy('dve', slv.rearrange("p a c -> p (a c)"), psM[:, 0:16], [psM], [sl])
                    k.stt(slv[:, :, 5], slv[:, :, 0], 128.0, slv[:, :, 1], ALU.mult, ALU.add, [sl], [sl])
                    k.stt(slv[:, :, 5], slv[:, :, 3], 2048.0, slv[:, :, 5], ALU.mult, ALU.add, [sl], [sl])
                    k.ts(slv[:, :, 6], slv[:, :, 2], -1.0e6, 1.0e6, ALU.mult, ALU.add, [sl], [sl])
                    for sh in range(2):
                        k.tt(dest_tt[buf][sh][:, :], slv[:, sh, 5:6], slv[:, sh, 6:7], ALU.add, [sl], [DEST[buf][sh]])
                    hte = HTE[buf]; htev = hte_v[buf]
                    for m in range(4):
                        psg = PS[5 + (m % 2) * 2 - (m % 2)]; psu = PS[6 + (m % 2)]
                        psg = PS[4 + (m % 2) * 2 + 1] if False else PS[5] if m % 2 == 0 else PS[7]
                        psu = PS[6] if m % 2 == 0 else PS[4]
                        for kk in range(8):
                            k.mm(psg, psg[:, 0:256], WG[:, kk, m * 128:(m + 1) * 128], xgv[:, kk, :], [EW[buf], xg],
                                 start=(kk == 0), stop=(kk == 7))
                        for kk in range(8):
                            k.mm(psu, psu[:, 256:512], WU[:, kk, m * 128:(m + 1) * 128], xgv[:, kk, :], [EW[buf], xg],
                                 start=(kk == 0), stop=(kk == 7))
                        sg = SG[m % 2]
                        k.act(sg[:], psg[:, 0:256], AF.Silu, [psg], [sg])
                        k.tt(htev[:, m, :], sg[:], psu[:, 256:512], ALU.mult, [sg, psu], [hte])
                    for sh in range(2):
                        ob = OUTB[sh]
                        for hf in range(2):
                            ps = PS[(sh * 2 + hf) % 4]
                            for m in range(4):
                                k.mm(ps, ps[:], htev[:, m, sh * 128:(sh + 1) * 128], WD[:, m, hf * 512:(hf + 1) * 512],
                                     [hte, EW[buf]], start=(m == 0), stop=(m == 3))
                            k.act(out_v[sh][:, hf * 512:(hf + 1) * 512], ps[:], AF.Copy, [ps, sl], [ob], scale=slv[:, sh, 4:5])
                        k.dma_scatter(yd_d[:, :], dest_tt[buf][sh][:, :], out_v[sh], ob, DEST[buf][sh], YD, 4095)
                if _os.environ.get('KDEBUG', '') == 'dbgslot':
                    k.barrier()
                    k.copy('dve', X[0][:, 0:16], slot_t[:, 1].rearrange("p a c -> p (a c)"), [SLOT[1]], [X[0]])
                    k.copy('dve', X[0][:, 16:17], dest_tt[1][0][:, :], [DEST[1][0]], [X[0]])
                    k.copy('dve', X[0][:, 17:18], dest_tt[1][1][:, :], [DEST[1][1]], [X[0]])
                    k.copy('dve', X[0][:, 32:64], gw_t[:, 0, :], [POS], [X[0]])
                    k.copy('dve', X[0][:, 64:96], mall_v[:, 0, :], [MALL], [X[0]])
                    k.copy('dve', X[0][:, 96:128], meta_v[:, 0, :, 1], [META], [X[0]])
                    k.copy('dve', X[0][:, 128:160], gw_t[:, 15, :], [POS], [X[0]])
                for i in range(NT if _os.environ.get('KDEBUG', '') != 'dbgslot' else 0):
                    for kq in range(2):
                        ob = OUTB[kq]
                        k.dma(sync, out_v[kq], ydv[:, kq * 16 + i, :], YD, ob)
                        if kq == 0:
                            k.stt(X[i][:], X[i][:], ALPHA, out_v[kq], ALU.mult, ALU.add, [X[i], ob], [X[i]])
                        else:
                            k.tt(X[i][:], X[i][:], out_v[kq], ALU.add, [X[i], ob], [X[i]], eng='pool')
                k.barrier()

            if 'C' in phases:
                load_row(ROW[0], ln2_g[li]); load_row(ROW[1], ln2_b[li])
                for i in range(NT):
                    layer_norm_tile(i, ROW[0], ROW[1])
                build_xT()
                load_row(ROW[0], b_pg[li]); load_row(ROW[1], ple_g[li])
                for hf in range(2):
                    k.dma('pool', ew_t[hf][:, 0:4096].rearrange("p (k c) -> p k c", k=8),
                          w_pg[li][:, hf * 512:(hf + 1) * 512].rearrange("(k p) c -> p k c", p=128), DI, EW[hf])
                k.dma('pool', ew_t[0][:, 4096:6144].rearrange("p (k c) -> p k c", k=2),
                      w_pp[li].rearrange("(k p) c -> p k c", p=128), DI, EW[0], parts=True)
                WPP = ew_t[0][:, 4096:6144].rearrange("p (k c) -> p k c", k=2)
                PTt = [UST[n] for n in range(4)]
                for n in range(4):
                    k.dma('pool', ust_t[:, 0:2, n * 512:(n + 1) * 512],
                          pT_in[li][:, n * 512:(n + 1) * 512].rearrange("(k p) t -> p k t", p=128), DI, UST[n])
                for i in range(NT):
                    sl = slice(i * 128, (i + 1) * 128)
                    ms = MS[i % 2]
                    psG = [PS[0 + (i % 2) * 4], PS[1 + (i % 2) * 4]]
                    psP = [PS[2 + (i % 2) * 4], PS[3 + (i % 2) * 4]]
                    for hf in range(2):
                        W = ew_t[hf][:, 0:4096].rearrange("p (k c) -> p k c", k=8)
                        for kk in range(8):
                            k.mm(psG[hf], psG[hf][:], xt_t[:, kk, sl], W[:, kk, :], [XT[i // 4], EW[hf]],
                                 start=(kk == 0), stop=(kk == 7))
                        for k2 in range(2):
                            k.mm(psP[hf], psP[hf][:], ust_t[:, k2, sl], WPP[:, k2, hf * 512:(hf + 1) * 512],
                                 [UST[i // 4], EW[0]], start=(k2 == 0), stop=(k2 == 1))
                    tq = CTMP[(i % 2) * 3]; tg = CTMP[(i % 2) * 3 + 1]; tp_ = CTMP[(i % 2) * 3 + 2]
                    for hf in range(2):
                        k.act(tq[:], psP[hf][:], AF.Square, [psP[hf]], [tq, ms], accum_out=ms[:, 16 + hf:17 + hf])
                    k.tt(ms[:, 18:19], ms[:, 16:17], ms[:, 17:18], ALU.add, [ms], [ms])
                    k.rsqrt_eps(ms[:, 19:20], ms[:, 18:19], [ms], [ms], pre_scale=1.0 / 1024.0)
                    for hf in range(2):
                        hs = slice(hf * 512, (hf + 1) * 512)
                        k.tt(tg[:], psG[hf][:], row_t[0][:, hs], ALU.add, [psG[hf], ROW[0]], [tg])
                        k.act(tg[:], tg[:], AF.Sigmoid, [tg], [tg])
                        k.stt(tp_[:], psP[hf][:], ms[:, 19:20], row_t[1][:, hs], ALU.mult, ALU.mult, [psP[hf], ms, ROW[1]], [tp_])
                        k.tt(tg[:], tg[:], tp_[:], ALU.mult, [tg, tp_], [tg], eng='pool')
                        k.tt(X[i][:, hs], X[i][:, hs], tg[:], ALU.add, [X[i], tg], [X[i]], eng='pool')
                k.barrier()

        for _pi in range(int(_os.environ.get('KPAD', '0'))):
            k.op('dve', lambda e: e.memset(sm2_t[:, 300:301], 0.0), writes=[])
        yv = y_out.rearrange("(n p) d -> p n d", p=128)
        for i in range(NT):
            k.dma(sync, yv[:, i, :], X[i][:], X[i], YO, parts=True)
        if not k.dry:
            nc.sync.wait_ge(YO.dsem, YO.dval)
    nc._declared_inputs = declared
    return nc, k.waited


def prep_shared(inp):
    f = lambda a: np.ascontiguousarray(np.asarray(a, dtype=np.float32))
    L = DEPTH
    sh = {}
    for n in ['w_in', 'conv_w', 'conv_b', 'w_q', 'w_k', 'mh_g', 'w_glu', 'b_glu', 's5_g', 'w_out', 'ln1_g', 'ln1_b',
              'w_eg', 'w_eu', 'w_ed', 'ln2_g', 'ln2_b', 'w_pg', 'b_pg', 'w_pp', 'ple_g']:
        sh[n] = f(inp[n])
    sh['bg'] = f(np.concatenate([np.asarray(inp['b_i']), np.asarray(inp['b_f'])], axis=1))
    sh['lam_re'] = f(np.asarray(inp['lam_re']).reshape(L, 2048))
    sh['lam_im'] = f(np.asarray(inp['lam_im']).reshape(L, 2048))
    sh['logdt_x'] = f(np.repeat(np.asarray(inp['log_dt'])[:, :, None], 64, axis=2).reshape(L, 2048))
    sh['d_skip'] = f(np.asarray(inp['d_skip']).reshape(L, 512))
    b_re = np.asarray(inp['b_re']); b_im = np.asarray(inp['b_im'])
    c_re = np.asarray(inp['c_re']); c_im = np.asarray(inp['c_im'])
    brp = np.zeros((L, 128, 32, 64), np.float32); bip = np.zeros((L, 128, 32, 64), np.float32)
    crp = np.zeros((L, 2, 64, 16, 128), np.float32); cip = np.zeros((L, 2, 64, 16, 128), np.float32)
    for g in range(32):
        r0 = (g % 8) * 16
        brp[:, r0:r0 + 16, g, :] = np.transpose(b_re[:, g], (0, 2, 1))
        bip[:, r0:r0 + 16, g, :] = np.transpose(b_im[:, g], (0, 2, 1))
        j, gi = g // 2, g % 2
        crp[:, gi, :, j, r0:r0 + 16] = np.transpose(c_re[:, g], (0, 2, 1))
        cip[:, gi, :, j, r0:r0 + 16] = np.transpose(c_im[:, g], (0, 2, 1))
    sh['brp'] = brp.reshape(L, 128, 2048); sh['bip'] = bip.reshape(L, 128, 2048)
    sh['crp'] = crp.reshape(L, 128, 16, 128); sh['cip'] = cip.reshape(L, 128, 16, 128)
    sh['w_r'] = f(np.concatenate([np.asarray(inp['w_grp']), np.asarray(inp['w_rt'])], axis=2))
    sh['b_r'] = f(np.concatenate([np.asarray(inp['b_grp']), np.asarray(inp['b_rt'])], axis=1))
    sh['ident'] = np.eye(128, dtype=np.float32)
    sh['tri'] = np.triu(np.ones((128, 128), np.float32))
    sh['tau'] = np.ascontiguousarray(np.broadcast_to(np.arange(128, dtype=np.float32), (128, 128)))
    sh['iota256'] = np.ascontiguousarray(np.broadcast_to(np.arange(256, dtype=np.float32), (128, 256)))
    sh['stri'] = np.triu(np.ones((128, 128), np.float32), k=1)
    cm = np.zeros((128, 16, 3), np.float32)
    cm[:, :, 0] = np.arange(16, dtype=np.float32)[None, :]
    cm[:, :, 1] = np.arange(128, dtype=np.float32)[:, None]
    cm[:, :, 2] = 1.0
    sh['cmeta'] = cm
    return sh


_NC_CACHE = {}
PER_LAYER = ['w_in', 'conv_w', 'conv_b', 'w_q', 'w_k', 'bg', 'mh_g', 'lam_re', 'lam_im', 'logdt_x', 'brp', 'bip', 'crp',
             'cip', 'd_skip', 'w_glu', 'b_glu', 's5_g', 'w_out', 'ln1_g', 'ln1_b', 'w_r', 'b_r', 'w_eg', 'w_eu', 'w_ed',
             'ln2_g', 'ln2_b', 'w_pg', 'b_pg', 'w_pp', 'ple_g']


def _launch(nc, sh, xs, pTs, li):
    names = set(nc._declared_inputs)
    base = {}
    for kname in names:
        if kname in ('x', 'pT'):
            continue
        a = sh[kname]
        base[kname] = np.ascontiguousarray(a[li:li + 1]) if kname in PER_LAYER else a
    in_maps = []
    for b in range(8):
        m = dict(base)
        m['x'] = xs[b]
        m['pT'] = pTs[b][li:li + 1]
        in_maps.append(m)
    res = run_bass_kernel_spmd(nc, in_maps, core_ids=list(range(8)))
    return [np.ascontiguousarray(res.results[b]['y']) for b in range(8)]


def kernel(**inputs):
    sh = prep_shared(inputs)
    x = np.asarray(inputs['x'], dtype=np.float32)
    p = np.asarray(inputs['p'], dtype=np.float32)
    if 'full' not in _NC_CACHE:
        _NC_CACHE['full'] = build_program()
    nc = _NC_CACHE['full']
    names = set(nc._declared_inputs)
    base = {kname: sh[kname] for kname in names if kname not in ('x', 'pT')}
    in_maps = []
    for b in range(8):
        m = dict(base)
        m['x'] = np.ascontiguousarray(x[b])
        m['pT'] = np.ascontiguousarray(np.transpose(p[:, b], (0, 2, 1)))
        in_maps.append(m)
    res = run_bass_kernel_spmd(nc, in_maps, core_ids=list(range(8)))
    return np.stack([res.results[b]['y'] for b in range(8)], axis=0).astype(np.float32)
```

```python
import math
import bisect
import os as _os
import numpy as np
import concourse.bass as bass
import concourse.mybir as mybir
from concourse.bass_utils import run_bass_kernel_spmd
from contextlib import ExitStack

F32 = mybir.dt.float32
BF16 = mybir.dt.bfloat16
ALU = mybir.AluOpType
AF = mybir.ActivationFunctionType

D = 1024; S = 2048; NT = 16; DEPTH = 4
D_IN = 2056
ALPHA = (2 * DEPTH) ** 0.25
EPS = 1e-5
PI = math.pi


class T:
    def __init__(s, ap, name):
        s.ap = ap; s.name = name; s.w = []; s.r = []; s.dsem = None; s.dval = 0

    def __getitem__(s, idx):
        return s.ap[idx]


class K:
    def __init__(s, nc, es, needed=None):
        s.nc = nc; s.es = es
        s.dry = needed is None
        s.needed = needed or {}
        s.needed_set = {e: set(v) for e, v in s.needed.items()}
        s.waited = {}
        s.E = {'pe': nc.tensor, 'dve': nc.vector, 'act': nc.scalar, 'pool': nc.gpsimd, 'sp': nc.sync}
        s.sem = {e: es.enter_context(nc.semaphore('sem_' + e)) for e in s.E}
        s.cnt = {e: 0 for e in s.E}
        s.known = {e: {} for e in s.E}
        s.dma_tiles = []
        s.nt = 0

    def tile(s, ap, name=None):
        s.nt += 1
        return T(ap, name or ('t%d' % s.nt))

    def sb(s, name, shape, dt):
        return s.es.enter_context(s.nc.sbuf_tensor(name, shape, dt))

    def _wait(s, eng, ev):
        sem, val, deng, key = ev
        if s.known[eng].get(key, 0) >= val:
            return
        s.known[eng][key] = val
        if deng is not None:
            s.waited.setdefault(deng, set()).add(val)
            if not s.dry:
                rank = bisect.bisect_right(s.needed[deng], val)
                s.E[eng].wait_ge(sem, rank)
        elif not s.dry:
            s.E[eng].wait_ge(sem, val)

    def _deps(s, eng, reads, writes, skipkey=None):
        for t in reads:
            for ev in t.w:
                if ev[2] == eng and eng == 'pe':
                    continue
                s._wait(eng, ev)
        for t in writes:
            for ev in t.w + t.r:
                if ev[2] == eng:
                    continue
                if skipkey is not None and ev[3] == skipkey:
                    continue
                s._wait(eng, ev)

    def op(s, eng, fn, reads=(), writes=()):
        s._deps(eng, reads, writes)
        s.cnt[eng] += 1
        if not s.dry:
            ins = fn(s.E[eng])
            if s.cnt[eng] in s.needed_set.get(eng, ()):
                ins.then_inc(s.sem[eng], 1)
        ev = (s.sem[eng], s.cnt[eng], eng, eng)
        for t in writes:
            t.w = [ev]; t.r = []
        for t in reads:
            if t in writes:
                continue
            t.r = [e for e in t.r if e[3] != eng] + [ev]

    def dma(s, q, out_ap, in_ap, src, dst, parts=False, **kw):
        key = 'd_' + dst.name
        s._deps(q, [src], [dst], skipkey=key if parts else None)
        if dst.dsem is None:
            dst.dsem = True if s.dry else s.es.enter_context(s.nc.semaphore(key))
            s.dma_tiles.append(dst)
        dst.dval += 16
        if not s.dry:
            ins = s.E[q].dma_start(out=out_ap, in_=in_ap, **kw)
            ins.then_inc(dst.dsem, 16)
        ev = (dst.dsem, dst.dval, None, key)
        dst.w = [ev]; dst.r = []
        src.r = [e for e in src.r if e[3] != key] + [ev]

    def dma_scatter(s, out_ap, idx_ap, in_ap, src, idxt, dst, bound):
        key = 'd_' + dst.name
        s._deps('pool', [src, idxt], [dst], skipkey=key)
        if dst.dsem is None:
            dst.dsem = True if s.dry else s.es.enter_context(s.nc.semaphore(key))
            s.dma_tiles.append(dst)
        dst.dval += 16
        ev = (dst.dsem, dst.dval, None, key)
        if not s.dry:
            ins = s.nc.gpsimd.indirect_dma_start(out=out_ap, out_offset=bass.IndirectOffsetOnAxis(ap=idx_ap, axis=0),
                                                 in_=in_ap, in_offset=None, bounds_check=s.bnd_reg, oob_is_err=False)
            ins.then_inc(dst.dsem, 16)
        dst.w = [ev]; dst.r = []
        for t in (src, idxt):
            t.r = [e for e in t.r if e[3] != key] + [ev]

    def barrier(s):
        for e in s.E:
            for e2 in s.E:
                if e2 != e and s.cnt[e2] > 0:
                    s._wait(e, (s.sem[e2], s.cnt[e2], e2, e2))
            for t in s.dma_tiles:
                if t.dval > 0:
                    s._wait(e, (t.dsem, t.dval, None, 'd_' + t.name))

    def mm(s, ps, out_ap, lhsT_ap, rhs_ap, reads, start=True, stop=True):
        s.op('pe', lambda e: e.matmul(out_ap, lhsT=lhsT_ap, rhs=rhs_ap, start=start, stop=stop),
             reads=reads, writes=[ps])

    def tr(s, ps, out_ap, in_ap, ident, reads):
        s.op('pe', lambda e: e.transpose(out_ap, in_ap, ident[:]), reads=list(reads) + [ident], writes=[ps])

    def act(s, out_ap, in_ap, func, reads, writes, bias=None, scale=None, accum_out=None, eng='act'):
        kw = {}
        if bias is not None: kw['bias'] = bias
        if scale is not None: kw['scale'] = scale
        if accum_out is not None: kw['accum_out'] = accum_out
        s.op('act', lambda e: e.activation(out=out_ap, in_=in_ap, func=func, **kw), reads=reads, writes=writes)

    def tt(s, out_ap, in0, in1, op, reads, writes, eng='dve'):
        if eng == 'pool' and _os.environ.get('KPOOL', '0') != '1':
            eng = 'dve'
        if eng == 'POOL':
            eng = 'pool'
        s.op(eng, lambda e: e.tensor_tensor(out=out_ap, in0=in0, in1=in1, op=op), reads=reads, writes=writes)

    def ts(s, out_ap, in0, s1, s2, op0, op1, reads, writes, eng='dve'):
        if eng == 'pool' and _os.environ.get('KPOOL', '0') != '1':
            eng = 'dve'
        if op1 is None:
            s.op(eng, lambda e: e.tensor_scalar(out=out_ap, in0=in0, scalar1=s1, scalar2=None, op0=op0),
                 reads=reads, writes=writes)
        else:
            s.op(eng, lambda e: e.tensor_scalar(out=out_ap, in0=in0, scalar1=s1, scalar2=s2, op0=op0, op1=op1),
                 reads=reads, writes=writes)

    def stt(s, out_ap, in0, scalar, in1, op0, op1, reads, writes):
        s.op('dve', lambda e: e.scalar_tensor_tensor(out=out_ap, in0=in0, scalar=scalar, in1=in1, op0=op0, op1=op1),
             reads=reads, writes=writes)

    def rsqrt_eps(s, out_ap, in_ap, reads, writes, pre_scale=1.0):
        s.act(out_ap, in_ap, AF.Ln, reads, writes, bias=s.eps_ap, scale=pre_scale)
        s.act(out_ap, out_ap, AF.Exp, writes, writes, scale=-0.5)

    def copy(s, eng, out_ap, in_ap, reads, writes):
        if eng == 'act':
            s.op('act', lambda e: e.copy(out=out_ap, in_=in_ap), reads=reads, writes=writes)
        else:
            s.op(eng, lambda e: e.tensor_copy(out=out_ap, in_=in_ap), reads=reads, writes=writes)


PARAM_NAMES = ['w_in', 'conv_w', 'conv_b', 'w_q', 'w_k', 'bg', 'mh_g', 'lam_re', 'lam_im', 'logdt_x',
               'brp', 'bip', 'crp', 'cip', 'd_skip', 'w_glu', 'b_glu', 's5_g', 'w_out', 'ln1_g', 'ln1_b',
               'w_r', 'b_r', 'w_eg', 'w_eu', 'w_ed', 'ln2_g', 'ln2_b', 'w_pg', 'b_pg', 'w_pp', 'ple_g']


def build_program(layers=tuple(range(DEPTH)), phases=('A', 'B', 'C'), n_experts=32, dbg=None, L=DEPTH):
    _, waited = _build(layers, phases, n_experts, L, None)
    needed = {e: sorted(v) for e, v in waited.items()}
    nc, _ = _build(layers, phases, n_experts, L, needed)
    return nc


def _build(layers, phases, n_experts, L, needed):
    nc = bass.Bass("TRN2", target_bir_lowering=False)

    declared = []

    def din(name, shape, dt=F32):
        declared.append(name)
        return nc.dram_tensor(name, shape, dt, kind="ExternalInput").ap()

    x_in = din('x', [S, D])
    pT_in = din('pT', [L, 256, S])
    ident_in = din('ident', [128, 128]); tri_in = din('tri', [128, 128]); tau_in = din('tau', [128, 128])
    iota_in = din('iota256', [128, 256]); stri_in = din('stri', [128, 128]); cmeta_in = din('cmeta', [128, 16, 3])
    w_in = din('w_in', [L, D, D_IN]); conv_w = din('conv_w', [L, 4, 512]); conv_b = din('conv_b', [L, 512])
    w_q = din('w_q', [L, 4, 128, 128]); w_k = din('w_k', [L, 4, 128, 128]); bg_in = din('bg', [L, 8])
    mh_g = din('mh_g', [L, 512])
    lam_re = din('lam_re', [L, 2048]); lam_im = din('lam_im', [L, 2048]); logdt_x = din('logdt_x', [L, 2048])
    brp = din('brp', [L, 128, 2048]); bip = din('bip', [L, 128, 2048])
    crp = din('crp', [L, 128, 16, 128]); cip = din('cip', [L, 128, 16, 128])
    d_skip = din('d_skip', [L, 512]); w_glu = din('w_glu', [L, 512, 512]); b_glu = din('b_glu', [L, 512])
    s5_g = din('s5_g', [L, 512]); w_out = din('w_out', [L, D, D])
    ln1_g = din('ln1_g', [L, D]); ln1_b = din('ln1_b', [L, D])
    w_r = din('w_r', [L, D, 36]); b_r = din('b_r', [L, 36])
    if 'B' in phases:
        w_eg = din('w_eg', [L, 32, D, 512]); w_eu = din('w_eu', [L, 32, D, 512]); w_ed = din('w_ed', [L, 32, 512, D])
    ln2_g = din('ln2_g', [L, D]); ln2_b = din('ln2_b', [L, D])
    w_pg = din('w_pg', [L, D, D]); b_pg = din('b_pg', [L, D]); w_pp = din('w_pp', [L, 256, D]); ple_g = din('ple_g', [L, D])
    y_out = nc.dram_tensor('y', [S, D], F32, kind="ExternalOutput").ap()
    xs_d = nc.dram_tensor('xs_scr', [S, D], F32, kind="Internal").ap()
    cri_d = nc.dram_tensor('cri_scr', [2, 2048], F32, kind="Internal").ap()
    yd_d = nc.dram_tensor('yd_scr', [4096, D], F32, kind="Internal").ap()

    es = ExitStack()
    with es:
        k = K(nc, es, needed)
        k.bnd_reg = None
        if not k.dry:
            k.bnd_reg = nc.gpsimd.alloc_register('bnd')
            nc.gpsimd.reg_mov(k.bnd_reg, 4095)
        DI = k.tile(None, 'dram_in')
        XS = k.tile(xs_d, 'xs'); CRI = k.tile(cri_d, 'cri'); YO = k.tile(y_out, 'yo'); YD = k.tile(yd_d, 'yd')

        arena = k.sb('arena', [128, 32768], BF16)
        Xv = arena[:].bitcast(F32).rearrange("p (n d) -> p n d", n=NT)
        X = [k.tile(Xv[:, i, :], 'X%d' % i) for i in range(NT)]
        xt_t = k.sb('xt', [128, 8, S], BF16)
        XT = [k.tile(xt_t[:, :, n * 512:(n + 1) * 512], 'XT%d' % n) for n in range(4)]
        ust_t = k.sb('ust', [128, 4, S], BF16)
        UST = [k.tile(ust_t[:, :, n * 512:(n + 1) * 512], 'UST%d' % n) for n in range(4)]
        HT = UST
        ew_t = [k.sb('ew%d' % i, [128, 12288], BF16) for i in range(2)]
        EW = [k.tile(ew_t[i][:], 'EW%d' % i) for i in range(2)]
        cw_t = k.sb('cw', [128, 3, 16, 128], BF16); CW = k.tile(cw_t[:], 'CW')
        ktm_t = k.sb('ktm', [128, NT, 128], BF16); KTM = k.tile(ktm_t[:], 'KTM')
        row_t = [k.sb('row%d' % i, [128, D], F32) for i in range(2)]
        ROW = [k.tile(row_t[i][:], 'ROW%d' % i) for i in range(2)]
        xr_t = k.sb('xr', [128, 8, 128], F32); XR = k.tile(xr_t[:], 'XR')
        XLB = [k.tile(xr_t[:].rearrange("p a b -> p (a b)"), 'XLB0'),
               k.tile(ust_t[:, 0, :].bitcast(F32), 'XLB1')]
        ctmp_v = [xr_t[:].rearrange("p a b -> p (a b)"), ust_t[:, 2, :].bitcast(F32), ust_t[:, 3, :].bitcast(F32)]
        CTMP = [k.tile(ctmp_v[a][:, b * 512:(b + 1) * 512], 'CTMP%d' % (a * 2 + b)) for a in range(3) for b in range(2)]
        gw_t = k.sb('gw', [128, NT, 32], F32); GW = [k.tile(gw_t[:, i, :], 'GW%d' % i) for i in range(NT)]
        ident_t = k.sb('ident_s', [128, 128], F32); IDENT = k.tile(ident_t[:], 'IDENT')
        tri_t = k.sb('tri_s', [128, 128], F32); TRI = k.tile(tri_t[:], 'TRI')
        tau_t = k.sb('tau_s', [128, 128], F32); TAU = k.tile(tau_t[:], 'TAU')
        ones_t = k.sb('ones', [128, 128], F32); ONES = k.tile(ones_t[:], 'ONES')
        onesk_t = k.sb('onesk', [128, 128], F32); ONESK = k.tile(onesk_t[:], 'ONESK')
        sm_t = k.sb('small', [128, 512], F32)
        SM = k.tile(sm_t[:], 'SM')
        sm2_t = k.sb('small2', [128, 512], F32)
        smi_t = k.sb('smi', [128, 16], mybir.dt.int32)
        eps_t = k.sb('eps', [128, 1], F32)
        k.op('pool', lambda e: e.memset(eps_t[:], EPS), writes=[])
        k.eps_ap = eps_t[:]
        wr_t = k.sb('wr_s', [128, 8, 36], F32); WR = k.tile(wr_t[:], 'WR')
        wqk_t = k.sb('wqk', [128, 2, 4, 128], BF16); WQK = k.tile(wqk_t[:], 'WQK')
        gat_t = k.sb('gates', [128, NT, 8], F32); GATES = k.tile(gat_t[:], 'GATES')
        gx_t = k.sb('gx', [128, 5, NT, 4], F32); GX = k.tile(gx_t[:], 'GX')
        cst_t = k.sb('cst', [128, 129], F32); CST = k.tile(cst_t[:], 'CST')
        cbf_t = k.sb('cbf', [128, 129], BF16); CBF = k.tile(cbf_t[:], 'CBF')
        stb_t = k.sb('stb', [128, 2, 128], BF16); STB = [k.tile(stb_t[:, i, :], 'STB%d' % i) for i in range(2)]
        wv_t = k.sb('wv', [128, 2, 129], BF16); WV = [k.tile(wv_t[:, i, :], 'WV%d' % i) for i in range(2)]
        hh_t = k.sb('hh', [128, 2, 128], F32); HH = [k.tile(hh_t[:, i, :], 'HH%d' % i) for i in range(2)]
        ms_t = k.sb('ms', [128, 2, 32], F32); MS = [k.tile(ms_t[:, i, :], 'MS%d' % i) for i in range(2)]
        PS = []
        for b in range(8):
            pt = es.enter_context(nc.psum_tensor('ps%d' % b, [128, 512], F32))
            PS.append(k.tile(pt[:], 'PS%d' % b))

        o0 = 0
        vext_v = arena[:, o0:o0 + NT * 4 * 129].rearrange("p (n h c) -> p n h c", n=NT, h=4); o0 += NT * 4 * 129
        sigo_v = arena[:, o0:o0 + NT * 512].rearrange("p (n c) -> p n c", n=NT); o0 += NT * 512
        um_v = arena[:, o0:o0 + 4 * 2051].rearrange("p (h t) -> p h t", h=4); o0 += 4 * 2052
        c_v = arena[:, o0:o0 + S]; o0 += S
        qt_v = arena[:, o0:o0 + S]; o0 += S
        kt_v = arena[:, o0:o0 + S]; o0 += S
        assert o0 <= 32768, o0
        VEXT = [k.tile(vext_v[:, i], 'VEXT%d' % i) for i in range(NT)]
        SIGO = [k.tile(sigo_v[:, i], 'SIGO%d' % i) for i in range(NT)]
        UM = [k.tile(um_v[:, h], 'UM%d' % h) for h in range(4)]
        CC = k.tile(c_v, 'CC'); QT = k.tile(qt_v, 'QT'); KT = k.tile(kt_v, 'KT')
        o1 = 0
        tp_v = arena[:, 0:12 * 1024].bitcast(F32).rearrange("p (n c) -> p n c", n=12); o1 = 12 * 1024
        TP = [k.tile(tp_v[:, i], 'TP%d' % i) for i in range(12)]
        gyb_v = arena[:, o1:o1 + 2048].rearrange("p (n c) -> p n c", n=4); o1 += 2048
        GYB = k.tile(gyb_v, 'GYB')
        rb_v = arena[:, o1:o1 + 2048].rearrange("p (n c) -> p n c", n=4); o1 += 2048
        RB = [k.tile(rb_v[:, i], 'RB%d' % i) for i in range(4)]
        tab_v = arena[:, o1:o1 + 4 * 16 * 128].rearrange("p (a j t) -> p a j t", a=4, j=16); o1 += 4 * 16 * 128
        TAB = k.tile(tab_v, 'TAB')
        bb_v = arena[:, o1:o1 + 2 * 2048].rearrange("p (a c) -> p a c", a=2); o1 += 4096
        BB = k.tile(bb_v, 'BB')
        tp2_v = arena[:, o1:o1 + 4096].bitcast(F32).rearrange("p (n c) -> p n c", n=4); o1 += 4096
        TP2 = [k.tile(tp2_v[:, i], 'TP2_%d' % i) for i in range(4)]
        assert o1 <= 32768, o1

        iota_t = k.sb('iota_s', [128, 256], F32); IOTA = k.tile(iota_t[:], 'IOTA')
        stri_t = k.sb('stri_s', [128, 128], BF16); STRI = k.tile(stri_t[:], 'STRI')
        onesb_t = k.sb('onesb', [128, 128], BF16); ONESB = k.tile(onesb_t[:], 'ONESB')
        cmeta_t = k.sb('cmeta_s', [128, 16, 3], BF16); CMETA = k.tile(cmeta_t[:], 'CMETA')
        slot_t = k.sb('slot', [128, 2, 2, 8], F32); SLOT = [k.tile(slot_t[:, b], 'SLOT%d' % b) for b in range(2)]
        dest_tt = [[k.sb('dest%d%d' % (b, h), [128, 1], mybir.dt.int32) for h in range(2)] for b in range(2)]
        DEST = [[k.tile(dest_tt[b][h][:, :], 'DEST%d%d' % (b, h)) for h in range(2)] for b in range(2)]
        xtm_v = xt_t[:].rearrange("p k t -> p (k t)").rearrange("p (n d) -> p n d", n=NT)
        XTM = [k.tile(xtm_v[:, i, :], 'XTM%d' % i) for i in range(NT)]
        ustf = ust_t[:].rearrange("p k t -> p (k t)")
        xg_v = [ustf[:, b * 2048:(b + 1) * 2048].rearrange("p (k c) -> p k c", k=8) for b in range(2)]
        XG = [k.tile(xg_v[b], 'XG%d' % b) for b in range(2)]
        hte_v = [ustf[:, 4096 + b * 1024:4096 + (b + 1) * 1024].rearrange("p (k c) -> p k c", k=4) for b in range(2)]
        HTE = [k.tile(hte_v[b], 'HTE%d' % b) for b in range(2)]
        sg_v = [ustf[:, 6144 + b * 512:6144 + (b + 1) * 512].bitcast(F32) for b in range(2)]
        SG = [k.tile(sg_v[b], 'SG%d' % b) for b in range(2)]
        cwf = cw_t[:].rearrange("p a j c -> p (a j c)")
        out_v = [cwf[:, b * 2048:(b + 1) * 2048].bitcast(F32) for b in range(2)]
        OUTB = [k.tile(out_v[b], 'OUTB%d' % b) for b in range(2)]
        meta_v = cwf[:, 4096:4096 + 1024].rearrange("p (n e c) -> p n e c", n=NT, e=32)
        META = k.tile(meta_v, 'META')
        mall_v = sm_t[:, 0:256].bitcast(BF16).rearrange("p (n e) -> p n e", n=NT)
        MALL = k.tile(mall_v, 'MALL')
        sel_v = [row_t[b][:].bitcast(BF16).rearrange("p (n c) -> p n c", n=8) for b in range(2)]
        sync = 'sp'
        k.dma(sync, iota_t[:], iota_in, DI, IOTA)
        k.dma('pool', stri_t[:], stri_in, DI, STRI)
        k.dma('pool', cmeta_t[:], cmeta_in, DI, CMETA)
        k.op('pool', lambda e: e.memset(onesb_t[:], 1.0), writes=[ONESB])
        k.dma(sync, ident_t[:], ident_in, DI, IDENT)
        k.dma(sync, tri_t[:], tri_in, DI, TRI)
        k.dma(sync, tau_t[:], tau_in, DI, TAU)
        k.op('pool', lambda e: e.memset(ones_t[:], 1.0), writes=[ONES])
        k.op('pool', lambda e: e.memset(onesk_t[:], 1.0 / 512.0), writes=[ONESK])
        xin_v = x_in.rearrange("(n p) d -> p n d", p=128)
        for i in range(NT):
            k.dma(sync, X[i][:], xin_v[:, i, :], DI, X[i])

        evac_flip = [0]

        def evac(out_ap, in_ap, reads, writes):
            evac_flip[0] ^= 1
            k.copy('act' if evac_flip[0] else 'dve', out_ap, in_ap, reads, writes)

        def build_xT(extra=None):
            for i in range(NT):
                for half in range(2):
                    ps = PS[(2 * i + half) % 4]
                    for q in range(4):
                        kk = half * 4 + q
                        k.tr(ps, ps[:, q * 128:(q + 1) * 128], X[i][:, kk * 128:(kk + 1) * 128], IDENT, [X[i]])
                    psv = ps[:].rearrange("p (q t) -> p q t", q=4)
                    if extra is None:
                        evac(xt_t[:, half * 4:half * 4 + 4, i * 128:(i + 1) * 128], psv, [ps], [XT[i // 4]])
                    else:
                        k.copy('dve', xr_t[:, half * 4:half * 4 + 4, :], psv, [ps], [XR])
                        k.copy('act', xt_t[:, half * 4:half * 4 + 4, i * 128:(i + 1) * 128], xr_t[:, half * 4:half * 4 + 4, :],
                               [XR], [XT[i // 4]])
                if extra is not None:
                    extra(i)

        def load_row(row, vec_ap):
            k.dma(sync, row[:], vec_ap.partition_broadcast(128), DI, row)

        def layer_norm_tile(i, G, Bt):
            st = MS[i % 2]
            xi = X[i]
            k.op('dve', lambda e: e.bn_stats(out=st[:, 0:6], in_=xi[:, 0:512]), reads=[xi], writes=[st])
            k.op('dve', lambda e: e.bn_stats(out=st[:, 6:12], in_=xi[:, 512:1024]), reads=[xi, st], writes=[st])
            k.op('dve', lambda e: e.bn_aggr(out=st[:, 12:14], in_=st[:, 0:12]), reads=[st], writes=[st])
            k.rsqrt_eps(st[:, 14:15], st[:, 13:14], [st], [st])
            k.ts(xi[:], xi[:], st[:, 12:13], st[:, 14:15], ALU.subtract, ALU.mult, [xi, st], [xi])
            k.tt(xi[:], xi[:], G[:], ALU.mult, [xi, G], [xi], eng='pool')
            k.tt(xi[:], xi[:], Bt[:], ALU.add, [xi, Bt], [xi], eng='pool')

        for li in layers:
            if 'A' in phases:
                build_xT()
                for i in range(NT):
                    k.dma(sync, xs_d.rearrange("(n p) d -> p n d", p=128)[:, i, :], X[i][:], X[i], XS, parts=True)
                k.barrier()
                for j in range(4):
                    k.dma(sync, sm_t[:, j * 4:j * 4 + 4], conv_w[li][j].rearrange("(h p) -> p h", p=128), DI, SM,
                          parts=(j > 0), allow_slow_non_contiguous=True)
                for (c0, src) in ((16, conv_b), (20, mh_g), (24, d_skip), (28, b_glu), (32, s5_g)):
                    k.dma(sync, sm_t[:, c0:c0 + 4], src[li].rearrange("(h p) -> p h", p=128), DI, SM, parts=True,
                          allow_slow_non_contiguous=True)
                k.dma(sync, sm_t[:, 40:48], bg_in[li].partition_broadcast(128), DI, SM, parts=True)
                k.dma('pool', wqk_t[:, 0], w_q[li].rearrange("h d e -> d h e"), DI, WQK)
                k.dma('pool', wqk_t[:, 1], w_k[li].rearrange("h d e -> d h e"), DI, WQK, parts=True)

                def load_w(buf, col0, ncols):
                    k.dma('pool', ew_t[buf][:, 0:8 * ncols].rearrange("p (k c) -> p k c", k=8),
                          w_in[li][:, col0:col0 + ncols].rearrange("(k p) c -> p k c", p=128), DI, EW[buf],
                          allow_slow_non_contiguous=(ncols < 128))
                    return ew_t[buf][:, 0:8 * ncols].rearrange("p (k c) -> p k c", k=8)

                def fm_piece(buf, col0, dest_fn, dest_tiles):
                    W = load_w(buf, col0, 512)
                    for m in range(4):
                        for n in range(4):
                            ps = PS[(m * 4 + n) % 4]
                            for kk in range(8):
                                k.mm(ps, ps[:], W[:, kk, m * 128:(m + 1) * 128], xt_t[:, kk, n * 512:(n + 1) * 512],
                                     [EW[buf], XT[n]], start=(kk == 0), stop=(kk == 7))
                            evac(dest_fn(m, n), ps[:], [ps], [dest_tiles(m, n)])

                fm_piece(0, 1544, lambda m, n: ust_t[:, m, n * 512:(n + 1) * 512], lambda m, n: UST[n])
                for h in range(4):
                    k.op('pool', lambda e, h=h: e.memset(um_v[:, h, 0:3], 0.0), writes=[UM[h]])
                fm_piece(1, 0, lambda m, n: um_v[:, m, 3 + n * 512:3 + (n + 1) * 512], lambda m, n: UM[m])
                W = load_w(0, 512, 512)
                for i in range(NT):
                    ps = PS[i % 4]
                    for kk in range(8):
                        k.mm(ps, ps[:], xt_t[:, kk, i * 128:(i + 1) * 128], W[:, kk, :], [EW[0], XT[i // 4]],
                             start=(kk == 0), stop=(kk == 7))
                    k.op('pool', lambda e, i=i: e.memset(vext_v[:, i, :, 128:129], 1.0), writes=[VEXT[i]])
                    evac(vext_v[:, i, :, 0:128], ps[:].rearrange("p (h c) -> p h c", h=4), [ps], [VEXT[i]])
                W = load_w(1, 1024, 512)
                for i in range(NT):
                    ps = PS[i % 4]
                    for kk in range(8):
                        k.mm(ps, ps[:], xt_t[:, kk, i * 128:(i + 1) * 128], W[:, kk, :], [EW[1], XT[i // 4]],
                             start=(kk == 0), stop=(kk == 7))
                    k.act(sigo_v[:, i, :], ps[:], AF.Sigmoid, [ps], [SIGO[i]])
                W = load_w(0, 1536, 8)
                for i in range(NT):
                    ps = PS[i % 4]
                    for kk in range(8):
                        k.mm(ps, ps[:, 0:8], xt_t[:, kk, i * 128:(i + 1) * 128], W[:, kk, :], [EW[0], XT[i // 4]],
                             start=(kk == 0), stop=(kk == 7))
                    k.tt(gat_t[:, i, :], ps[:, 0:8], sm_t[:, 40:48], ALU.add, [ps, SM], [GATES])

                LF = gx_t[:, 0]; BC = gx_t[:, 1]; GG = gx_t[:, 2]; AA = gx_t[:, 3]; EE = gx_t[:, 4]
                k.act(LF, gat_t[:, :, 4:8], AF.Sigmoid, [GATES], [GX])
                k.act(LF, LF, AF.Ln, [GX], [GX])
                ps = PS[4]
                for i in range(NT):
                    k.mm(ps, ps[:, i * 8:i * 8 + 4], tri_t[:], gx_t[:, 0, i, :], [TRI, GX], start=True, stop=True)
                    k.mm(ps, ps[:, i * 8 + 4:i * 8 + 8], ones_t[:], gx_t[:, 0, i, :], [ONES, GX], start=True, stop=True)
                psv = ps[:, 0:128].rearrange("p (n c) -> p n c", n=NT)
                k.copy('dve', BC, psv[:, :, 0:4], [ps], [GX])
                k.act(GG, psv[:, :, 4:8], AF.Exp, [ps], [GX])
                k.tt(AA, gat_t[:, :, 0:4], BC, ALU.subtract, [GATES, GX], [GX])
                k.act(AA, AA, AF.Exp, [GX], [GX])
                k.act(EE, BC, AF.Exp, [GX], [GX])

                for h in range(4):
                    for n in range(4):
                        acc = ROW[n % 2]
                        k.ts(acc[:, 0:512], um_v[:, h, n * 512:n * 512 + 512], sm_t[:, h:h + 1], None,
                             ALU.mult, None, [UM[h], SM], [acc])
                        for j in range(1, 4):
                            k.stt(acc[:, 0:512], um_v[:, h, n * 512 + j:n * 512 + j + 512],
                                  sm_t[:, j * 4 + h:j * 4 + h + 1], acc[:, 0:512], ALU.mult, ALU.add,
                                  [UM[h], SM, acc], [acc])
                        k.act(c_v[:, n * 512:(n + 1) * 512], acc[:, 0:512], AF.Silu, [acc, SM], [CC],
                              bias=sm_t[:, 16 + h:17 + h])
                    for n in range(4):
                        ps = PS[n % 4]
                        k.mm(ps, ps[:], wqk_t[:, 0, h, :], c_v[:, n * 512:(n + 1) * 512], [WQK, CC])
                        evac(qt_v[:, n * 512:(n + 1) * 512], ps[:], [ps], [QT])
                        ps = PS[(n + 2) % 4]
                        k.mm(ps, ps[:], wqk_t[:, 1, h, :], c_v[:, n * 512:(n + 1) * 512], [WQK, CC])
                        k.act(kt_v[:, n * 512:(n + 1) * 512], ps[:], AF.Copy, [ps], [KT], scale=128.0 ** -0.5)
                    for i4 in range(4):
                        ps = PS[i4 % 4]
                        for q in range(4):
                            i = i4 * 4 + q
                            k.mm(ps, ps[:, q * 128:(q + 1) * 128], c_v[:, i * 128:(i + 1) * 128], wqk_t[:, 1, h, :],
                                 [CC, WQK])
                        k.act(ktm_t[:, i4 * 4:i4 * 4 + 4, :], ps[:].rearrange("p (q c) -> p q c", q=4), AF.Copy,
                              [ps], [KTM], scale=128.0 ** -0.5)
                    k.op('pool', lambda e: e.memset(cst_t[:], 0.0), writes=[CST])
                    k.op('pool', lambda e: e.memset(cbf_t[:], 0.0), writes=[CBF])
                    def stage1(i, h=h):
                        sl = slice(i * 128, (i + 1) * 128)
                        b2 = i % 2
                        psS = PS[4 + b2]; psN = PS[6 + b2]; psC = PS[b2]
                        st = STB[b2]; wv = WV[b2]
                        k.mm(psS, psS[:, 0:128], kt_v[:, sl], qt_v[:, sl], [KT, QT])
                        k.tt(st[:], psS[:, 0:128], tri_t[:], ALU.mult, [psS, TRI], [st])
                        k.act(wv[:], vext_v[:, i, h, :], AF.Copy, [VEXT[i], GX], [wv], scale=gx_t[:, 3, i, h:h + 1])
                        k.mm(psN, psN[:, 0:129], st[:], wv[:], [st, wv], start=True, stop=False)
                        k.mm(psC, psC[:, 0:129], ktm_t[:, i, :], wv[:], [KTM, wv])

                    def stage2(i, h=h):
                        sl = slice(i * 128, (i + 1) * 128)
                        b2 = i % 2
                        psN = PS[6 + b2]; psC = PS[b2]; psT = PS[2 + b2]
                        hh = HH[b2]; ms = MS[b2]
                        k.mm(psN, psN[:, 0:129], qt_v[:, sl], cbf_t[:], [QT, CBF], start=False, stop=True)
                        k.tt(cst_t[:], psC[:, 0:129], cst_t[:], ALU.add, [psC, CST], [CST])
                        k.ts(cst_t[:], cst_t[:], gx_t[:, 2, i, h:h + 1], None, ALU.mult, None, [CST, GX], [CST])
                        k.copy('act', cbf_t[:], cst_t[:], [CST], [CBF])
                        k.ts(ms[:, 3:4], psN[:, 128:129], gx_t[:, 4, i, h:h + 1], None, ALU.mult, None, [psN, GX], [ms])
                        k.stt(ms[:, 0:1], ms[:, 3:4], -1.0, ms[:, 3:4], ALU.mult, ALU.max, [ms], [ms])
                        k.ts(ms[:, 0:1], ms[:, 0:1], 1.0, None, ALU.max, None, [ms], [ms])
                        k.op('dve', lambda e, ms=ms: e.reciprocal(out=ms[:, 1:2], in_=ms[:, 0:1]), reads=[ms], writes=[ms])
                        k.tt(ms[:, 2:3], ms[:, 1:2], gx_t[:, 4, i, h:h + 1], ALU.mult, [ms, GX], [ms])
                        k.stt(hh[:], psN[:, 0:128], ms[:, 2:3], sigo_v[:, i, h * 128:(h + 1) * 128], ALU.mult, ALU.mult,
                              [psN, ms, SIGO[i]], [hh])
                        k.op('dve', lambda e, ms=ms, hh=hh: e.bn_stats(out=ms[:, 4:10], in_=hh[:]), reads=[hh, ms], writes=[ms])
                        k.op('dve', lambda e, ms=ms: e.bn_aggr(out=ms[:, 10:12], in_=ms[:, 4:10]), reads=[ms], writes=[ms])
                        k.rsqrt_eps(ms[:, 12:13], ms[:, 11:12], [ms], [ms])

                    def stage3(i, h=h):
                        sl = slice(i * 128, (i + 1) * 128)
                        b2 = i % 2
                        psT = PS[2 + b2]; hh = HH[b2]; ms = MS[b2]
                        k.ts(hh[:], hh[:], ms[:, 10:11], ms[:, 12:13], ALU.subtract, ALU.mult, [hh, ms], [hh])
                        k.tr(psT, psT[:, 0:128], hh[:], IDENT, [hh])
                        k.act(xt_t[:, h, sl], psT[:, 0:128], AF.Copy, [psT, SM], [XT[i // 4]], scale=sm_t[:, 20 + h:21 + h])

                    stage1(0)
                    for i in range(NT):
                        if i + 1 < NT:
                            stage1(i + 1)
                        stage2(i)
                        if i >= 1:
                            stage3(i - 1)
                    stage3(NT - 1)
                k.barrier()

                for (c0, src) in ((64, lam_re), (80, lam_im), (96, logdt_x)):
                    k.dma(sync, sm_t[:, c0:c0 + 16], src[li].rearrange("(j q) -> q j", q=128), DI, SM, parts=True,
                          allow_slow_non_contiguous=True)
                LR = sm_t[:, 64:80]; LI = sm_t[:, 80:96]; LDT = sm_t[:, 96:112]
                c = lambda a: sm_t[:, a:a + 16]
                DT = c(112); LRDT = c(128); LIDT = c(144); ER = c(160); CO = c(176); SI = c(192); AR = c(208); AI = c(224)
                MAG = c(240); XRr = c(256); T1 = c(272); T2 = c(288); CR = c(304); CI = c(320)
                VC128 = c(336); VS128 = c(352); ZR = c(368); ZI = c(384); ZT = c(400); ZT2 = c(416); T3 = c(432)
                smo = lambda o, a, f, **kw: k.act(o, a, f, [SM], [SM], **kw)
                smt = lambda o, a, b, op: k.tt(o, a, b, op, [SM], [SM])
                sms = lambda o, a, s1, s2, op0, op1: k.ts(o, a, s1, s2, op0, op1, [SM], [SM])
                smo(DT, LDT, AF.Exp)
                smt(LRDT, LR, DT, ALU.mult); smt(LIDT, LI, DT, ALU.mult)
                smo(ER, LRDT, AF.Exp)

                def sincos(o_sin, o_cos, ang, tmp):
                    sms(smi_t[:], ang, 1.0 / (2 * PI), None, ALU.mult, None)
                    k.stt(tmp, smi_t[:], -2 * PI, ang, ALU.mult, ALU.add, [SM], [SM])
                    smo(o_sin, tmp, AF.Sin, scale=0.999999)
                    sms(o_cos, ang, 0.5 * PI, None, ALU.add, None)
                    sms(smi_t[:], o_cos, 1.0 / (2 * PI), None, ALU.mult, None)
                    k.stt(tmp, smi_t[:], -2 * PI, o_cos, ALU.mult, ALU.add, [SM], [SM])
                    smo(o_cos, tmp, AF.Sin, scale=0.999999)
                sincos(SI, CO, LIDT, T1)
                smt(AR, ER, CO, ALU.mult); smt(AI, ER, SI, ALU.mult)
                smt(MAG, LR, LR, ALU.mult); smt(T1, LI, LI, ALU.mult); smt(MAG, MAG, T1, ALU.add)
                k.op('dve', lambda e: e.reciprocal(out=MAG, in_=MAG), reads=[SM], writes=[SM])
                sms(XRr, AR, -1.0, None, ALU.add, None)
                smt(T1, XRr, LR, ALU.mult); smt(T2, AI, LI, ALU.mult); smt(T1, T1, T2, ALU.add); smt(CR, T1, MAG, ALU.mult)
                smt(T1, AI, LR, ALU.mult); smt(T2, XRr, LI, ALU.mult); smt(T1, T1, T2, ALU.subtract); smt(CI, T1, MAG, ALU.mult)
                sms(T3, LIDT, 128.0, None, ALU.mult, None)
                sincos(VS128, VC128, T3, T1)
                smo(T2, LRDT, AF.Exp, scale=128.0)
                smt(VC128, VC128, T2, ALU.mult); smt(VS128, VS128, T2, ALU.mult)
                k.dma(sync, cri_d[0].rearrange("(j q) -> q j", q=128), CR, SM, CRI, allow_slow_non_contiguous=True)
                k.dma(sync, cri_d[1].rearrange("(j q) -> q j", q=128), CI, SM, CRI, parts=True, allow_slow_non_contiguous=True)
                for ct in range(4):
                    cs = slice(ct * 512, (ct + 1) * 512)
                    k.dma(sync, TP[0][:], brp[li][:, cs], DI, TP[0])
                    k.dma(sync, TP[1][:], bip[li][:, cs], DI, TP[1])
                    k.dma(sync, TP[2][:], cri_d[0, cs].partition_broadcast(128), CRI, TP[2])
                    k.dma(sync, TP[3][:], cri_d[1, cs].partition_broadcast(128), CRI, TP[3])
                    k.tt(TP[4][:], TP[2][:], TP[0][:], ALU.mult, [TP[2], TP[0]], [TP[4]])
                    k.tt(TP[5][:], TP[3][:], TP[1][:], ALU.mult, [TP[3], TP[1]], [TP[5]])
                    k.tt(bb_v[:, 0, cs], TP[4][:], TP[5][:], ALU.subtract, [TP[4], TP[5]], [BB])
                    k.tt(TP[4][:], TP[2][:], TP[1][:], ALU.mult, [TP[2], TP[1]], [TP[4]])
                    k.tt(TP[5][:], TP[3][:], TP[0][:], ALU.mult, [TP[3], TP[0]], [TP[5]])
                    k.tt(bb_v[:, 1, cs], TP[4][:], TP[5][:], ALU.add, [TP[4], TP[5]], [BB])
                for jb in range(4):
                    jq = slice(jb * 4, jb * 4 + 4)
                    t1_ = TP[6 + jb % 2]; t2_ = TP[8 + jb % 2]
                    v1_ = t1_[:].rearrange("p (j c) -> p j c", j=4); v2_ = t2_[:].rearrange("p (j c) -> p j c", j=4)
                    k.dma(sync, v1_, crp[li][:, jq, :], DI, t1_)
                    k.dma(sync, v2_, cip[li][:, jq, :], DI, t2_)
                    k.copy('act', cw_t[:, 0, jq, :], v1_, [t1_], [CW])
                    k.act(cw_t[:, 1, jq, :], v1_, AF.Copy, [t1_], [CW], scale=-1.0)
                    k.act(cw_t[:, 2, jq, :], v2_, AF.Copy, [t2_], [CW], scale=-1.0)
                for jb in range(4):
                    jsl = slice(jb * 4, jb * 4 + 4)
                    v3 = lambda t: t[:].rearrange("p (j c) -> p j c", j=4)
                    taub = tau_t[:].unsqueeze(1).broadcast_to([128, 4, 128])
                    lidb = sm_t[:, 144 + jb * 4:144 + jb * 4 + 4].unsqueeze(2).broadcast_to([128, 4, 128])
                    lrdb = sm_t[:, 128 + jb * 4:128 + jb * 4 + 4].unsqueeze(2).broadcast_to([128, 4, 128])
                    ANG, TMPa, SINT, COST, LTt, MAGP, MAGN = TP[0], TP[1], TP[2], TP[3], TP[4], TP[5], TP[6]
                    k.tt(v3(ANG), taub, lidb, ALU.mult, [TAU, SM], [ANG])
                    KI = TP[7]; kiv = KI[:].bitcast(mybir.dt.int32)
                    k.ts(kiv, ANG[:], 1.0 / (2 * PI), None, ALU.mult, None, [ANG], [KI])
                    k.stt(TMPa[:], kiv, -2 * PI, ANG[:], ALU.mult, ALU.add, [KI, ANG], [TMPa])
                    k.act(SINT[:], TMPa[:], AF.Sin, [TMPa], [SINT], scale=0.999999)
                    k.ts(ANG[:], ANG[:], 0.5 * PI, None, ALU.add, None, [ANG], [ANG])
                    k.ts(kiv, ANG[:], 1.0 / (2 * PI), None, ALU.mult, None, [ANG], [KI])
                    k.stt(TMPa[:], kiv, -2 * PI, ANG[:], ALU.mult, ALU.add, [KI, ANG], [TMPa])
                    k.act(COST[:], TMPa[:], AF.Sin, [TMPa], [COST], scale=0.999999)
                    k.tt(v3(LTt), taub, lrdb, ALU.mult, [TAU, SM], [LTt])
                    k.act(MAGP[:], LTt[:], AF.Exp, [LTt], [MAGP])
                    k.act(MAGN[:], LTt[:], AF.Exp, [LTt], [MAGN], scale=-1.0)
                    k.tt(tab_v[:, 0, jsl, :], v3(MAGN), v3(COST), ALU.mult, [MAGN, COST], [TAB])
                    k.tt(tab_v[:, 1, jsl, :], v3(MAGN), v3(SINT), ALU.mult, [MAGN, SINT], [TAB])
                    k.tt(tab_v[:, 2, jsl, :], v3(MAGP), v3(COST), ALU.mult, [MAGP, COST], [TAB])
                    k.tt(tab_v[:, 3, jsl, :], v3(MAGP), v3(SINT), ALU.mult, [MAGP, SINT], [TAB])
                k.dma('pool', ew_t[0][:, 0:2048].rearrange("p (k c) -> p k c", k=4),
                      w_glu[li].rearrange("(k p) c -> p k c", p=128), DI, EW[0])
                WGLU = ew_t[0][:, 0:2048].rearrange("p (k c) -> p k c", k=4)
                k.op('pool', lambda e: e.memset(sm_t[:, 368:400], 0.0), reads=[SM], writes=[SM])
                P1, P2, P3, P4, SR, SIi = TP[0], TP[1], TP[2], TP[3], TP[4], TP[5]
                GYF = [TP[6], TP[7], TP[10], TP[11]]
                YF = TP2[0]; TQ = TP2[1]
                b4 = lambda ap: ap.unsqueeze(1).broadcast_to([128, 4, 128])
                v4 = lambda t: t[:].rearrange("p (c t) -> p c t", c=4)
                for n in range(4):
                    ns = slice(n * 512, (n + 1) * 512)
                    for ct in range(4):
                        psY = PS[4 + ct % 2]
                        for g2 in range(2):
                            grp = []
                            for q in range(2):
                                jj = 2 * g2 + q
                                j = ct * 4 + jj
                                psR = PS[q]; psI = PS[2 + q]
                                Pq = TP[0:4] if q == 0 else TP2
                                Sq = (TP[4], TP[8]) if q == 0 else (TP[5], TP[9])
                                k.mm(psR, psR[:], bb_v[:, 0, j * 128:(j + 1) * 128], ust_t[:, ct, ns], [BB, UST[n]])
                                k.mm(psI, psI[:], bb_v[:, 1, j * 128:(j + 1) * 128], ust_t[:, ct, ns], [BB, UST[n]])
                                pr = psR[:].rearrange("p (c t) -> p c t", c=4); pi_ = psI[:].rearrange("p (c t) -> p c t", c=4)
                                k.tt(v4(Pq[0]), pr, b4(tab_v[:, 0, j, :]), ALU.mult, [psR, TAB], [Pq[0]])
                                k.tt(v4(Pq[1]), pi_, b4(tab_v[:, 1, j, :]), ALU.mult, [psI, TAB], [Pq[1]])
                                k.tt(v4(Pq[2]), pi_, b4(tab_v[:, 0, j, :]), ALU.mult, [psI, TAB], [Pq[2]])
                                k.tt(v4(Pq[3]), pr, b4(tab_v[:, 1, j, :]), ALU.mult, [psR, TAB], [Pq[3]])
                                grp.append((jj, j, Pq, Sq))
                            for cc in range(4):
                                cs = slice(cc * 128, (cc + 1) * 128)
                                last = cc * 128 + 127
                                for (jj, j, Pq, Sq) in grp:
                                    k.op('dve', lambda e, cs=cs, j=j, Pq=Pq, Sq=Sq: e.tensor_tensor_scan(
                                        out=Sq[0][:, cs], data0=Pq[0][:, cs], data1=Pq[1][:, cs], initial=sm_t[:, 368 + j:369 + j],
                                        op0=ALU.add, op1=ALU.add), reads=[Pq[0], Pq[1], SM], writes=[Sq[0]])
                                    k.op('dve', lambda e, cs=cs, j=j, Pq=Pq, Sq=Sq: e.tensor_tensor_scan(
                                        out=Sq[1][:, cs], data0=Pq[2][:, cs], data1=Pq[3][:, cs], initial=sm_t[:, 384 + j:385 + j],
                                        op0=ALU.add, op1=ALU.subtract), reads=[Pq[2], Pq[3], SM], writes=[Sq[1]])
                                j0 = grp[0][1]
                                s4 = tp_v[:, 4:12, :].rearrange("p (a c) t -> p a c t", c=4)[:, :, 0:2, last]
                                Sall = [grp[0][3][0], grp[0][3][1], grp[1][3][0], grp[1][3][1]]
                                vc2 = sm_t[:, 336 + j0:338 + j0].unsqueeze(1).broadcast_to([128, 2, 2])
                                vs2 = sm_t[:, 352 + j0:354 + j0].unsqueeze(1).broadcast_to([128, 2, 2])
                                pz = sm_t[:, 400:404].rearrange("p (a c) -> p a c", a=2)
                                qz = sm_t[:, 416:420].rearrange("p (a c) -> p a c", a=2)
                                k.tt(pz, s4, vc2, ALU.mult, Sall + [SM], [SM])
                                k.tt(qz, s4, vs2, ALU.mult, Sall + [SM], [SM])
                                k.tt(sm_t[:, 368 + j0:370 + j0], sm_t[:, 400:402], sm_t[:, 418:420], ALU.subtract, [SM], [SM])
                                k.tt(sm_t[:, 384 + j0:386 + j0], sm_t[:, 402:404], sm_t[:, 416:418], ALU.add, [SM], [SM])
                            for (jj, j, Pq, Sq) in grp:
                                SRq, SIq = Sq
                                k.tt(v4(RB[0]), v4(SRq), b4(tab_v[:, 2, j, :]), ALU.mult, [SRq, TAB], [RB[0]], eng='pool')
                                k.tt(v4(RB[1]), v4(SIq), b4(tab_v[:, 3, j, :]), ALU.mult, [SIq, TAB], [RB[1]], eng='pool')
                                k.tt(v4(RB[2]), v4(SIq), b4(tab_v[:, 2, j, :]), ALU.mult, [SIq, TAB], [RB[2]], eng='pool')
                                k.tt(v4(RB[3]), v4(SRq), b4(tab_v[:, 3, j, :]), ALU.mult, [SRq, TAB], [RB[3]], eng='pool')
                                for r, wsel in enumerate((0, 1, 2, 2)):
                                    k.mm(psY, psY[:], cw_t[:, wsel, j, :], RB[r][:], [CW, RB[r]],
                                         start=(jj == 0 and r == 0), stop=(jj == 3 and r == 3))
                        k.stt(YF[:], ust_t[:, ct, ns], sm_t[:, 24 + ct:25 + ct], psY[:], ALU.mult, ALU.add,
                              [UST[n], SM, psY], [YF])
                        k.tt(TQ[:], YF[:], YF[:], ALU.mult, [YF], [TQ], eng='pool')
                        k.ts(TQ[:], TQ[:], 0.044715, 1.0, ALU.mult, ALU.add, [TQ], [TQ], eng='pool')
                        k.tt(TQ[:], TQ[:], YF[:], ALU.mult, [TQ, YF], [TQ], eng='pool')
                        k.act(TQ[:], TQ[:], AF.Sigmoid, [TQ], [TQ], scale=2.0 * math.sqrt(2.0 / PI))
                        k.tt(GYF[ct][:], YF[:], TQ[:], ALU.mult, [YF, TQ], [GYF[ct]], eng='pool')
                        k.copy('act', gyb_v[:, ct, :], GYF[ct][:], [GYF[ct]], [GYB])
                    for cto in range(4):
                        ps = PS[6 + cto % 2]
                        for ci in range(4):
                            k.mm(ps, ps[:], WGLU[:, ci, cto * 128:(cto + 1) * 128], gyb_v[:, ci, :], [EW[0], GYB],
                                 start=(ci == 0), stop=(ci == 3))
                        k.act(TQ[:], ps[:], AF.Sigmoid, [ps, SM], [TQ], bias=sm_t[:, 28 + cto:29 + cto])
                        k.tt(GYF[cto][:], GYF[cto][:], TQ[:], ALU.mult, [GYF[cto], TQ], [GYF[cto]])
                        k.act(TP[cto][:], GYF[cto][:], AF.Square, [GYF[cto]], [TP[cto]])
                    ps = PS[6]
                    for ct in range(4):
                        k.mm(ps, ps[:], onesk_t[:], TP[ct][:], [ONESK, TP[ct]], start=(ct == 0), stop=(ct == 3))
                    k.rsqrt_eps(YF[:], ps[:], [ps], [YF])
                    for ct in range(4):
                        k.stt(xt_t[:, 4 + ct, ns], GYF[ct][:], sm_t[:, 32 + ct:33 + ct], YF[:], ALU.mult, ALU.mult,
                              [GYF[ct], SM, YF], [XT[n]])
                k.barrier()

                load_row(ROW[0], ln1_g[li]); load_row(ROW[1], ln1_b[li])
                for hf in range(2):
                    k.dma('pool', ew_t[hf][:, 0:4096].rearrange("p (k c) -> p k c", k=8),
                          w_out[li][:, hf * 512:(hf + 1) * 512].rearrange("(k p) c -> p k c", p=128), DI, EW[hf])
                for i in range(NT):
                    xlt = XLB[1]
                    xsv = xs_d.rearrange("(n p) d -> p n d", p=128)
                    k.dma(sync, xlt[:], xsv[:, i, :], XS, xlt)
                    for hf in range(2):
                        ps = PS[(2 * i + hf) % 4]
                        W = ew_t[hf][:, 0:4096].rearrange("p (k c) -> p k c", k=8)
                        for kk in range(8):
                            k.mm(ps, ps[:], xt_t[:, kk, i * 128:(i + 1) * 128], W[:, kk, :], [XT[i // 4], EW[hf]],
                                 start=(kk == 0), stop=(kk == 7))
                        k.stt(X[i][:, hf * 512:(hf + 1) * 512], xlt[:, hf * 512:(hf + 1) * 512], ALPHA, ps[:], ALU.mult, ALU.add, [xlt, ps], [X[i]])
                    layer_norm_tile(i, ROW[0], ROW[1])
                k.barrier()

            if 'B' in phases:
                k.dma(sync, wr_t[:], w_r[li].rearrange("(k p) c -> p k c", p=128), DI, WR)
                k.dma(sync, gx_t[:].rearrange("p a n c -> p (a n c)")[:, 0:36], b_r[li].partition_broadcast(128), DI, GX)
                BIASR = gx_t[:].rearrange("p a n c -> p (a n c)")[:, 0:36]
                POS = k.tile(gw_t[:], 'POS')
                lgg_v = hh_t[:, 0, 0:64].rearrange("p (n g) -> p n g", n=NT)
                ej_v = hh_t[:, 0, 64:128].rearrange("p (n g) -> p n g", n=NT)
                oh_v = hh_t[:, 1, 0:64].rearrange("p (n g) -> p n g", n=NT)
                mk_v = hh_t[:, 1, 64:128].rearrange("p (n g) -> p n g", n=NT)
                vec = lambda a: sm_t[:, 256 + a * 16:256 + (a + 1) * 16]
                GMX, SEv, Dv, W1v, W2v, C1v, C2v = [vec(a) for a in range(7)]
                em_v = sm2_t[:].rearrange("p (n e) -> p n e", n=NT)
                m8_v = gat_t

                def routing(i):
                    ps = PS[4 + i % 2]
                    for kk in range(8):
                        k.mm(ps, ps[:, 0:36], xr_t[:, kk, :], wr_t[:, kk, :], [XR, WR], start=(kk == 0), stop=(kk == 7))
                    k.tt(lgg_v[:, i, :], ps[:, 0:4], BIASR[:, 0:4], ALU.add, [ps, GX], [HH[0]])
                    k.tt(gw_t[:, i, :], ps[:, 4:36], BIASR[:, 4:36], ALU.add, [ps, GX], [POS])

                def routing_batched():
                    b3 = lambda ap, n: ap.unsqueeze(2).broadcast_to([128, NT, n])
                    k.op('dve', lambda e: e.tensor_reduce(out=GMX, in_=lgg_v, axis=mybir.AxisListType.X, op=ALU.max),
                         reads=[HH[0]], writes=[SM])
                    k.tt(lgg_v, lgg_v, b3(GMX, 4), ALU.subtract, [HH[0], SM], [HH[0]])
                    k.act(ej_v, lgg_v, AF.Exp, [HH[0]], [HH[0]])
                    k.op('dve', lambda e: e.tensor_reduce(out=SEv, in_=ej_v, axis=mybir.AxisListType.X, op=ALU.add),
                         reads=[HH[0]], writes=[SM])
                    k.op('dve', lambda e: e.reciprocal(out=SEv, in_=SEv), reads=[SM], writes=[SM])
                    k.ts(oh_v, lgg_v, 0.0, None, ALU.is_equal, None, [HH[0]], [HH[1]])
                    k.ts(mk_v, oh_v, -1.0, 1e30, ALU.add, ALU.mult, [HH[1]], [HH[1]])
                    k.tt(sm2_t[:].rearrange("p (n g e) -> p n g e", n=NT, g=4),
                         gw_t[:].rearrange("p n (g e) -> p n g e", g=4),
                         mk_v.unsqueeze(3).broadcast_to([128, NT, 4, 8]), ALU.add, [POS, HH[1]], [SM])
                    for i in range(NT):
                        k.op('dve', lambda e, i=i: e.max(out=m8_v[:, i, :], in_=em_v[:, i, :]), reads=[SM], writes=[GATES])
                    V1 = m8_v[:, :, 0]; V2 = m8_v[:, :, 1]
                    k.tt(Dv, V1, V2, ALU.subtract, [GATES], [SM])
                    k.act(W1v, Dv, AF.Sigmoid, [SM], [SM])
                    k.ts(W2v, W1v, -1.0, 1.0, ALU.mult, ALU.add, [SM], [SM])
                    k.tt(C1v, W1v, SEv, ALU.mult, [SM], [SM]); k.tt(C2v, W2v, SEv, ALU.mult, [SM], [SM])
                    k.tt(gw_t[:], em_v, b3(V1, 32), ALU.is_equal, [SM, GATES], [POS])
                    k.tt(gw_t[:], gw_t[:], b3(C1v, 32), ALU.mult, [POS, SM], [POS])
                    k.tt(meta_v[:, :, :, 0], em_v, b3(V2, 32), ALU.is_equal, [SM, GATES], [META])
                    k.tt(em_v, meta_v[:, :, :, 0], b3(C2v, 32), ALU.mult, [META, SM], [SM])
                    k.tt(gw_t[:], gw_t[:], em_v, ALU.add, [POS, SM], [POS])
                    k.copy('dve', meta_v[:, :, :, 1], gw_t[:], [POS], [META])
                    k.ts(mall_v, gw_t[:], 0.0, None, ALU.is_gt, None, [POS], [MALL])

                k.op('pool', lambda e: e.memset(out_v[0], 0.0), writes=[OUTB[0]])
                ydv = yd_d.rearrange("(n p) d -> p n d", p=128)
                for r in range(32):
                    k.dma(sync, ydv[:, r, :], out_v[0], OUTB[0], YD, parts=True)
                k._wait('pool', (YD.dsem, YD.dval, None, 'd_yd'))
                for i in range(NT):
                    for half in range(2):
                        ps = PS[(2 * i + half) % 4]
                        for q in range(4):
                            kk = half * 4 + q
                            k.tr(ps, ps[:, q * 128:(q + 1) * 128], X[i][:, kk * 128:(kk + 1) * 128], IDENT, [X[i]])
                        k.copy('dve', xr_t[:, half * 4:half * 4 + 4, :], ps[:].rearrange("p (q t) -> p q t", q=4), [ps], [XR])
                    k.copy('act', xtm_v[:, i, :], X[i][:], [X[i]], [XTM[i]])
                    routing(i)
                routing_batched()
                for i in range(NT):
                    ps = PS[4 + i % 2]
                    k.mm(ps, ps[:, 0:32], stri_t[:], mall_v[:, i, :], [STRI, MALL], start=True, stop=(i == 0))
                    for j in range(i):
                        k.mm(ps, ps[:, 0:32], onesb_t[:], mall_v[:, j, :], [ONESB, MALL], start=False, stop=(j == i - 1))
                    k.stt(gw_t[:, i, :], ps[:, 0:32], 1.0, mall_v[:, i, :], ALU.add, ALU.mult, [ps, MALL], [POS])
                k.ts(gw_t[:].rearrange("p n e -> p (n e)"), gw_t[:].rearrange("p n e -> p (n e)"), -1.0, None, ALU.add, None,
                     [POS], [POS])

                k.op('pool', lambda e: e.memset(out_v[0], 0.0), writes=[OUTB[0]])
                ydv = yd_d.rearrange("(n p) d -> p n d", p=128)
                for r in range(32):
                    k.dma(sync, ydv[:, r, :], out_v[0], OUTB[0], YD, parts=True)
                k._wait('pool', (YD.dsem, YD.dval, None, 'd_yd'))
                for i in range(NT):
                    for half in range(2):
                        ps = PS[(2 * i + half) % 4]
                        for q in range(4):
                            kk = half * 4 + q
                            k.tr(ps, ps[:, q * 128:(q + 1) * 128], X[i][:, kk * 128:(kk + 1) * 128], IDENT, [X[i]])
                        k.copy('dve', xr_t[:, half * 4:half * 4 + 4, :], ps[:].rearrange("p (q t) -> p q t", q=4), [ps], [XR])
                    k.copy('act', xtm_v[:, i, :], X[i][:], [X[i]], [XTM[i]])
                    routing(i)
                routing_batched()
                for i in range(NT):
                    ps = PS[4 + i % 2]
                    k.mm(ps, ps[:, 0:32], stri_t[:], mall_v[:, i, :], [STRI, MALL], start=True, stop=(i == 0))
                    for j in range(i):
                        k.mm(ps, ps[:, 0:32], onesb_t[:], mall_v[:, j, :], [ONESB, MALL], start=False, stop=(j == i - 1))
                    k.stt(gw_t[:, i, :], ps[:, 0:32], 1.0, mall_v[:, i, :], ALU.add, ALU.mult, [ps, MALL], [POS])
                k.ts(gw_t[:].rearrange("p n e -> p (n e)"), gw_t[:].rearrange("p n e -> p (n e)"), -1.0, None, ALU.add, None,
                     [POS], [POS])

                def load_expert(e, buf):
                    k.dma('pool', ew_t[buf][:, 0:4096].rearrange("p (k c) -> p k c", k=8),
                          w_eg[li, e].rearrange("(k p) c -> p k c", p=128), DI, EW[buf])
                    k.dma('pool', ew_t[buf][:, 4096:8192].rearrange("p (k c) -> p k c", k=8),
                          w_eu[li, e].rearrange("(k p) c -> p k c", p=128), DI, EW[buf], parts=True)
                    k.dma('pool', ew_t[buf][:, 8192:12288].rearrange("p (k c) -> p k c", k=4),
                          w_ed[li, e].rearrange("(k p) c -> p k c", p=128), DI, EW[buf], parts=True)

                if n_experts > 0:
                    load_expert(0, 0)
                for e_ in range(n_experts):
                    buf = e_ % 2
                    if e_ + 1 < n_experts:
                        load_expert(e_ + 1, 1 - buf)
                    WG = ew_t[buf][:, 0:4096].rearrange("p (k c) -> p k c", k=8)
                    WU = ew_t[buf][:, 4096:8192].rearrange("p (k c) -> p k c", k=8)
                    WD = ew_t[buf][:, 8192:12288].rearrange("p (k c) -> p k c", k=4)
                    for i in range(NT):
                        selt = ROW[i // 8]
                        k.ts(sel_v[i // 8][:, i % 8, :], iota_t[:], gw_t[:, i, e_:e_ + 1], None, ALU.is_equal, None,
                             [IOTA, POS], [selt], eng=('dve' if (i % 2 == 0 or _os.environ.get('KSEL', 'dve') == 'dve') else 'pool'))
                    xg = XG[buf]; xgv = xg_v[buf]
                    for kf in range(8):
                        ps = PS[kf % 4]
                        for i in range(NT):
                            k.mm(ps, ps[:, 0:256], xtm_v[:, i, kf * 128:(kf + 1) * 128], sel_v[i // 8][:, i % 8, :],
                                 [XTM[i], ROW[i // 8]], start=(i == 0), stop=(i == NT - 1))
                        evac(xgv[:, kf, :], ps[:, 0:256], [ps], [xg])
                    psM = PS[4]
                    for sh in range(2):
                        for i in range(NT):
                            k.mm(psM, psM[:, sh * 8:sh * 8 + 3], sel_v[i // 8][:, i % 8, sh * 128:(sh + 1) * 128],
                                 cmeta_t[:, i, :], [ROW[i // 8], CMETA], start=(i == 0), stop=(i == NT - 1))
                        for i in range(NT):
                            k.mm(psM, psM[:, sh * 8 + 3:sh * 8 + 5], sel_v[i // 8][:, i % 8, sh * 128:(sh + 1) * 128],
                                 meta_v[:, i, e_, :], [ROW[i // 8], META], start=(i == 0), stop=(i == NT - 1))
                    sl = SLOT[buf]; slv = slot_t[:, buf]
                    k.copy('dve', slv.rearrange("p a c -> p (a c)"), psM[:, 0:16], [psM], [sl])
                    k.stt(slv[:, :, 5], slv[:, :, 0], 128.0, slv[:, :, 1], ALU.mult, ALU.add, [sl], [sl])
                    k.stt(slv[:, :, 5], slv[:, :, 3], 2048.0, slv[:, :, 5], ALU.mult, ALU.add, [sl], [sl])
                    k.ts(slv[:, :, 6], slv[:, :, 2], -1.0e6, 1.0e6, ALU.mult, ALU.add, [sl], [sl])
                    for sh in range(2):
                        k.tt(dest_tt[buf][sh][:, :], slv[:, sh, 5:6], slv[:, sh, 6:7], ALU.add, [sl], [DEST[buf][sh]])
                    hte = HTE[buf]; htev = hte_v[buf]
                    for m in range(4):
                        psg = PS[5 + (m % 2) * 2 - (m % 2)]; psu = PS[6 + (m % 2)]
                        psg = PS[4 + (m % 2) * 2 + 1] if False else PS[5] if m % 2 == 0 else PS[7]
                        psu = PS[6] if m % 2 == 0 else PS[4]
                        for kk in range(8):
                            k.mm(psg, psg[:, 0:256], WG[:, kk, m * 128:(m + 1) * 128], xgv[:, kk, :], [EW[buf], xg],
                                 start=(kk == 0), stop=(kk == 7))
                        for kk in range(8):
                            k.mm(psu, psu[:, 256:512], WU[:, kk, m * 128:(m + 1) * 128], xgv[:, kk, :], [EW[buf], xg],
                                 start=(kk == 0), stop=(kk == 7))
                        sg = SG[m % 2]
                        k.act(sg[:], psg[:, 0:256], AF.Silu, [psg], [sg])
                        k.tt(htev[:, m, :], sg[:], psu[:, 256:512], ALU.mult, [sg, psu], [hte])
                    for sh in range(2):
                        ob = OUTB[sh]
                        for hf in range(2):
                            ps = PS[(sh * 2 + hf) % 4]
                            for m in range(4):
                                k.mm(ps, ps[:], htev[:, m, sh * 128:(sh + 1) * 128], WD[:, m, hf * 512:(hf + 1) * 512],
                                     [hte, EW[buf]], start=(m == 0), stop=(m == 3))
                            k.act(out_v[sh][:, hf * 512:(hf + 1) * 512], ps[:], AF.Copy, [ps, sl], [ob], scale=slv[:, sh, 4:5])
                        k.dma_scatter(yd_d[:, :], dest_tt[buf][sh][:, :], out_v[sh], ob, DEST[buf][sh], YD, 4095)
                if _os.environ.get('KDEBUG', '') == 'dbgslot':
                    k.barrier()
                    k.copy('dve', X[0][:, 0:16], slot_t[:, 1].rearrange("p a c -> p (a c)"), [SLOT[1]], [X[0]])
                    k.copy('dve', X[0][:, 16:17], dest_tt[1][0][:, :], [DEST[1][0]], [X[0]])
                    k.copy('dve', X[0][:, 17:18], dest_tt[1][1][:, :], [DEST[1][1]], [X[0]])
                    k.copy('dve', X[0][:, 32:64], gw_t[:, 0, :], [POS], [X[0]])
                    k.copy('dve', X[0][:, 64:96], mall_v[:, 0, :], [MALL], [X[0]])
                    k.copy('dve', X[0][:, 96:128], meta_v[:, 0, :, 1], [META], [X[0]])
                    k.copy('dve', X[0][:, 128:160], gw_t[:, 15, :], [POS], [X[0]])
                for i in range(NT if _os.environ.get('KDEBUG', '') != 'dbgslot' else 0):
                    for kq in range(2):
                        ob = OUTB[kq]
                        k.dma(sync, out_v[kq], ydv[:, kq * 16 + i, :], YD, ob)
                        if kq == 0:
                            k.stt(X[i][:], X[i][:], ALPHA, out_v[kq], ALU.mult, ALU.add, [X[i], ob], [X[i]])
                        else:
                            k.tt(X[i][:], X[i][:], out_v[kq], ALU.add, [X[i], ob], [X[i]], eng='pool')
                k.barrier()

            if 'C' in phases:
                load_row(ROW[0], ln2_g[li]); load_row(ROW[1], ln2_b[li])
                for i in range(NT):
                    layer_norm_tile(i, ROW[0], ROW[1])
                build_xT()
                load_row(ROW[0], b_pg[li]); load_row(ROW[1], ple_g[li])
                for hf in range(2):
                    k.dma('pool', ew_t[hf][:, 0:4096].rearrange("p (k c) -> p k c", k=8),
                          w_pg[li][:, hf * 512:(hf + 1) * 512].rearrange("(k p) c -> p k c", p=128), DI, EW[hf])
                k.dma('pool', ew_t[0][:, 4096:6144].rearrange("p (k c) -> p k c", k=2),
                      w_pp[li].rearrange("(k p) c -> p k c", p=128), DI, EW[0], parts=True)
                WPP = ew_t[0][:, 4096:6144].rearrange("p (k c) -> p k c", k=2)
                PTt = [UST[n] for n in range(4)]
                for n in range(4):
                    k.dma('pool', ust_t[:, 0:2, n * 512:(n + 1) * 512],
                          pT_in[li][:, n * 512:(n + 1) * 512].rearrange("(k p) t -> p k t", p=128), DI, UST[n])
                for i in range(NT):
                    sl = slice(i * 128, (i + 1) * 128)
                    ms = MS[i % 2]
                    psG = [PS[0 + (i % 2) * 4], PS[1 + (i % 2) * 4]]
                    psP = [PS[2 + (i % 2) * 4], PS[3 + (i % 2) * 4]]
                    for hf in range(2):
                        W = ew_t[hf][:, 0:4096].rearrange("p (k c) -> p k c", k=8)
                        for kk in range(8):
                            k.mm(psG[hf], psG[hf][:], xt_t[:, kk, sl], W[:, kk, :], [XT[i // 4], EW[hf]],
                                 start=(kk == 0), stop=(kk == 7))
                        for k2 in range(2):
                            k.mm(psP[hf], psP[hf][:], ust_t[:, k2, sl], WPP[:, k2, hf * 512:(hf + 1) * 512],
                                 [UST[i // 4], EW[0]], start=(k2 == 0), stop=(k2 == 1))
                    tq = CTMP[(i % 2) * 3]; tg = CTMP[(i % 2) * 3 + 1]; tp_ = CTMP[(i % 2) * 3 + 2]
                    for hf in range(2):
                        k.act(tq[:], psP[hf][:], AF.Square, [psP[hf]], [tq, ms], accum_out=ms[:, 16 + hf:17 + hf])
                    k.tt(ms[:, 18:19], ms[:, 16:17], ms[:, 17:18], ALU.add, [ms], [ms])
                    k.rsqrt_eps(ms[:, 19:20], ms[:, 18:19], [ms], [ms], pre_scale=1.0 / 1024.0)
                    for hf in range(2):
                        hs = slice(hf * 512, (hf + 1) * 512)
                        k.tt(tg[:], psG[hf][:], row_t[0][:, hs], ALU.add, [psG[hf], ROW[0]], [tg])
                        k.act(tg[:], tg[:], AF.Sigmoid, [tg], [tg])
                        k.stt(tp_[:], psP[hf][:], ms[:, 19:20], row_t[1][:, hs], ALU.mult, ALU.mult, [psP[hf], ms, ROW[1]], [tp_])
                        k.tt(tg[:], tg[:], tp_[:], ALU.mult, [tg, tp_], [tg], eng='pool')
                        k.tt(X[i][:, hs], X[i][:, hs], tg[:], ALU.add, [X[i], tg], [X[i]], eng='pool')
                k.barrier()

        for _pi in range(int(_os.environ.get('KPAD', '0'))):
            k.op('dve', lambda e: e.memset(sm2_t[:, 300:301], 0.0), writes=[])
        yv = y_out.rearrange("(n p) d -> p n d", p=128)
        for i in range(NT):
            k.dma(sync, yv[:, i, :], X[i][:], X[i], YO, parts=True)
        if not k.dry:
            nc.sync.wait_ge(YO.dsem, YO.dval)
    nc._declared_inputs = declared
    return nc, k.waited


def prep_shared(inp):
    f = lambda a: np.ascontiguousarray(np.asarray(a, dtype=np.float32))
    L = DEPTH
    sh = {}
    for n in ['w_in', 'conv_w', 'conv_b', 'w_q', 'w_k', 'mh_g', 'w_glu', 'b_glu', 's5_g', 'w_out', 'ln1_g', 'ln1_b',
              'w_eg', 'w_eu', 'w_ed', 'ln2_g', 'ln2_b', 'w_pg', 'b_pg', 'w_pp', 'ple_g']:
        sh[n] = f(inp[n])
    sh['bg'] = f(np.concatenate([np.asarray(inp['b_i']), np.asarray(inp['b_f'])], axis=1))
    sh['lam_re'] = f(np.asarray(inp['lam_re']).reshape(L, 2048))
    sh['lam_im'] = f(np.asarray(inp['lam_im']).reshape(L, 2048))
    sh['logdt_x'] = f(np.repeat(np.asarray(inp['log_dt'])[:, :, None], 64, axis=2).reshape(L, 2048))
    sh['d_skip'] = f(np.asarray(inp['d_skip']).reshape(L, 512))
    b_re = np.asarray(inp['b_re']); b_im = np.asarray(inp['b_im'])
    c_re = np.asarray(inp['c_re']); c_im = np.asarray(inp['c_im'])
    brp = np.zeros((L, 128, 32, 64), np.float32); bip = np.zeros((L, 128, 32, 64), np.float32)
    crp = np.zeros((L, 2, 64, 16, 128), np.float32); cip = np.zeros((L, 2, 64, 16, 128), np.float32)
    for g in range(32):
        r0 = (g % 8) * 16
        brp[:, r0:r0 + 16, g, :] = np.transpose(b_re[:, g], (0, 2, 1))
        bip[:, r0:r0 + 16, g, :] = np.transpose(b_im[:, g], (0, 2, 1))
        j, gi = g // 2, g % 2
        crp[:, gi, :, j, r0:r0 + 16] = np.transpose(c_re[:, g], (0, 2, 1))
        cip[:, gi, :, j, r0:r0 + 16] = np.transpose(c_im[:, g], (0, 2, 1))
    sh['brp'] = brp.reshape(L, 128, 2048); sh['bip'] = bip.reshape(L, 128, 2048)
    sh['crp'] = crp.reshape(L, 128, 16, 128); sh['cip'] = cip.reshape(L, 128, 16, 128)
    sh['w_r'] = f(np.concatenate([np.asarray(inp['w_grp']), np.asarray(inp['w_rt'])], axis=2))
    sh['b_r'] = f(np.concatenate([np.asarray(inp['b_grp']), np.asarray(inp['b_rt'])], axis=1))
    sh['ident'] = np.eye(128, dtype=np.float32)
    sh['tri'] = np.triu(np.ones((128, 128), np.float32))
    sh['tau'] = np.ascontiguousarray(np.broadcast_to(np.arange(128, dtype=np.float32), (128, 128)))
    sh['iota256'] = np.ascontiguousarray(np.broadcast_to(np.arange(256, dtype=np.float32), (128, 256)))
    sh['stri'] = np.triu(np.ones((128, 128), np.float32), k=1)
    cm = np.zeros((128, 16, 3), np.float32)
    cm[:, :, 0] = np.arange(16, dtype=np.float32)[None, :]
    cm[:, :, 1] = np.arange(128, dtype=np.float32)[:, None]
    cm[:, :, 2] = 1.0
    sh['cmeta'] = cm
    return sh


_NC_CACHE = {}
PER_LAYER = ['w_in', 'conv_w', 'conv_b', 'w_q', 'w_k', 'bg', 'mh_g', 'lam_re', 'lam_im', 'logdt_x', 'brp', 'bip', 'crp',
             'cip', 'd_skip', 'w_glu', 'b_glu', 's5_g', 'w_out', 'ln1_g', 'ln1_b', 'w_r', 'b_r', 'w_eg', 'w_eu', 'w_ed',
             'ln2_g', 'ln2_b', 'w_pg', 'b_pg', 'w_pp', 'ple_g']


def _launch(nc, sh, xs, pTs, li):
    names = set(nc._declared_inputs)
    base = {}
    for kname in names:
        if kname in ('x', 'pT'):
            continue
        a = sh[kname]
        base[kname] = np.ascontiguousarray(a[li:li + 1]) if kname in PER_LAYER else a
    in_maps = []
    for b in range(8):
        m = dict(base)
        m['x'] = xs[b]
        m['pT'] = pTs[b][li:li + 1]
        in_maps.append(m)
    res = run_bass_kernel_spmd(nc, in_maps, core_ids=list(range(8)))
    return [np.ascontiguousarray(res.results[b]['y']) for b in range(8)]


def kernel(**inputs):
    sh = prep_shared(inputs)
    x = np.asarray(inputs['x'], dtype=np.float32)
    p = np.asarray(inputs['p'], dtype=np.float32)
    if 'full' not in _NC_CACHE:
        _NC_CACHE['full'] = build_program()
    nc = _NC_CACHE['full']
    names = set(nc._declared_inputs)
    base = {kname: sh[kname] for kname in names if kname not in ('x', 'pT')}
    in_maps = []
    for b in range(8):
        m = dict(base)
        m['x'] = np.ascontiguousarray(x[b])
        m['pT'] = np.ascontiguousarray(np.transpose(p[:, b], (0, 2, 1)))
        in_maps.append(m)
    res = run_bass_kernel_spmd(nc, in_maps, core_ids=list(range(8)))
    return np.stack([res.results[b]['y'] for b in range(8)], axis=0).astype(np.float32)
```

```python
import math
import bisect
import os as _os
import numpy as np
import concourse.bass as bass
import concourse.mybir as mybir
from concourse.bass_utils import run_bass_kernel_spmd
from contextlib import ExitStack

F32 = mybir.dt.float32
BF16 = mybir.dt.bfloat16
ALU = mybir.AluOpType
AF = mybir.ActivationFunctionType

D = 1024; S = 2048; NT = 16; DEPTH = 4
D_IN = 2056
ALPHA = (2 * DEPTH) ** 0.25
EPS = 1e-5
PI = math.pi


class T:
    def __init__(s, ap, name):
        s.ap = ap; s.name = name; s.w = []; s.r = []; s.dsem = None; s.dval = 0

    def __getitem__(s, idx):
        return s.ap[idx]


class K:
    def __init__(s, nc, es, needed=None):
        s.nc = nc; s.es = es
        s.dry = needed is None
        s.needed = needed or {}
        s.needed_set = {e: set(v) for e, v in s.needed.items()}
        s.waited = {}
        s.E = {'pe': nc.tensor, 'dve': nc.vector, 'act': nc.scalar, 'pool': nc.gpsimd, 'sp': nc.sync}
        s.sem = {e: es.enter_context(nc.semaphore('sem_' + e)) for e in s.E}
        s.cnt = {e: 0 for e in s.E}
        s.known = {e: {} for e in s.E}
        s.dma_tiles = []
        s.nt = 0

    def tile(s, ap, name=None):
        s.nt += 1
        return T(ap, name or ('t%d' % s.nt))

    def sb(s, name, shape, dt):
        return s.es.enter_context(s.nc.sbuf_tensor(name, shape, dt))

    def _wait(s, eng, ev):
        sem, val, deng, key = ev
        if s.known[eng].get(key, 0) >= val:
            return
        s.known[eng][key] = val
        if deng is not None:
            s.waited.setdefault(deng, set()).add(val)
            if not s.dry:
                rank = bisect.bisect_right(s.needed[deng], val)
                s.E[eng].wait_ge(sem, rank)
        elif not s.dry:
            s.E[eng].wait_ge(sem, val)

    def _deps(s, eng, reads, writes, skipkey=None):
        for t in reads:
            for ev in t.w:
                if ev[2] == eng and eng == 'pe':
                    continue
                s._wait(eng, ev)
        for t in writes:
            for ev in t.w + t.r:
                if ev[2] == eng:
                    continue
                if skipkey is not None and ev[3] == skipkey:
                    continue
                s._wait(eng, ev)

    def op(s, eng, fn, reads=(), writes=()):
        s._deps(eng, reads, writes)
        s.cnt[eng] += 1
        if not s.dry:
            ins = fn(s.E[eng])
            if s.cnt[eng] in s.needed_set.get(eng, ()):
                ins.then_inc(s.sem[eng], 1)
        ev = (s.sem[eng], s.cnt[eng], eng, eng)
        for t in writes:
            t.w = [ev]; t.r = []
        for t in reads:
            if t in writes:
                continue
            t.r = [e for e in t.r if e[3] != eng] + [ev]

    def dma(s, q, out_ap, in_ap, src, dst, parts=False, **kw):
        key = 'd_' + dst.name
        s._deps(q, [src], [dst], skipkey=key if parts else None)
        if dst.dsem is None:
            dst.dsem = True if s.dry else s.es.enter_context(s.nc.semaphore(key))
            s.dma_tiles.append(dst)
        dst.dval += 16
        if not s.dry:
            ins = s.E[q].dma_start(out=out_ap, in_=in_ap, **kw)
            ins.then_inc(dst.dsem, 16)
        ev = (dst.dsem, dst.dval, None, key)
        dst.w = [ev]; dst.r = []
        src.r = [e for e in src.r if e[3] != key] + [ev]

    def dma_scatter(s, out_ap, idx_ap, in_ap, src, idxt, dst, bound):
        key = 'd_' + dst.name
        s._deps('pool', [src, idxt], [dst], skipkey=key)
        if dst.dsem is None:
            dst.dsem = True if s.dry else s.es.enter_context(s.nc.semaphore(key))
            s.dma_tiles.append(dst)
        dst.dval += 16
        ev = (dst.dsem, dst.dval, None, key)
        if not s.dry:
            ins = s.nc.gpsimd.indirect_dma_start(out=out_ap, out_offset=bass.IndirectOffsetOnAxis(ap=idx_ap, axis=0),
                                                 in_=in_ap, in_offset=None, bounds_check=s.bnd_reg, oob_is_err=False)
            ins.then_inc(dst.dsem, 16)
        dst.w = [ev]; dst.r = []
        for t in (src, idxt):
            t.r = [e for e in t.r if e[3] != key] + [ev]

    def barrier(s):
        for e in s.E:
            for e2 in s.E:
                if e2 != e and s.cnt[e2] > 0:
                    s._wait(e, (s.sem[e2], s.cnt[e2], e2, e2))
            for t in s.dma_tiles:
                if t.dval > 0:
                    s._wait(e, (t.dsem, t.dval, None, 'd_' + t.name))

    def mm(s, ps, out_ap, lhsT_ap, rhs_ap, reads, start=True, stop=True):
        s.op('pe', lambda e: e.matmul(out_ap, lhsT=lhsT_ap, rhs=rhs_ap, start=start, stop=stop),
             reads=reads, writes=[ps])

    def tr(s, ps, out_ap, in_ap, ident, reads):
        s.op('pe', lambda e: e.transpose(out_ap, in_ap, ident[:]), reads=list(reads) + [ident], writes=[ps])

    def act(s, out_ap, in_ap, func, reads, writes, bias=None, scale=None, accum_out=None, eng='act'):
        kw = {}
        if bias is not None: kw['bias'] = bias
        if scale is not None: kw['scale'] = scale
        if accum_out is not None: kw['accum_out'] = accum_out
        s.op('act', lambda e: e.activation(out=out_ap, in_=in_ap, func=func, **kw), reads=reads, writes=writes)

    def tt(s, out_ap, in0, in1, op, reads, writes, eng='dve'):
        if eng == 'pool' and _os.environ.get('KPOOL', '0') != '1':
            eng = 'dve'
        if eng == 'POOL':
            eng = 'pool'
        s.op(eng, lambda e: e.tensor_tensor(out=out_ap, in0=in0, in1=in1, op=op), reads=reads, writes=writes)

    def ts(s, out_ap, in0, s1, s2, op0, op1, reads, writes, eng='dve'):
        if eng == 'pool' and _os.environ.get('KPOOL', '0') != '1':
            eng = 'dve'
        if op1 is None:
            s.op(eng, lambda e: e.tensor_scalar(out=out_ap, in0=in0, scalar1=s1, scalar2=None, op0=op0),
                 reads=reads, writes=writes)
        else:
            s.op(eng, lambda e: e.tensor_scalar(out=out_ap, in0=in0, scalar1=s1, scalar2=s2, op0=op0, op1=op1),
                 reads=reads, writes=writes)

    def stt(s, out_ap, in0, scalar, in1, op0, op1, reads, writes):
        s.op('dve', lambda e: e.scalar_tensor_tensor(out=out_ap, in0=in0, scalar=scalar, in1=in1, op0=op0, op1=op1),
             reads=reads, writes=writes)

    def rsqrt_eps(s, out_ap, in_ap, reads, writes, pre_scale=1.0):
        s.act(out_ap, in_ap, AF.Ln, reads, writes, bias=s.eps_ap, scale=pre_scale)
        s.act(out_ap, out_ap, AF.Exp, writes, writes, scale=-0.5)

    def copy(s, eng, out_ap, in_ap, reads, writes):
        if eng == 'act':
            s.op('act', lambda e: e.copy(out=out_ap, in_=in_ap), reads=reads, writes=writes)
        else:
            s.op(eng, lambda e: e.tensor_copy(out=out_ap, in_=in_ap), reads=reads, writes=writes)


PARAM_NAMES = ['w_in', 'conv_w', 'conv_b', 'w_q', 'w_k', 'bg', 'mh_g', 'lam_re', 'lam_im', 'logdt_x',
               'brp', 'bip', 'crp', 'cip', 'd_skip', 'w_glu', 'b_glu', 's5_g', 'w_out', 'ln1_g', 'ln1_b',
               'w_r', 'b_r', 'w_eg', 'w_eu', 'w_ed', 'ln2_g', 'ln2_b', 'w_pg', 'b_pg', 'w_pp', 'ple_g']


def build_program(layers=tuple(range(DEPTH)), phases=('A', 'B', 'C'), n_experts=32, dbg=None, L=DEPTH):
    _, waited = _build(layers, phases, n_experts, L, None)
    needed = {e: sorted(v) for e, v in waited.items()}
    nc, _ = _build(layers, phases, n_experts, L, needed)
    return nc


def _build(layers, phases, n_experts, L, needed):
    nc = bass.Bass("TRN2", target_bir_lowering=False)

    declared = []

    def din(name, shape, dt=F32):
        declared.append(name)
        return nc.dram_tensor(name, shape, dt, kind="ExternalInput").ap()

    x_in = din('x', [S, D])
    pT_in = din('pT', [L, 256, S])
    ident_in = din('ident', [128, 128]); tri_in = din('tri', [128, 128]); tau_in = din('tau', [128, 128])
    iota_in = din('iota256', [128, 256]); stri_in = din('stri', [128, 128]); cmeta_in = din('cmeta', [128, 16, 3])
    w_in = din('w_in', [L, D, D_IN]); conv_w = din('conv_w', [L, 4, 512]); conv_b = din('conv_b', [L, 512])
    w_q = din('w_q', [L, 4, 128, 128]); w_k = din('w_k', [L, 4, 128, 128]); bg_in = din('bg', [L, 8])
    mh_g = din('mh_g', [L, 512])
    lam_re = din('lam_re', [L, 2048]); lam_im = din('lam_im', [L, 2048]); logdt_x = din('logdt_x', [L, 2048])
    brp = din('brp', [L, 128, 2048]); bip = din('bip', [L, 128, 2048])
    crp = din('crp', [L, 128, 16, 128]); cip = din('cip', [L, 128, 16, 128])
    d_skip = din('d_skip', [L, 512]); w_glu = din('w_glu', [L, 512, 512]); b_glu = din('b_glu', [L, 512])
    s5_g = din('s5_g', [L, 512]); w_out = din('w_out', [L, D, D])
    ln1_g = din('ln1_g', [L, D]); ln1_b = din('ln1_b', [L, D])
    w_r = din('w_r', [L, D, 36]); b_r = din('b_r', [L, 36])
    if 'B' in phases:
        w_eg = din('w_eg', [L, 32, D, 512]); w_eu = din('w_eu', [L, 32, D, 512]); w_ed = din('w_ed', [L, 32, 512, D])
    ln2_g = din('ln2_g', [L, D]); ln2_b = din('ln2_b', [L, D])
    w_pg = din('w_pg', [L, D, D]); b_pg = din('b_pg', [L, D]); w_pp = din('w_pp', [L, 256, D]); ple_g = din('ple_g', [L, D])
    y_out = nc.dram_tensor('y', [S, D], F32, kind="ExternalOutput").ap()
    xs_d = nc.dram_tensor('xs_scr', [S, D], F32, kind="Internal").ap()
    cri_d = nc.dram_tensor('cri_scr', [2, 2048], F32, kind="Internal").ap()
    yd_d = nc.dram_tensor('yd_scr', [4096, D], F32, kind="Internal").ap()

    es = ExitStack()
    with es:
        k = K(nc, es, needed)
        k.bnd_reg = None
        if not k.dry:
            k.bnd_reg = nc.gpsimd.alloc_register('bnd')
            nc.gpsimd.reg_mov(k.bnd_reg, 4095)
        DI = k.tile(None, 'dram_in')
        XS = k.tile(xs_d, 'xs'); CRI = k.tile(cri_d, 'cri'); YO = k.tile(y_out, 'yo'); YD = k.tile(yd_d, 'yd')

        arena = k.sb('arena', [128, 32768], BF16)
        Xv = arena[:].bitcast(F32).rearrange("p (n d) -> p n d", n=NT)
        X = [k.tile(Xv[:, i, :], 'X%d' % i) for i in range(NT)]
        xt_t = k.sb('xt', [128, 8, S], BF16)
        XT = [k.tile(xt_t[:, :, n * 512:(n + 1) * 512], 'XT%d' % n) for n in range(4)]
        ust_t = k.sb('ust', [128, 4, S], BF16)
        UST = [k.tile(ust_t[:, :, n * 512:(n + 1) * 512], 'UST%d' % n) for n in range(4)]
        HT = UST
        ew_t = [k.sb('ew%d' % i, [128, 12288], BF16) for i in range(2)]
        EW = [k.tile(ew_t[i][:], 'EW%d' % i) for i in range(2)]
        cw_t = k.sb('cw', [128, 3, 16, 128], BF16); CW = k.tile(cw_t[:], 'CW')
        ktm_t = k.sb('ktm', [128, NT, 128], BF16); KTM = k.tile(ktm_t[:], 'KTM')
        row_t = [k.sb('row%d' % i, [128, D], F32) for i in range(2)]
        ROW = [k.tile(row_t[i][:], 'ROW%d' % i) for i in range(2)]
        xr_t = k.sb('xr', [128, 8, 128], F32); XR = k.tile(xr_t[:], 'XR')
        XLB = [k.tile(xr_t[:].rearrange("p a b -> p (a b)"), 'XLB0'),
               k.tile(ust_t[:, 0, :].bitcast(F32), 'XLB1')]
        ctmp_v = [xr_t[:].rearrange("p a b -> p (a b)"), ust_t[:, 2, :].bitcast(F32), ust_t[:, 3, :].bitcast(F32)]
        CTMP = [k.tile(ctmp_v[a][:, b * 512:(b + 1) * 512], 'CTMP%d' % (a * 2 + b)) for a in range(3) for b in range(2)]
        gw_t = k.sb('gw', [128, NT, 32], F32); GW = [k.tile(gw_t[:, i, :], 'GW%d' % i) for i in range(NT)]
        ident_t = k.sb('ident_s', [128, 128], F32); IDENT = k.tile(ident_t[:], 'IDENT')
        tri_t = k.sb('tri_s', [128, 128], F32); TRI = k.tile(tri_t[:], 'TRI')
        tau_t = k.sb('tau_s', [128, 128], F32); TAU = k.tile(tau_t[:], 'TAU')
        ones_t = k.sb('ones', [128, 128], F32); ONES = k.tile(ones_t[:], 'ONES')
        onesk_t = k.sb('onesk', [128, 128], F32); ONESK = k.tile(onesk_t[:], 'ONESK')
        sm_t = k.sb('small', [128, 512], F32)
        SM = k.tile(sm_t[:], 'SM')
        sm2_t = k.sb('small2', [128, 512], F32)
        smi_t = k.sb('smi', [128, 16], mybir.dt.int32)
        eps_t = k.sb('eps', [128, 1], F32)
        k.op('pool', lambda e: e.memset(eps_t[:], EPS), writes=[])
        k.eps_ap = eps_t[:]
        wr_t = k.sb('wr_s', [128, 8, 36], F32); WR = k.tile(wr_t[:], 'WR')
        wqk_t = k.sb('wqk', [128, 2, 4, 128], BF16); WQK = k.tile(wqk_t[:], 'WQK')
        gat_t = k.sb('gates', [128, NT, 8], F32); GATES = k.tile(gat_t[:], 'GATES')
        gx_t = k.sb('gx', [128, 5, NT, 4], F32); GX = k.tile(gx_t[:], 'GX')
        cst_t = k.sb('cst', [128, 129], F32); CST = k.tile(cst_t[:], 'CST')
        cbf_t = k.sb('cbf', [128, 129], BF16); CBF = k.tile(cbf_t[:], 'CBF')
        stb_t = k.sb('stb', [128, 2, 128], BF16); STB = [k.tile(stb_t[:, i, :], 'STB%d' % i) for i in range(2)]
        wv_t = k.sb('wv', [128, 2, 129], BF16); WV = [k.tile(wv_t[:, i, :], 'WV%d' % i) for i in range(2)]
        hh_t = k.sb('hh', [128, 2, 128], F32); HH = [k.tile(hh_t[:, i, :], 'HH%d' % i) for i in range(2)]
        ms_t = k.sb('ms', [128, 2, 32], F32); MS = [k.tile(ms_t[:, i, :], 'MS%d' % i) for i in range(2)]
        PS = []
        for b in range(8):
            pt = es.enter_context(nc.psum_tensor('ps%d' % b, [128, 512], F32))
            PS.append(k.tile(pt[:], 'PS%d' % b))

        o0 = 0
        vext_v = arena[:, o0:o0 + NT * 4 * 129].rearrange("p (n h c) -> p n h c", n=NT, h=4); o0 += NT * 4 * 129
        sigo_v = arena[:, o0:o0 + NT * 512].rearrange("p (n c) -> p n c", n=NT); o0 += NT * 512
        um_v = arena[:, o0:o0 + 4 * 2051].rearrange("p (h t) -> p h t", h=4); o0 += 4 * 2052
        c_v = arena[:, o0:o0 + S]; o0 += S
        qt_v = arena[:, o0:o0 + S]; o0 += S
        kt_v = arena[:, o0:o0 + S]; o0 += S
        assert o0 <= 32768, o0
        VEXT = [k.tile(vext_v[:, i], 'VEXT%d' % i) for i in range(NT)]
        SIGO = [k.tile(sigo_v[:, i], 'SIGO%d' % i) for i in range(NT)]
        UM = [k.tile(um_v[:, h], 'UM%d' % h) for h in range(4)]
        CC = k.tile(c_v, 'CC'); QT = k.tile(qt_v, 'QT'); KT = k.tile(kt_v, 'KT')
        o1 = 0
        tp_v = arena[:, 0:12 * 1024].bitcast(F32).rearrange("p (n c) -> p n c", n=12); o1 = 12 * 1024
        TP = [k.tile(tp_v[:, i], 'TP%d' % i) for i in range(12)]
        gyb_v = arena[:, o1:o1 + 2048].rearrange("p (n c) -> p n c", n=4); o1 += 2048
        GYB = k.tile(gyb_v, 'GYB')
        rb_v = arena[:, o1:o1 + 2048].rearrange("p (n c) -> p n c", n=4); o1 += 2048
        RB = [k.tile(rb_v[:, i], 'RB%d' % i) for i in range(4)]
        tab_v = arena[:, o1:o1 + 4 * 16 * 128].rearrange("p (a j t) -> p a j t", a=4, j=16); o1 += 4 * 16 * 128
        TAB = k.tile(tab_v, 'TAB')
        bb_v = arena[:, o1:o1 + 2 * 2048].rearrange("p (a c) -> p a c", a=2); o1 += 4096
        BB = k.tile(bb_v, 'BB')
        tp2_v = arena[:, o1:o1 + 4096].bitcast(F32).rearrange("p (n c) -> p n c", n=4); o1 += 4096
        TP2 = [k.tile(tp2_v[:, i], 'TP2_%d' % i) for i in range(4)]
        assert o1 <= 32768, o1

        iota_t = k.sb('iota_s', [128, 256], F32); IOTA = k.tile(iota_t[:], 'IOTA')
        stri_t = k.sb('stri_s', [128, 128], BF16); STRI = k.tile(stri_t[:], 'STRI')
        onesb_t = k.sb('onesb', [128, 128], BF16); ONESB = k.tile(onesb_t[:], 'ONESB')
        cmeta_t = k.sb('cmeta_s', [128, 16, 3], BF16); CMETA = k.tile(cmeta_t[:], 'CMETA')
        slot_t = k.sb('slot', [128, 2, 2, 8], F32); SLOT = [k.tile(slot_t[:, b], 'SLOT%d' % b) for b in range(2)]
        dest_tt = [[k.sb('dest%d%d' % (b, h), [128, 1], mybir.dt.int32) for h in range(2)] for b in range(2)]
        DEST = [[k.tile(dest_tt[b][h][:, :], 'DEST%d%d' % (b, h)) for h in range(2)] for b in range(2)]
        xtm_v = xt_t[:].rearrange("p k t -> p (k t)").rearrange("p (n d) -> p n d", n=NT)
        XTM = [k.tile(xtm_v[:, i, :], 'XTM%d' % i) for i in range(NT)]
        ustf = ust_t[:].rearrange("p k t -> p (k t)")
        xg_v = [ustf[:, b * 2048:(b + 1) * 2048].rearrange("p (k c) -> p k c", k=8) for b in range(2)]
        XG = [k.tile(xg_v[b], 'XG%d' % b) for b in range(2)]
        hte_v = [ustf[:, 4096 + b * 1024:4096 + (b + 1) * 1024].rearrange("p (k c) -> p k c", k=4) for b in range(2)]
        HTE = [k.tile(hte_v[b], 'HTE%d' % b) for b in range(2)]
        sg_v = [ustf[:, 6144 + b * 512:6144 + (b + 1) * 512].bitcast(F32) for b in range(2)]
        SG = [k.tile(sg_v[b], 'SG%d' % b) for b in range(2)]
        cwf = cw_t[:].rearrange("p a j c -> p (a j c)")
        out_v = [cwf[:, b * 2048:(b + 1) * 2048].bitcast(F32) for b in range(2)]
        OUTB = [k.tile(out_v[b], 'OUTB%d' % b) for b in range(2)]
        meta_v = cwf[:, 4096:4096 + 1024].rearrange("p (n e c) -> p n e c", n=NT, e=32)
        META = k.tile(meta_v, 'META')
        mall_v = sm_t[:, 0:256].bitcast(BF16).rearrange("p (n e) -> p n e", n=NT)
        MALL = k.tile(mall_v, 'MALL')
        sel_v = [row_t[b][:].bitcast(BF16).rearrange("p (n c) -> p n c", n=8) for b in range(2)]
        sync = 'sp'
        k.dma(sync, iota_t[:], iota_in, DI, IOTA)
        k.dma('pool', stri_t[:], stri_in, DI, STRI)
        k.dma('pool', cmeta_t[:], cmeta_in, DI, CMETA)
        k.op('pool', lambda e: e.memset(onesb_t[:], 1.0), writes=[ONESB])
        k.dma(sync, ident_t[:], ident_in, DI, IDENT)
        k.dma(sync, tri_t[:], tri_in, DI, TRI)
        k.dma(sync, tau_t[:], tau_in, DI, TAU)
        k.op('pool', lambda e: e.memset(ones_t[:], 1.0), writes=[ONES])
        k.op('pool', lambda e: e.memset(onesk_t[:], 1.0 / 512.0), writes=[ONESK])
        xin_v = x_in.rearrange("(n p) d -> p n d", p=128)
        for i in range(NT):
            k.dma(sync, X[i][:], xin_v[:, i, :], DI, X[i])

        evac_flip = [0]

        def evac(out_ap, in_ap, reads, writes):
            evac_flip[0] ^= 1
            k.copy('act' if evac_flip[0] else 'dve', out_ap, in_ap, reads, writes)

        def build_xT(extra=None):
            for i in range(NT):
                for half in range(2):
                    ps = PS[(2 * i + half) % 4]
                    for q in range(4):
                        kk = half * 4 + q
                        k.tr(ps, ps[:, q * 128:(q + 1) * 128], X[i][:, kk * 128:(kk + 1) * 128], IDENT, [X[i]])
                    psv = ps[:].rearrange("p (q t) -> p q t", q=4)
                    if extra is None:
                        evac(xt_t[:, half * 4:half * 4 + 4, i * 128:(i + 1) * 128], psv, [ps], [XT[i // 4]])
                    else:
                        k.copy('dve', xr_t[:, half * 4:half * 4 + 4, :], psv, [ps], [XR])
                        k.copy('act', xt_t[:, half * 4:half * 4 + 4, i * 128:(i + 1) * 128], xr_t[:, half * 4:half * 4 + 4, :],
                               [XR], [XT[i // 4]])
                if extra is not None:
                    extra(i)

        def load_row(row, vec_ap):
            k.dma(sync, row[:], vec_ap.partition_broadcast(128), DI, row)

        def layer_norm_tile(i, G, Bt):
            st = MS[i % 2]
            xi = X[i]
            k.op('dve', lambda e: e.bn_stats(out=st[:, 0:6], in_=xi[:, 0:512]), reads=[xi], writes=[st])
            k.op('dve', lambda e: e.bn_stats(out=st[:, 6:12], in_=xi[:, 512:1024]), reads=[xi, st], writes=[st])
            k.op('dve', lambda e: e.bn_aggr(out=st[:, 12:14], in_=st[:, 0:12]), reads=[st], writes=[st])
            k.rsqrt_eps(st[:, 14:15], st[:, 13:14], [st], [st])
            k.ts(xi[:], xi[:], st[:, 12:13], st[:, 14:15], ALU.subtract, ALU.mult, [xi, st], [xi])
            k.tt(xi[:], xi[:], G[:], ALU.mult, [xi, G], [xi], eng='pool')
            k.tt(xi[:], xi[:], Bt[:], ALU.add, [xi, Bt], [xi], eng='pool')

        for li in layers:
            if 'A' in phases:
                build_xT()
                for i in range(NT):
                    k.dma(sync, xs_d.rearrange("(n p) d -> p n d", p=128)[:, i, :], X[i][:], X[i], XS, parts=True)
                k.barrier()
                for j in range(4):
                    k.dma(sync, sm_t[:, j * 4:j * 4 + 4], conv_w[li][j].rearrange("(h p) -> p h", p=128), DI, SM,
                          parts=(j > 0), allow_slow_non_contiguous=True)
                for (c0, src) in ((16, conv_b), (20, mh_g), (24, d_skip), (28, b_glu), (32, s5_g)):
                    k.dma(sync, sm_t[:, c0:c0 + 4], src[li].rearrange("(h p) -> p h", p=128), DI, SM, parts=True,
                          allow_slow_non_contiguous=True)
                k.dma(sync, sm_t[:, 40:48], bg_in[li].partition_broadcast(128), DI, SM, parts=True)
                k.dma('pool', wqk_t[:, 0], w_q[li].rearrange("h d e -> d h e"), DI, WQK)
                k.dma('pool', wqk_t[:, 1], w_k[li].rearrange("h d e -> d h e"), DI, WQK, parts=True)

                def load_w(buf, col0, ncols):
                    k.dma('pool', ew_t[buf][:, 0:8 * ncols].rearrange("p (k c) -> p k c", k=8),
                          w_in[li][:, col0:col0 + ncols].rearrange("(k p) c -> p k c", p=128), DI, EW[buf],
                          allow_slow_non_contiguous=(ncols < 128))
                    return ew_t[buf][:, 0:8 * ncols].rearrange("p (k c) -> p k c", k=8)

                def fm_piece(buf, col0, dest_fn, dest_tiles):
                    W = load_w(buf, col0, 512)
                    for m in range(4):
                        for n in range(4):
                            ps = PS[(m * 4 + n) % 4]
                            for kk in range(8):
                                k.mm(ps, ps[:], W[:, kk, m * 128:(m + 1) * 128], xt_t[:, kk, n * 512:(n + 1) * 512],
                                     [EW[buf], XT[n]], start=(kk == 0), stop=(kk == 7))
                            evac(dest_fn(m, n), ps[:], [ps], [dest_tiles(m, n)])

                fm_piece(0, 1544, lambda m, n: ust_t[:, m, n * 512:(n + 1) * 512], lambda m, n: UST[n])
                for h in range(4):
                    k.op('pool', lambda e, h=h: e.memset(um_v[:, h, 0:3], 0.0), writes=[UM[h]])
                fm_piece(1, 0, lambda m, n: um_v[:, m, 3 + n * 512:3 + (n + 1) * 512], lambda m, n: UM[m])
                W = load_w(0, 512, 512)
                for i in range(NT):
                    ps = PS[i % 4]
                    for kk in range(8):
                        k.mm(ps, ps[:], xt_t[:, kk, i * 128:(i + 1) * 128], W[:, kk, :], [EW[0], XT[i // 4]],
                             start=(kk == 0), stop=(kk == 7))
                    k.op('pool', lambda e, i=i: e.memset(vext_v[:, i, :, 128:129], 1.0), writes=[VEXT[i]])
                    evac(vext_v[:, i, :, 0:128], ps[:].rearrange("p (h c) -> p h c", h=4), [ps], [VEXT[i]])
                W = load_w(1, 1024, 512)
                for i in range(NT):
                    ps = PS[i % 4]
                    for kk in range(8):
                        k.mm(ps, ps[:], xt_t[:, kk, i * 128:(i + 1) * 128], W[:, kk, :], [EW[1], XT[i // 4]],
                             start=(kk == 0), stop=(kk == 7))
                    k.act(sigo_v[:, i, :], ps[:], AF.Sigmoid, [ps], [SIGO[i]])
                W = load_w(0, 1536, 8)
                for i in range(NT):
                    ps = PS[i % 4]
                    for kk in range(8):
                        k.mm(ps, ps[:, 0:8], xt_t[:, kk, i * 128:(i + 1) * 128], W[:, kk, :], [EW[0], XT[i // 4]],
                             start=(kk == 0), stop=(kk == 7))
                    k.tt(gat_t[:, i, :], ps[:, 0:8], sm_t[:, 40:48], ALU.add, [ps, SM], [GATES])

                LF = gx_t[:, 0]; BC = gx_t[:, 1]; GG = gx_t[:, 2]; AA = gx_t[:, 3]; EE = gx_t[:, 4]
                k.act(LF, gat_t[:, :, 4:8], AF.Sigmoid, [GATES], [GX])
                k.act(LF, LF, AF.Ln, [GX], [GX])
                ps = PS[4]
                for i in range(NT):
                    k.mm(ps, ps[:, i * 8:i * 8 + 4], tri_t[:], gx_t[:, 0, i, :], [TRI, GX], start=True, stop=True)
                    k.mm(ps, ps[:, i * 8 + 4:i * 8 + 8], ones_t[:], gx_t[:, 0, i, :], [ONES, GX], start=True, stop=True)
                psv = ps[:, 0:128].rearrange("p (n c) -> p n c", n=NT)
                k.copy('dve', BC, psv[:, :, 0:4], [ps], [GX])
                k.act(GG, psv[:, :, 4:8], AF.Exp, [ps], [GX])
                k.tt(AA, gat_t[:, :, 0:4], BC, ALU.subtract, [GATES, GX], [GX])
                k.act(AA, AA, AF.Exp, [GX], [GX])
                k.act(EE, BC, AF.Exp, [GX], [GX])
                k.act(LF, BC, AF.Exp, [GX], [GX], scale=-1.0)

                for h in range(4):
                    for n in range(4):
                        acc = ROW[n % 2]
                        k.ts(acc[:, 0:512], um_v[:, h, n * 512:n * 512 + 512], sm_t[:, h:h + 1], None,
                             ALU.mult, None, [UM[h], SM], [acc])
                        for j in range(1, 4):
                            k.stt(acc[:, 0:512], um_v[:, h, n * 512 + j:n * 512 + j + 512],
                                  sm_t[:, j * 4 + h:j * 4 + h + 1], acc[:, 0:512], ALU.mult, ALU.add,
                                  [UM[h], SM, acc], [acc])
                        k.act(c_v[:, n * 512:(n + 1) * 512], acc[:, 0:512], AF.Silu, [acc, SM], [CC],
                              bias=sm_t[:, 16 + h:17 + h])
                    for n in range(4):
                        ps = PS[n % 4]
                        k.mm(ps, ps[:], wqk_t[:, 0, h, :], c_v[:, n * 512:(n + 1) * 512], [WQK, CC])
                        evac(qt_v[:, n * 512:(n + 1) * 512], ps[:], [ps], [QT])
                        ps = PS[(n + 2) % 4]
                        k.mm(ps, ps[:], wqk_t[:, 1, h, :], c_v[:, n * 512:(n + 1) * 512], [WQK, CC])
                        k.act(kt_v[:, n * 512:(n + 1) * 512], ps[:], AF.Copy, [ps], [KT], scale=128.0 ** -0.5)
                    for i4 in range(4):
                        ps = PS[i4 % 4]
                        for q in range(4):
                            i = i4 * 4 + q
                            k.mm(ps, ps[:, q * 128:(q + 1) * 128], c_v[:, i * 128:(i + 1) * 128], wqk_t[:, 1, h, :],
                                 [CC, WQK])
                        k.act(ktm_t[:, i4 * 4:i4 * 4 + 4, :], ps[:].rearrange("p (q c) -> p q c", q=4), AF.Copy,
                              [ps], [KTM], scale=128.0 ** -0.5)
                    k.op('pool', lambda e: e.memset(cst_t[:], 0.0), writes=[CST])
                    k.op('pool', lambda e: e.memset(cbf_t[:], 0.0), writes=[CBF])
                    PSN3 = [PS[6], PS[7], PS[3]]

                    def stage1(i, h=h):
                        sl = slice(i * 128, (i + 1) * 128)
                        b2 = i % 2
                        psS = PS[4 + b2]; psN = PSN3[i % 3]; psC = PS[b2]
                        st = STB[b2]; wv = WV[b2]
                        k.mm(psS, psS[:, 0:128], kt_v[:, sl], qt_v[:, sl], [KT, QT])
                        k.tt(st[:], psS[:, 0:128], tri_t[:], ALU.mult, [psS, TRI], [st])
                        k.act(wv[:], vext_v[:, i, h, :], AF.Copy, [VEXT[i], GX], [wv], scale=gx_t[:, 3, i, h:h + 1])
                        k.mm(psN, psN[:, 0:129], st[:], wv[:], [st, wv], start=True, stop=False)
                        k.mm(psC, psC[:, 0:129], ktm_t[:, i, :], wv[:], [KTM, wv])

                    def stage2a(i, h=h):
                        sl = slice(i * 128, (i + 1) * 128)
                        psN = PSN3[i % 3]; psC = PS[i % 2]
                        k.mm(psN, psN[:, 0:129], qt_v[:, sl], cbf_t[:], [QT, CBF], start=False, stop=True)
                        ip = max(i - 1, 0)
                        k.stt(cst_t[:], cst_t[:], gx_t[:, 2, ip, h:h + 1], psC[:, 0:129], ALU.mult, ALU.add, [CST, GX, psC], [CST])
                        k.act(cbf_t[:], cst_t[:], AF.Copy, [CST, GX], [CBF], scale=gx_t[:, 2, i, h:h + 1])

                    def stage2b(i, h=h):
                        b2 = i % 2
                        psN = PSN3[i % 3]
                        hh = HH[b2]; ms = MS[b2]
                        k.act(ms[:, 3:4], psN[:, 128:129], AF.Abs, [psN], [ms])
                        k.ts(ms[:, 0:1], ms[:, 3:4], gx_t[:, 0, i, h:h + 1], None, ALU.max, None, [ms, GX], [ms])
                        k.op('dve', lambda e, ms=ms: e.reciprocal(out=ms[:, 2:3], in_=ms[:, 0:1]), reads=[ms], writes=[ms])
                        k.stt(hh[:], psN[:, 0:128], ms[:, 2:3], sigo_v[:, i, h * 128:(h + 1) * 128], ALU.mult, ALU.mult,
                              [psN, ms, SIGO[i]], [hh])
                        k.op('dve', lambda e, ms=ms, hh=hh: e.bn_stats(out=ms[:, 4:10], in_=hh[:]), reads=[hh, ms], writes=[ms])
                        k.op('dve', lambda e, ms=ms: e.bn_aggr(out=ms[:, 10:12], in_=ms[:, 4:10]), reads=[ms], writes=[ms])
                        k.rsqrt_eps(ms[:, 12:13], ms[:, 11:12], [ms], [ms])

                    def stage3(i, h=h):
                        sl = slice(i * 128, (i + 1) * 128)
                        b2 = i % 2
                        psT = PS[2]; hh = HH[b2]; ms = MS[b2]
                        k.ts(hh[:], hh[:], ms[:, 10:11], ms[:, 12:13], ALU.subtract, ALU.mult, [hh, ms], [hh])
                        k.tr(psT, psT[:, 0:128], hh[:], IDENT, [hh])
                        k.act(xt_t[:, h, sl], psT[:, 0:128], AF.Copy, [psT, SM], [XT[i // 4]], scale=sm_t[:, 20 + h:21 + h])

                    stage1(0)
                    for it in range(NT + 2):
                        if it + 1 < NT:
                            stage1(it + 1)
                        if it < NT:
                            stage2a(it)
                        if 0 <= it - 1 < NT:
                            stage2b(it - 1)
                        if 0 <= it - 2 < NT:
                            stage3(it - 2)
                k.barrier()

                for (c0, src) in ((64, lam_re), (80, lam_im), (96, logdt_x)):
                    k.dma(sync, sm_t[:, c0:c0 + 16], src[li].rearrange("(j q) -> q j", q=128), DI, SM, parts=True,
                          allow_slow_non_contiguous=True)
                LR = sm_t[:, 64:80]; LI = sm_t[:, 80:96]; LDT = sm_t[:, 96:112]
                c = lambda a: sm_t[:, a:a + 16]
                DT = c(112); LRDT = c(128); LIDT = c(144); ER = c(160); CO = c(176); SI = c(192); AR = c(208); AI = c(224)
                MAG = c(240); XRr = c(256); T1 = c(272); T2 = c(288); CR = c(304); CI = c(320)
                VC128 = c(336); VS128 = c(352); ZR = c(368); ZI = c(384); ZT = c(400); ZT2 = c(416); T3 = c(432)
                smo = lambda o, a, f, **kw: k.act(o, a, f, [SM], [SM], **kw)
                smt = lambda o, a, b, op: k.tt(o, a, b, op, [SM], [SM])
                sms = lambda o, a, s1, s2, op0, op1: k.ts(o, a, s1, s2, op0, op1, [SM], [SM])
                smo(DT, LDT, AF.Exp)
                smt(LRDT, LR, DT, ALU.mult); smt(LIDT, LI, DT, ALU.mult)
                smo(ER, LRDT, AF.Exp)

                def sincos(o_sin, o_cos, ang, tmp):
                    sms(smi_t[:], ang, 1.0 / (2 * PI), None, ALU.mult, None)
                    k.stt(tmp, smi_t[:], -2 * PI, ang, ALU.mult, ALU.add, [SM], [SM])
                    smo(o_sin, tmp, AF.Sin, scale=0.999999)
                    sms(o_cos, ang, 0.5 * PI, None, ALU.add, None)
                    sms(smi_t[:], o_cos, 1.0 / (2 * PI), None, ALU.mult, None)
                    k.stt(tmp, smi_t[:], -2 * PI, o_cos, ALU.mult, ALU.add, [SM], [SM])
                    smo(o_cos, tmp, AF.Sin, scale=0.999999)
                sincos(SI, CO, LIDT, T1)
                smt(AR, ER, CO, ALU.mult); smt(AI, ER, SI, ALU.mult)
                smt(MAG, LR, LR, ALU.mult); smt(T1, LI, LI, ALU.mult); smt(MAG, MAG, T1, ALU.add)
                k.op('dve', lambda e: e.reciprocal(out=MAG, in_=MAG), reads=[SM], writes=[SM])
                sms(XRr, AR, -1.0, None, ALU.add, None)
                smt(T1, XRr, LR, ALU.mult); smt(T2, AI, LI, ALU.mult); smt(T1, T1, T2, ALU.add); smt(CR, T1, MAG, ALU.mult)
                smt(T1, AI, LR, ALU.mult); smt(T2, XRr, LI, ALU.mult); smt(T1, T1, T2, ALU.subtract); smt(CI, T1, MAG, ALU.mult)
                sms(T3, LIDT, 128.0, None, ALU.mult, None)
                sincos(VS128, VC128, T3, T1)
                smo(T2, LRDT, AF.Exp, scale=128.0)
                smt(VC128, VC128, T2, ALU.mult); smt(VS128, VS128, T2, ALU.mult)
                k.dma(sync, cri_d[0].rearrange("(j q) -> q j", q=128), CR, SM, CRI, allow_slow_non_contiguous=True)
                k.dma(sync, cri_d[1].rearrange("(j q) -> q j", q=128), CI, SM, CRI, parts=True, allow_slow_non_contiguous=True)
                for ct in range(4):
                    cs = slice(ct * 512, (ct + 1) * 512)
                    k.dma(sync, TP[0][:], brp[li][:, cs], DI, TP[0])
                    k.dma(sync, TP[1][:], bip[li][:, cs], DI, TP[1])
                    k.dma(sync, TP[2][:], cri_d[0, cs].partition_broadcast(128), CRI, TP[2])
                    k.dma(sync, TP[3][:], cri_d[1, cs].partition_broadcast(128), CRI, TP[3])
                    k.tt(TP[4][:], TP[2][:], TP[0][:], ALU.mult, [TP[2], TP[0]], [TP[4]])
                    k.tt(TP[5][:], TP[3][:], TP[1][:], ALU.mult, [TP[3], TP[1]], [TP[5]])
                    k.tt(bb_v[:, 0, cs], TP[4][:], TP[5][:], ALU.subtract, [TP[4], TP[5]], [BB])
                    k.tt(TP[4][:], TP[2][:], TP[1][:], ALU.mult, [TP[2], TP[1]], [TP[4]])
                    k.tt(TP[5][:], TP[3][:], TP[0][:], ALU.mult, [TP[3], TP[0]], [TP[5]])
                    k.tt(bb_v[:, 1, cs], TP[4][:], TP[5][:], ALU.add, [TP[4], TP[5]], [BB])
                for jb in range(4):
                    jq = slice(jb * 4, jb * 4 + 4)
                    t1_ = TP[6 + jb % 2]; t2_ = TP[8 + jb % 2]
                    v1_ = t1_[:].rearrange("p (j c) -> p j c", j=4); v2_ = t2_[:].rearrange("p (j c) -> p j c", j=4)
                    k.dma(sync, v1_, crp[li][:, jq, :], DI, t1_)
                    k.dma(sync, v2_, cip[li][:, jq, :], DI, t2_)
                    k.copy('act', cw_t[:, 0, jq, :], v1_, [t1_], [CW])
                    k.act(cw_t[:, 1, jq, :], v1_, AF.Copy, [t1_], [CW], scale=-1.0)
                    k.act(cw_t[:, 2, jq, :], v2_, AF.Copy, [t2_], [CW], scale=-1.0)
                for jb in range(4):
                    jsl = slice(jb * 4, jb * 4 + 4)
                    v3 = lambda t: t[:].rearrange("p (j c) -> p j c", j=4)
                    taub = tau_t[:].unsqueeze(1).broadcast_to([128, 4, 128])
                    lidb = sm_t[:, 144 + jb * 4:144 + jb * 4 + 4].unsqueeze(2).broadcast_to([128, 4, 128])
                    lrdb = sm_t[:, 128 + jb * 4:128 + jb * 4 + 4].unsqueeze(2).broadcast_to([128, 4, 128])
                    ANG, TMPa, SINT, COST, LTt, MAGP, MAGN = TP[0], TP[1], TP[2], TP[3], TP[4], TP[5], TP[6]
                    k.tt(v3(ANG), taub, lidb, ALU.mult, [TAU, SM], [ANG])
                    KI = TP[7]; kiv = KI[:].bitcast(mybir.dt.int32)
                    k.ts(kiv, ANG[:], 1.0 / (2 * PI), None, ALU.mult, None, [ANG], [KI])
                    k.stt(TMPa[:], kiv, -2 * PI, ANG[:], ALU.mult, ALU.add, [KI, ANG], [TMPa])
                    k.act(SINT[:], TMPa[:], AF.Sin, [TMPa], [SINT], scale=0.999999)
                    k.ts(ANG[:], ANG[:], 0.5 * PI, None, ALU.add, None, [ANG], [ANG])
                    k.ts(kiv, ANG[:], 1.0 / (2 * PI), None, ALU.mult, None, [ANG], [KI])
                    k.stt(TMPa[:], kiv, -2 * PI, ANG[:], ALU.mult, ALU.add, [KI, ANG], [TMPa])
                    k.act(COST[:], TMPa[:], AF.Sin, [TMPa], [COST], scale=0.999999)
                    k.tt(v3(LTt), taub, lrdb, ALU.mult, [TAU, SM], [LTt])
                    k.act(MAGP[:], LTt[:], AF.Exp, [LTt], [MAGP])
                    k.act(MAGN[:], LTt[:], AF.Exp, [LTt], [MAGN], scale=-1.0)
                    k.tt(tab_v[:, 0, jsl, :], v3(MAGN), v3(COST), ALU.mult, [MAGN, COST], [TAB])
                    k.tt(tab_v[:, 1, jsl, :], v3(MAGN), v3(SINT), ALU.mult, [MAGN, SINT], [TAB])
                    k.tt(tab_v[:, 2, jsl, :], v3(MAGP), v3(COST), ALU.mult, [MAGP, COST], [TAB])
                    k.tt(tab_v[:, 3, jsl, :], v3(MAGP), v3(SINT), ALU.mult, [MAGP, SINT], [TAB])
                k.dma('pool', ew_t[0][:, 0:2048].rearrange("p (k c) -> p k c", k=4),
                      w_glu[li].rearrange("(k p) c -> p k c", p=128), DI, EW[0])
                WGLU = ew_t[0][:, 0:2048].rearrange("p (k c) -> p k c", k=4)
                k.op('pool', lambda e: e.memset(sm_t[:, 368:400], 0.0), reads=[SM], writes=[SM])
                P1, P2, P3, P4, SR, SIi = TP[0], TP[1], TP[2], TP[3], TP[4], TP[5]
                GYF = [TP[6], TP[7], TP[10], TP[11]]
                YF = TP2[0]; TQ = TP2[1]
                b4 = lambda ap: ap.unsqueeze(1).broadcast_to([128, 4, 128])
                v4 = lambda t: t[:].rearrange("p (c t) -> p c t", c=4)
                for n in range(4):
                    ns = slice(n * 512, (n + 1) * 512)
                    for ct in range(4):
                        psY = PS[4 + ct % 2]
                        for g2 in range(2):
                            grp = []
                            for q in range(2):
                                jj = 2 * g2 + q
                                j = ct * 4 + jj
                                psR = PS[q]; psI = PS[2 + q]
                                Pq = TP[0:4] if q == 0 else TP2
                                Sq = (TP[4], TP[8]) if q == 0 else (TP[5], TP[9])
                                k.mm(psR, psR[:], bb_v[:, 0, j * 128:(j + 1) * 128], ust_t[:, ct, ns], [BB, UST[n]])
                                k.mm(psI, psI[:], bb_v[:, 1, j * 128:(j + 1) * 128], ust_t[:, ct, ns], [BB, UST[n]])
                                pr = psR[:].rearrange("p (c t) -> p c t", c=4); pi_ = psI[:].rearrange("p (c t) -> p c t", c=4)
                                k.tt(v4(Pq[0]), pr, b4(tab_v[:, 0, j, :]), ALU.mult, [psR, TAB], [Pq[0]])
                                k.tt(v4(Pq[1]), pi_, b4(tab_v[:, 1, j, :]), ALU.mult, [psI, TAB], [Pq[1]])
                                k.tt(v4(Pq[2]), pi_, b4(tab_v[:, 0, j, :]), ALU.mult, [psI, TAB], [Pq[2]])
                                k.tt(v4(Pq[3]), pr, b4(tab_v[:, 1, j, :]), ALU.mult, [psR, TAB], [Pq[3]])
                                grp.append((jj, j, Pq, Sq))
                            for cc in range(4):
                                cs = slice(cc * 128, (cc + 1) * 128)
                                last = cc * 128 + 127
                                for (jj, j, Pq, Sq) in grp:
                                    k.op('dve', lambda e, cs=cs, j=j, Pq=Pq, Sq=Sq: e.tensor_tensor_scan(
                                        out=Sq[0][:, cs], data0=Pq[0][:, cs], data1=Pq[1][:, cs], initial=sm_t[:, 368 + j:369 + j],
                                        op0=ALU.add, op1=ALU.add), reads=[Pq[0], Pq[1], SM], writes=[Sq[0]])
                                    k.op('dve', lambda e, cs=cs, j=j, Pq=Pq, Sq=Sq: e.tensor_tensor_scan(
                                        out=Sq[1][:, cs], data0=Pq[2][:, cs], data1=Pq[3][:, cs], initial=sm_t[:, 384 + j:385 + j],
                                        op0=ALU.add, op1=ALU.subtract), reads=[Pq[2], Pq[3], SM], writes=[Sq[1]])
                                j0 = grp[0][1]
                                s4 = tp_v[:, 4:12, :].rearrange("p (a c) t -> p a c t", c=4)[:, :, 0:2, last]
                                Sall = [grp[0][3][0], grp[0][3][1], grp[1][3][0], grp[1][3][1]]
                                vc2 = sm_t[:, 336 + j0:338 + j0].unsqueeze(1).broadcast_to([128, 2, 2])
                                vs2 = sm_t[:, 352 + j0:354 + j0].unsqueeze(1).broadcast_to([128, 2, 2])
                                pz = sm_t[:, 400:404].rearrange("p (a c) -> p a c", a=2)
                                qz = sm_t[:, 416:420].rearrange("p (a c) -> p a c", a=2)
                                k.tt(pz, s4, vc2, ALU.mult, Sall + [SM], [SM])
                                k.tt(qz, s4, vs2, ALU.mult, Sall + [SM], [SM])
                                k.tt(sm_t[:, 368 + j0:370 + j0], sm_t[:, 400:402], sm_t[:, 418:420], ALU.subtract, [SM], [SM])
                                k.tt(sm_t[:, 384 + j0:386 + j0], sm_t[:, 402:404], sm_t[:, 416:418], ALU.add, [SM], [SM])
                            for (jj, j, Pq, Sq) in grp:
                                SRq, SIq = Sq
                                k.tt(v4(RB[0]), v4(SRq), b4(tab_v[:, 2, j, :]), ALU.mult, [SRq, TAB], [RB[0]], eng='pool')
                                k.tt(v4(RB[1]), v4(SIq), b4(tab_v[:, 3, j, :]), ALU.mult, [SIq, TAB], [RB[1]], eng='pool')
                                k.tt(v4(RB[2]), v4(SIq), b4(tab_v[:, 2, j, :]), ALU.mult, [SIq, TAB], [RB[2]], eng='pool')
                                k.tt(v4(RB[3]), v4(SRq), b4(tab_v[:, 3, j, :]), ALU.mult, [SRq, TAB], [RB[3]], eng='pool')
                                for r, wsel in enumerate((0, 1, 2, 2)):
                                    k.mm(psY, psY[:], cw_t[:, wsel, j, :], RB[r][:], [CW, RB[r]],
                                         start=(jj == 0 and r == 0), stop=(jj == 3 and r == 3))
                        k.stt(YF[:], ust_t[:, ct, ns], sm_t[:, 24 + ct:25 + ct], psY[:], ALU.mult, ALU.add,
                              [UST[n], SM, psY], [YF])
                        k.tt(TQ[:], YF[:], YF[:], ALU.mult, [YF], [TQ], eng='pool')
                        k.ts(TQ[:], TQ[:], 0.044715, 1.0, ALU.mult, ALU.add, [TQ], [TQ], eng='pool')
                        k.tt(TQ[:], TQ[:], YF[:], ALU.mult, [TQ, YF], [TQ], eng='pool')
                        k.act(TQ[:], TQ[:], AF.Sigmoid, [TQ], [TQ], scale=2.0 * math.sqrt(2.0 / PI))
                        k.tt(GYF[ct][:], YF[:], TQ[:], ALU.mult, [YF, TQ], [GYF[ct]], eng='pool')
                        k.copy('act', gyb_v[:, ct, :], GYF[ct][:], [GYF[ct]], [GYB])
                    for cto in range(4):
                        ps = PS[6 + cto % 2]
                        for ci in range(4):
                            k.mm(ps, ps[:], WGLU[:, ci, cto * 128:(cto + 1) * 128], gyb_v[:, ci, :], [EW[0], GYB],
                                 start=(ci == 0), stop=(ci == 3))
                        k.act(TQ[:], ps[:], AF.Sigmoid, [ps, SM], [TQ], bias=sm_t[:, 28 + cto:29 + cto])
                        k.tt(GYF[cto][:], GYF[cto][:], TQ[:], ALU.mult, [GYF[cto], TQ], [GYF[cto]])
                        k.act(TP[cto][:], GYF[cto][:], AF.Square, [GYF[cto]], [TP[cto]])
                    ps = PS[6]
                    for ct in range(4):
                        k.mm(ps, ps[:], onesk_t[:], TP[ct][:], [ONESK, TP[ct]], start=(ct == 0), stop=(ct == 3))
                    k.rsqrt_eps(YF[:], ps[:], [ps], [YF])
                    for ct in range(4):
                        k.stt(xt_t[:, 4 + ct, ns], GYF[ct][:], sm_t[:, 32 + ct:33 + ct], YF[:], ALU.mult, ALU.mult,
                              [GYF[ct], SM, YF], [XT[n]])
                k.barrier()

                load_row(ROW[0], ln1_g[li]); load_row(ROW[1], ln1_b[li])
                for hf in range(2):
                    k.dma('pool', ew_t[hf][:, 0:4096].rearrange("p (k c) -> p k c", k=8),
                          w_out[li][:, hf * 512:(hf + 1) * 512].rearrange("(k p) c -> p k c", p=128), DI, EW[hf])
                for i in range(NT):
                    xlt = XLB[1]
                    xsv = xs_d.rearrange("(n p) d -> p n d", p=128)
                    k.dma(sync, xlt[:], xsv[:, i, :], XS, xlt)
                    for hf in range(2):
                        ps = PS[(2 * i + hf) % 4]
                        W = ew_t[hf][:, 0:4096].rearrange("p (k c) -> p k c", k=8)
                        for kk in range(8):
                            k.mm(ps, ps[:], xt_t[:, kk, i * 128:(i + 1) * 128], W[:, kk, :], [XT[i // 4], EW[hf]],
                                 start=(kk == 0), stop=(kk == 7))
                        k.stt(X[i][:, hf * 512:(hf + 1) * 512], xlt[:, hf * 512:(hf + 1) * 512], ALPHA, ps[:], ALU.mult, ALU.add, [xlt, ps], [X[i]])
                    layer_norm_tile(i, ROW[0], ROW[1])
                k.barrier()

            if 'B' in phases:
                k.dma(sync, wr_t[:], w_r[li].rearrange("(k p) c -> p k c", p=128), DI, WR)
                k.dma(sync, gx_t[:].rearrange("p a n c -> p (a n c)")[:, 0:36], b_r[li].partition_broadcast(128), DI, GX)
                BIASR = gx_t[:].rearrange("p a n c -> p (a n c)")[:, 0:36]
                POS = k.tile(gw_t[:], 'POS')
                lgg_v = hh_t[:, 0, 0:64].rearrange("p (n g) -> p n g", n=NT)
                ej_v = hh_t[:, 0, 64:128].rearrange("p (n g) -> p n g", n=NT)
                oh_v = hh_t[:, 1, 0:64].rearrange("p (n g) -> p n g", n=NT)
                mk_v = hh_t[:, 1, 64:128].rearrange("p (n g) -> p n g", n=NT)
                vec = lambda a: sm_t[:, 256 + a * 16:256 + (a + 1) * 16]
                GMX, SEv, Dv, W1v, W2v, C1v, C2v = [vec(a) for a in range(7)]
                em_v = sm2_t[:].rearrange("p (n e) -> p n e", n=NT)
                m8_v = gat_t

                def routing(i):
                    ps = PS[4 + i % 2]
                    for kk in range(8):
                        k.mm(ps, ps[:, 0:36], xr_t[:, kk, :], wr_t[:, kk, :], [XR, WR], start=(kk == 0), stop=(kk == 7))
                    k.tt(lgg_v[:, i, :], ps[:, 0:4], BIASR[:, 0:4], ALU.add, [ps, GX], [HH[0]])
                    k.tt(gw_t[:, i, :], ps[:, 4:36], BIASR[:, 4:36], ALU.add, [ps, GX], [POS])

                def routing_batched():
                    b3 = lambda ap, n: ap.unsqueeze(2).broadcast_to([128, NT, n])
                    k.op('dve', lambda e: e.tensor_reduce(out=GMX, in_=lgg_v, axis=mybir.AxisListType.X, op=ALU.max),
                         reads=[HH[0]], writes=[SM])
                    k.tt(lgg_v, lgg_v, b3(GMX, 4), ALU.subtract, [HH[0], SM], [HH[0]])
                    k.act(ej_v, lgg_v, AF.Exp, [HH[0]], [HH[0]])
                    k.op('dve', lambda e: e.tensor_reduce(out=SEv, in_=ej_v, axis=mybir.AxisListType.X, op=ALU.add),
                         reads=[HH[0]], writes=[SM])
                    k.op('dve', lambda e: e.reciprocal(out=SEv, in_=SEv), reads=[SM], writes=[SM])
                    k.ts(oh_v, lgg_v, 0.0, None, ALU.is_equal, None, [HH[0]], [HH[1]])
                    k.ts(mk_v, oh_v, -1.0, 1e30, ALU.add, ALU.mult, [HH[1]], [HH[1]])
                    k.tt(sm2_t[:].rearrange("p (n g e) -> p n g e", n=NT, g=4),
                         gw_t[:].rearrange("p n (g e) -> p n g e", g=4),
                         mk_v.unsqueeze(3).broadcast_to([128, NT, 4, 8]), ALU.add, [POS, HH[1]], [SM])
                    for i in range(NT):
                        k.op('dve', lambda e, i=i: e.max(out=m8_v[:, i, :], in_=em_v[:, i, :]), reads=[SM], writes=[GATES])
                    V1 = m8_v[:, :, 0]; V2 = m8_v[:, :, 1]
                    k.tt(Dv, V1, V2, ALU.subtract, [GATES], [SM])
                    k.act(W1v, Dv, AF.Sigmoid, [SM], [SM])
                    k.ts(W2v, W1v, -1.0, 1.0, ALU.mult, ALU.add, [SM], [SM])
                    k.tt(C1v, W1v, SEv, ALU.mult, [SM], [SM]); k.tt(C2v, W2v, SEv, ALU.mult, [SM], [SM])
                    k.tt(gw_t[:], em_v, b3(V1, 32), ALU.is_equal, [SM, GATES], [POS])
                    k.tt(gw_t[:], gw_t[:], b3(C1v, 32), ALU.mult, [POS, SM], [POS])
                    k.tt(meta_v[:, :, :, 0], em_v, b3(V2, 32), ALU.is_equal, [SM, GATES], [META])
                    k.tt(em_v, meta_v[:, :, :, 0], b3(C2v, 32), ALU.mult, [META, SM], [SM])
                    k.tt(gw_t[:], gw_t[:], em_v, ALU.add, [POS, SM], [POS])
                    k.copy('dve', meta_v[:, :, :, 1], gw_t[:], [POS], [META])
                    k.ts(mall_v, gw_t[:], 0.0, None, ALU.is_gt, None, [POS], [MALL])

                k.op('pool', lambda e: e.memset(out_v[0], 0.0), writes=[OUTB[0]])
                ydv = yd_d.rearrange("(n p) d -> p n d", p=128)
                for r in range(32):
                    k.dma(sync, ydv[:, r, :], out_v[0], OUTB[0], YD, parts=True)
                k._wait('pool', (YD.dsem, YD.dval, None, 'd_yd'))
                for i in range(NT):
                    for half in range(2):
                        ps = PS[(2 * i + half) % 4]
                        for q in range(4):
                            kk = half * 4 + q
                            k.tr(ps, ps[:, q * 128:(q + 1) * 128], X[i][:, kk * 128:(kk + 1) * 128], IDENT, [X[i]])
                        k.copy('dve', xr_t[:, half * 4:half * 4 + 4, :], ps[:].rearrange("p (q t) -> p q t", q=4), [ps], [XR])
                    k.copy('act', xtm_v[:, i, :], X[i][:], [X[i]], [XTM[i]])
                    routing(i)
                routing_batched()
                for i in range(NT):
                    ps = PS[4 + i % 2]
                    k.mm(ps, ps[:, 0:32], stri_t[:], mall_v[:, i, :], [STRI, MALL], start=True, stop=(i == 0))
                    for j in range(i):
                        k.mm(ps, ps[:, 0:32], onesb_t[:], mall_v[:, j, :], [ONESB, MALL], start=False, stop=(j == i - 1))
                    k.stt(gw_t[:, i, :], ps[:, 0:32], 1.0, mall_v[:, i, :], ALU.add, ALU.mult, [ps, MALL], [POS])
                k.ts(gw_t[:].rearrange("p n e -> p (n e)"), gw_t[:].rearrange("p n e -> p (n e)"), -1.0, None, ALU.add, None,
                     [POS], [POS])

                k.op('pool', lambda e: e.memset(out_v[0], 0.0), writes=[OUTB[0]])
                ydv = yd_d.rearrange("(n p) d -> p n d", p=128)
                for r in range(32):
                    k.dma(sync, ydv[:, r, :], out_v[0], OUTB[0], YD, parts=True)
                k._wait('pool', (YD.dsem, YD.dval, None, 'd_yd'))
                for i in range(NT):
                    for half in range(2):
                        ps = PS[(2 * i + half) % 4]
                        for q in range(4):
                            kk = half * 4 + q
                            k.tr(ps, ps[:, q * 128:(q + 1) * 128], X[i][:, kk * 128:(kk + 1) * 128], IDENT, [X[i]])
                        k.copy('dve', xr_t[:, half * 4:half * 4 + 4, :], ps[:].rearrange("p (q t) -> p q t", q=4), [ps], [XR])
                    k.copy('act', xtm_v[:, i, :], X[i][:], [X[i]], [XTM[i]])
                    routing(i)
                routing_batched()
                for i in range(NT):
                    ps = PS[4 + i % 2]
                    k.mm(ps, ps[:, 0:32], stri_t[:], mall_v[:, i, :], [STRI, MALL], start=True, stop=(i == 0))
                    for j in range(i):
                        k.mm(ps, ps[:, 0:32], onesb_t[:], mall_v[:, j, :], [ONESB, MALL], start=False, stop=(j == i - 1))
                    k.stt(gw_t[:, i, :], ps[:, 0:32], 1.0, mall_v[:, i, :], ALU.add, ALU.mult, [ps, MALL], [POS])
                k.ts(gw_t[:].rearrange("p n e -> p (n e)"), gw_t[:].rearrange("p n e -> p (n e)"), -1.0, None, ALU.add, None,
                     [POS], [POS])

                def load_expert(e, buf):
                    k.dma('pool', ew_t[buf][:, 0:4096].rearrange("p (k c) -> p k c", k=8),
                          w_eg[li, e].rearrange("(k p) c -> p k c", p=128), DI, EW[buf])
                    k.dma('pool', ew_t[buf][:, 4096:8192].rearrange("p (k c) -> p k c", k=8),
                          w_eu[li, e].rearrange("(k p) c -> p k c", p=128), DI, EW[buf], parts=True)
                    k.dma('pool', ew_t[buf][:, 8192:12288].rearrange("p (k c) -> p k c", k=4),
                          w_ed[li, e].rearrange("(k p) c -> p k c", p=128), DI, EW[buf], parts=True)

                if n_experts > 0:
                    load_expert(0, 0)
                for e_ in range(n_experts):
                    buf = e_ % 2
                    if e_ + 1 < n_experts:
                        load_expert(e_ + 1, 1 - buf)
                    WG = ew_t[buf][:, 0:4096].rearrange("p (k c) -> p k c", k=8)
                    WU = ew_t[buf][:, 4096:8192].rearrange("p (k c) -> p k c", k=8)
                    WD = ew_t[buf][:, 8192:12288].rearrange("p (k c) -> p k c", k=4)
                    for i in range(NT):
                        selt = ROW[i // 8]
                        k.ts(sel_v[i // 8][:, i % 8, :], iota_t[:], gw_t[:, i, e_:e_ + 1], None, ALU.is_equal, None,
                             [IOTA, POS], [selt], eng=('dve' if (i % 2 == 0 or _os.environ.get('KSEL', 'dve') == 'dve') else 'pool'))
                    xg = XG[buf]; xgv = xg_v[buf]
                    for kf in range(8):
                        ps = PS[kf % 4]
                        for i in range(NT):
                            k.mm(ps, ps[:, 0:256], xtm_v[:, i, kf * 128:(kf + 1) * 128], sel_v[i // 8][:, i % 8, :],
                                 [XTM[i], ROW[i // 8]], start=(i == 0), stop=(i == NT - 1))
                        evac(xgv[:, kf, :], ps[:, 0:256], [ps], [xg])
                    psM = PS[4]
                    for sh in range(2):
                        for i in range(NT):
                            k.mm(psM, psM[:, sh * 8:sh * 8 + 3], sel_v[i // 8][:, i % 8, sh * 128:(sh + 1) * 128],
                                 cmeta_t[:, i, :], [ROW[i // 8], CMETA], start=(i == 0), stop=(i == NT - 1))
                        for i in range(NT):
                            k.mm(psM, psM[:, sh * 8 + 3:sh * 8 + 5], sel_v[i // 8][:, i % 8, sh * 128:(sh + 1) * 128],
                                 meta_v[:, i, e_, :], [ROW[i // 8], META], start=(i == 0), stop=(i == NT - 1))
                    sl = SLOT[buf]; slv = slot_t[:, buf]
                    k.copy('dve', slv.rearrange("p a c -> p (a c)"), psM[:, 0:16], [psM], [sl])
                    k.stt(slv[:, :, 5], slv[:, :, 0], 128.0, slv[:, :, 1], ALU.mult, ALU.add, [sl], [sl])
                    k.stt(slv[:, :, 5], slv[:, :, 3], 2048.0, slv[:, :, 5], ALU.mult, ALU.add, [sl], [sl])
                    k.ts(slv[:, :, 6], slv[:, :, 2], -1.0e6, 1.0e6, ALU.mult, ALU.add, [sl], [sl])
                    for sh in range(2):
                        k.tt(dest_tt[buf][sh][:, :], slv[:, sh, 5:6], slv[:, sh, 6:7], ALU.add, [sl], [DEST[buf][sh]])
                    hte = HTE[buf]; htev = hte_v[buf]
                    for m in range(4):
                        psg = PS[5 + (m % 2) * 2 - (m % 2)]; psu = PS[6 + (m % 2)]
                        psg = PS[4 + (m % 2) * 2 + 1] if False else PS[5] if m % 2 == 0 else PS[7]
                        psu = PS[6] if m % 2 == 0 else PS[4]
                        for kk in range(8):
                            k.mm(psg, psg[:, 0:256], WG[:, kk, m * 128:(m + 1) * 128], xgv[:, kk, :], [EW[buf], xg],
                                 start=(kk == 0), stop=(kk == 7))
                        for kk in range(8):
                            k.mm(psu, psu[:, 256:512], WU[:, kk, m * 128:(m + 1) * 128], xgv[:, kk, :], [EW[buf], xg],
                                 start=(kk == 0), stop=(kk == 7))
                        sg = SG[m % 2]
                        k.act(sg[:], psg[:, 0:256], AF.Silu, [psg], [sg])
                        k.tt(htev[:, m, :], sg[:], psu[:, 256:512], ALU.mult, [sg, psu], [hte])
                    for sh in range(2):
                        ob = OUTB[sh]
                        for hf in range(2):
                            ps = PS[(sh * 2 + hf) % 4]
                            for m in range(4):
                                k.mm(ps, ps[:], htev[:, m, sh * 128:(sh + 1) * 128], WD[:, m, hf * 512:(hf + 1) * 512],
                                     [hte, EW[buf]], start=(m == 0), stop=(m == 3))
                            k.act(out_v[sh][:, hf * 512:(hf + 1) * 512], ps[:], AF.Copy, [ps, sl], [ob], scale=slv[:, sh, 4:5])
                        k.dma_scatter(yd_d[:, :], dest_tt[buf][sh][:, :], out_v[sh], ob, DEST[buf][sh], YD, 4095)
                if _os.environ.get('KDEBUG', '') == 'dbgslot':
                    k.barrier()
                    k.copy('dve', X[0][:, 0:16], slot_t[:, 1].rearrange("p a c -> p (a c)"), [SLOT[1]], [X[0]])
                    k.copy('dve', X[0][:, 16:17], dest_tt[1][0][:, :], [DEST[1][0]], [X[0]])
                    k.copy('dve', X[0][:, 17:18], dest_tt[1][1][:, :], [DEST[1][1]], [X[0]])
                    k.copy('dve', X[0][:, 32:64], gw_t[:, 0, :], [POS], [X[0]])
                    k.copy('dve', X[0][:, 64:96], mall_v[:, 0, :], [MALL], [X[0]])
                    k.copy('dve', X[0][:, 96:128], meta_v[:, 0, :, 1], [META], [X[0]])
                    k.copy('dve', X[0][:, 128:160], gw_t[:, 15, :], [POS], [X[0]])
                load_row(ROW[0], ln2_g[li]); load_row(ROW[1], ln2_b[li])
                for i in range(NT if _os.environ.get('KDEBUG', '') != 'dbgslot' else 0):
                    for kq in range(2):
                        ob = OUTB[kq]
                        k.dma(sync, out_v[kq], ydv[:, kq * 16 + i, :], YD, ob)
                        if kq == 0:
                            k.stt(X[i][:], X[i][:], ALPHA, out_v[kq], ALU.mult, ALU.add, [X[i], ob], [X[i]])
                        else:
                            k.tt(X[i][:], X[i][:], out_v[kq], ALU.add, [X[i], ob], [X[i]], eng='pool')
                    if 'C' in phases:
                        layer_norm_tile(i, ROW[0], ROW[1])
                k.barrier()

            if 'C' in phases:
                if 'B' not in phases:
                    load_row(ROW[0], ln2_g[li]); load_row(ROW[1], ln2_b[li])
                    for i in range(NT):
                        layer_norm_tile(i, ROW[0], ROW[1])
                build_xT()
                load_row(ROW[0], b_pg[li]); load_row(ROW[1], ple_g[li])
                for hf in range(2):
                    k.dma('pool', ew_t[hf][:, 0:4096].rearrange("p (k c) -> p k c", k=8),
                          w_pg[li][:, hf * 512:(hf + 1) * 512].rearrange("(k p) c -> p k c", p=128), DI, EW[hf])
                k.dma('pool', ew_t[0][:, 4096:6144].rearrange("p (k c) -> p k c", k=2),
                      w_pp[li].rearrange("(k p) c -> p k c", p=128), DI, EW[0], parts=True)
                WPP = ew_t[0][:, 4096:6144].rearrange("p (k c) -> p k c", k=2)
                PTt = [UST[n] for n in range(4)]
                for n in range(4):
                    k.dma('pool', ust_t[:, 0:2, n * 512:(n + 1) * 512],
                          pT_in[li][:, n * 512:(n + 1) * 512].rearrange("(k p) t -> p k t", p=128), DI, UST[n])
                for i in range(NT):
                    sl = slice(i * 128, (i + 1) * 128)
                    ms = MS[i % 2]
                    psG = [PS[0 + (i % 2) * 4], PS[1 + (i % 2) * 4]]
                    psP = [PS[2 + (i % 2) * 4], PS[3 + (i % 2) * 4]]
                    for hf in range(2):
                        W = ew_t[hf][:, 0:4096].rearrange("p (k c) -> p k c", k=8)
                        for kk in range(8):
                            k.mm(psG[hf], psG[hf][:], xt_t[:, kk, sl], W[:, kk, :], [XT[i // 4], EW[hf]],
                                 start=(kk == 0), stop=(kk == 7))
                        for k2 in range(2):
                            k.mm(psP[hf], psP[hf][:], ust_t[:, k2, sl], WPP[:, k2, hf * 512:(hf + 1) * 512],
                                 [UST[i // 4], EW[0]], start=(k2 == 0), stop=(k2 == 1))
                    tq = CTMP[(i % 2) * 3]; tg = CTMP[(i % 2) * 3 + 1]; tp_ = CTMP[(i % 2) * 3 + 2]
                    for hf in range(2):
                        k.act(tq[:], psP[hf][:], AF.Square, [psP[hf]], [tq, ms], accum_out=ms[:, 16 + hf:17 + hf])
                    k.tt(ms[:, 18:19], ms[:, 16:17], ms[:, 17:18], ALU.add, [ms], [ms])
                    k.rsqrt_eps(ms[:, 19:20], ms[:, 18:19], [ms], [ms], pre_scale=1.0 / 1024.0)
                    for hf in range(2):
                        hs = slice(hf * 512, (hf + 1) * 512)
                        k.tt(tg[:], psG[hf][:], row_t[0][:, hs], ALU.add, [psG[hf], ROW[0]], [tg])
                        k.act(tg[:], tg[:], AF.Sigmoid, [tg], [tg])
                        k.stt(tp_[:], psP[hf][:], ms[:, 19:20], row_t[1][:, hs], ALU.mult, ALU.mult, [psP[hf], ms, ROW[1]], [tp_])
                        k.tt(tg[:], tg[:], tp_[:], ALU.mult, [tg, tp_], [tg], eng='pool')
                        k.tt(X[i][:, hs], X[i][:, hs], tg[:], ALU.add, [X[i], tg], [X[i]], eng='pool')
                k.barrier()

        for _pi in range(int(_os.environ.get('KPAD', '0'))):
            k.op('dve', lambda e: e.memset(sm2_t[:, 300:301], 0.0), writes=[])
        yv = y_out.rearrange("(n p) d -> p n d", p=128)
        for i in range(NT):
            k.dma(sync, yv[:, i, :], X[i][:], X[i], YO, parts=True)
        if not k.dry:
            nc.sync.wait_ge(YO.dsem, YO.dval)
    nc._declared_inputs = declared
    return nc, k.waited


def prep_shared(inp):
    f = lambda a: np.ascontiguousarray(np.asarray(a, dtype=np.float32))
    L = DEPTH
    sh = {}
    for n in ['w_in', 'conv_w', 'conv_b', 'w_q', 'w_k', 'mh_g', 'w_glu', 'b_glu', 's5_g', 'w_out', 'ln1_g', 'ln1_b',
              'w_eg', 'w_eu', 'w_ed', 'ln2_g', 'ln2_b', 'w_pg', 'b_pg', 'w_pp', 'ple_g']:
        sh[n] = f(inp[n])
    sh['bg'] = f(np.concatenate([np.asarray(inp['b_i']), np.asarray(inp['b_f'])], axis=1))
    sh['lam_re'] = f(np.asarray(inp['lam_re']).reshape(L, 2048))
    sh['lam_im'] = f(np.asarray(inp['lam_im']).reshape(L, 2048))
    sh['logdt_x'] = f(np.repeat(np.asarray(inp['log_dt'])[:, :, None], 64, axis=2).reshape(L, 2048))
    sh['d_skip'] = f(np.asarray(inp['d_skip']).reshape(L, 512))
    b_re = np.asarray(inp['b_re']); b_im = np.asarray(inp['b_im'])
    c_re = np.asarray(inp['c_re']); c_im = np.asarray(inp['c_im'])
    brp = np.zeros((L, 128, 32, 64), np.float32); bip = np.zeros((L, 128, 32, 64), np.float32)
    crp = np.zeros((L, 2, 64, 16, 128), np.float32); cip = np.zeros((L, 2, 64, 16, 128), np.float32)
    for g in range(32):
        r0 = (g % 8) * 16
        brp[:, r0:r0 + 16, g, :] = np.transpose(b_re[:, g], (0, 2, 1))
        bip[:, r0:r0 + 16, g, :] = np.transpose(b_im[:, g], (0, 2, 1))
        j, gi = g // 2, g % 2
        crp[:, gi, :, j, r0:r0 + 16] = np.transpose(c_re[:, g], (0, 2, 1))
        cip[:, gi, :, j, r0:r0 + 16] = np.transpose(c_im[:, g], (0, 2, 1))
    sh['brp'] = brp.reshape(L, 128, 2048); sh['bip'] = bip.reshape(L, 128, 2048)
    sh['crp'] = crp.reshape(L, 128, 16, 128); sh['cip'] = cip.reshape(L, 128, 16, 128)
    sh['w_r'] = f(np.concatenate([np.asarray(inp['w_grp']), np.asarray(inp['w_rt'])], axis=2))
    sh['b_r'] = f(np.concatenate([np.asarray(inp['b_grp']), np.asarray(inp['b_rt'])], axis=1))
    sh['ident'] = np.eye(128, dtype=np.float32)
    sh['tri'] = np.triu(np.ones((128, 128), np.float32))
    sh['tau'] = np.ascontiguousarray(np.broadcast_to(np.arange(128, dtype=np.float32), (128, 128)))
    sh['iota256'] = np.ascontiguousarray(np.broadcast_to(np.arange(256, dtype=np.float32), (128, 256)))
    sh['stri'] = np.triu(np.ones((128, 128), np.float32), k=1)
    cm = np.zeros((128, 16, 3), np.float32)
    cm[:, :, 0] = np.arange(16, dtype=np.float32)[None, :]
    cm[:, :, 1] = np.arange(128, dtype=np.float32)[:, None]
    cm[:, :, 2] = 1.0
    sh['cmeta'] = cm
    return sh


_NC_CACHE = {}
PER_LAYER = ['w_in', 'conv_w', 'conv_b', 'w_q', 'w_k', 'bg', 'mh_g', 'lam_re', 'lam_im', 'logdt_x', 'brp', 'bip', 'crp',
             'cip', 'd_skip', 'w_glu', 'b_glu', 's5_g', 'w_out', 'ln1_g', 'ln1_b', 'w_r', 'b_r', 'w_eg', 'w_eu', 'w_ed',
             'ln2_g', 'ln2_b', 'w_pg', 'b_pg', 'w_pp', 'ple_g']


def _launch(nc, sh, xs, pTs, li):
    names = set(nc._declared_inputs)
    base = {}
    for kname in names:
        if kname in ('x', 'pT'):
            continue
        a = sh[kname]
        base[kname] = np.ascontiguousarray(a[li:li + 1]) if kname in PER_LAYER else a
    in_maps = []
    for b in range(8):
        m = dict(base)
        m['x'] = xs[b]
        m['pT'] = pTs[b][li:li + 1]
        in_maps.append(m)
    res = run_bass_kernel_spmd(nc, in_maps, core_ids=list(range(8)))
    return [np.ascontiguousarray(res.results[b]['y']) for b in range(8)]


def kernel(**inputs):
    sh = prep_shared(inputs)
    x = np.asarray(inputs['x'], dtype=np.float32)
    p = np.asarray(inputs['p'], dtype=np.float32)
    if 'full' not in _NC_CACHE:
        _NC_CACHE['full'] = build_program()
    nc = _NC_CACHE['full']
    names = set(nc._declared_inputs)
    base = {kname: sh[kname] for kname in names if kname not in ('x', 'pT')}
    in_maps = []
    for b in range(8):
        m = dict(base)
        m['x'] = np.ascontiguousarray(x[b])
        m['pT'] = np.ascontiguousarray(np.transpose(p[:, b], (0, 2, 1)))
        in_maps.append(m)
    res = run_bass_kernel_spmd(nc, in_maps, core_ids=list(range(8)))
    return np.stack([res.results[b]['y'] for b in range(8)], axis=0).astype(np.float32)
```
